# Optimizing a Trainium2 kernel written in Bass

```python
import math
import jax
import jax.numpy as jnp
from jax import lax
import numpy as np

D_MODEL = 1024
BATCH = 8
SEQ = 2048
DEPTH = 2

GRID_W = 64
CTX_LEN = 256
N_ATTN_LAYERS = (DEPTH + 1) // 2
N_REC_LAYERS = DEPTH // 2
EPS = 1e-6
Q_BLOCK = 128
ROPE_BASE = 10000.0

DA_HEADS = 4
DA_QK = 64
DA_V = 2 * DA_QK
DA_SCALE = DA_QK ** -0.5

MLA_HEADS = 8
MLA_NOPE = 64
MLA_ROPE = 32
MLA_V = 64
MLA_Q_RANK = 384
MLA_KV_RANK = 256
MLA_SCALE = (MLA_NOPE + MLA_ROPE) ** -0.5

ATT_SPLITS = (DA_HEADS * 2 * DA_QK, DA_HEADS * 2 * DA_QK, DA_HEADS * DA_V,
              MLA_Q_RANK, MLA_KV_RANK, MLA_ROPE)
ATT_IN = 3 * DA_HEADS * 2 * DA_QK + MLA_Q_RANK + MLA_KV_RANK + MLA_ROPE
ATT_OUT = DA_HEADS * DA_V + MLA_HEADS * MLA_V

GLA_HEADS = 4
GLA_DK = 64
GLA_DV = 128
GLA_GATE_RANK = 16
GLA_GATE_NORM = 16.0

HG_HEADS = 4
HG_DF = 128
HG_DV = 128

CHUNK = 64
REC_SPLITS = (GLA_HEADS * GLA_DK, GLA_HEADS * GLA_DK, GLA_HEADS * GLA_DV, GLA_HEADS * GLA_DV,
              2 * GLA_GATE_RANK, HG_HEADS * HG_DF, 2 * HG_HEADS * HG_DF, HG_HEADS * HG_DV, HG_HEADS * HG_DV)
REC_IN = (2 * GLA_HEADS * GLA_DK + 2 * GLA_HEADS * GLA_DV + 2 * GLA_GATE_RANK
          + 3 * HG_HEADS * HG_DF + 2 * HG_HEADS * HG_DV)
REC_OUT = GLA_HEADS * GLA_DV + HG_HEADS * HG_DV

N_EXPERTS = 16
EXPERT_FF = 2816
EC_CAPACITY_FACTOR = 2

kernel_name = 'hybrid_diffusion_diffattn_mla_gla_hgrn2_ecmoe'


def _split(t, sizes):
    return jnp.split(t, np.cumsum(sizes)[:-1].tolist(), axis=-1)


def rms_norm(x, gain):
    xf = x.astype(jnp.float32)
    y = xf * lax.rsqrt(jnp.mean(xf * xf, axis=-1, keepdims=True) + EPS)
    return (y * gain.astype(jnp.float32)).astype(x.dtype)


def modulate(x, gain, shift, scale):
    return rms_norm(x, gain) * (1 + scale) + shift


def axial_rope(rows, rot_dim):
    row = jnp.repeat(jnp.arange(rows, dtype=jnp.float32), GRID_W)
    col = jnp.tile(jnp.arange(GRID_W, dtype=jnp.float32), rows)
    n_freq = rot_dim // 4
    inv = ROPE_BASE ** (-jnp.arange(n_freq, dtype=jnp.float32) / n_freq)
    ang = jnp.concatenate([row[:, None] * inv, col[:, None] * inv], axis=-1)
    return jnp.cos(ang), jnp.sin(ang)


def apply_rope(x, cos, sin):
    shape = (1, x.shape[1]) + (1,) * (x.ndim - 3) + (cos.shape[-1],)
    cs = cos.reshape(shape).astype(x.dtype)
    sn = sin.reshape(shape).astype(x.dtype)
    x1, x2 = jnp.split(x, 2, axis=-1)
    return jnp.concatenate([x1 * cs - x2 * sn, x2 * cs + x1 * sn], axis=-1)


def over_query_blocks(fn, *qs):
    bsz, n = qs[0].shape[:2]
    nb = n // Q_BLOCK
    blocks = tuple(jnp.moveaxis(q.reshape((bsz, nb, Q_BLOCK) + q.shape[2:]), 1, 0) for q in qs)
    out = lax.map(lambda blk: fn(*blk), blocks)
    return jnp.moveaxis(out, 0, 1).reshape(bsz, n, out.shape[-1])


def attn_project(h, w_in, q_norm_g, q_up, kv_norm_g, kv_up, rope):
    bsz, n, _ = h.shape
    qa, ka, va, cq, ckv, kr = _split(h @ w_in, ATT_SPLITS)
    qa = qa.reshape(bsz, n, DA_HEADS, 2, DA_QK)
    ka = ka.reshape(bsz, n, DA_HEADS, 2, DA_QK)
    va = va.reshape(bsz, n, DA_HEADS, DA_V)
    qm = (rms_norm(cq, q_norm_g) @ q_up).reshape(bsz, n, MLA_HEADS, MLA_NOPE + MLA_ROPE)
    kvm = (rms_norm(ckv, kv_norm_g) @ kv_up).reshape(bsz, n, MLA_HEADS, MLA_NOPE + MLA_V)
    q_nope, q_rope = qm[..., :MLA_NOPE], qm[..., MLA_NOPE:]
    k_nope, vm = kvm[..., :MLA_NOPE], kvm[..., MLA_NOPE:]
    k_rope = kr[:, :, None, :]
    if rope is not None:
        cos_da, sin_da, cos_r, sin_r = rope
        qa = apply_rope(qa, cos_da, sin_da)
        ka = apply_rope(ka, cos_da, sin_da)
        q_rope = apply_rope(q_rope, cos_r, sin_r)
        k_rope = apply_rope(k_rope, cos_r, sin_r)
    qm = jnp.concatenate([q_nope, q_rope], axis=-1)
    km = jnp.concatenate([k_nope, jnp.broadcast_to(k_rope, (bsz, n, MLA_HEADS, MLA_ROPE))], axis=-1)
    queries = (qa[..., 0, :], qa[..., 1, :], qm)
    keys = (ka[..., 0, :], ka[..., 1, :], va, km, vm)
    return queries, keys


def attn_heads(q1, q2, qm, keys, lam, lam_init, subln_g):
    k1, k2, va, km, vm = keys
    bsz, nq = q1.shape[:2]
    s1 = jnp.einsum('bqhd,bkhd->bhqk', q1, k1).astype(jnp.float32) * DA_SCALE
    s2 = jnp.einsum('bqhd,bkhd->bhqk', q2, k2).astype(jnp.float32) * DA_SCALE
    p = jax.nn.softmax(s1, axis=-1) - lam * jax.nn.softmax(s2, axis=-1)
    oa = jnp.einsum('bhqk,bkhd->bqhd', p.astype(va.dtype), va)
    oa = rms_norm(oa, subln_g) * (1.0 - lam_init)
    sm = jnp.einsum('bqhd,bkhd->bhqk', qm, km).astype(jnp.float32) * MLA_SCALE
    om = jnp.einsum('bhqk,bkhd->bqhd', jax.nn.softmax(sm, axis=-1).astype(vm.dtype), vm)
    return jnp.concatenate([oa.reshape(bsz, nq, -1), om.reshape(bsz, nq, -1)], axis=-1)


def attn_mixer(hl, hc, rope, layer_idx, w_in, q_norm_g, q_up, kv_norm_g, kv_up,
               lam_q1, lam_k1, lam_q2, lam_k2, subln_g, w_out, need_ctx):
    lam_init = 0.8 - 0.6 * math.exp(-0.3 * layer_idx)
    f32 = jnp.float32
    lam = (jnp.exp(jnp.sum(lam_q1.astype(f32) * lam_k1.astype(f32)))
           - jnp.exp(jnp.sum(lam_q2.astype(f32) * lam_k2.astype(f32))) + lam_init)
    ql, kl = attn_project(hl, w_in, q_norm_g, q_up, kv_norm_g, kv_up, rope)
    qc, kc = attn_project(hc, w_in, q_norm_g, q_up, kv_norm_g, kv_up, None)
    k_all = tuple(jnp.concatenate([a, b], axis=1) for a, b in zip(kc, kl))
    out_l = over_query_blocks(
        lambda q1, q2, qm: attn_heads(q1, q2, qm, k_all, lam, lam_init, subln_g), *ql) @ w_out
    out_c = attn_heads(qc[0], qc[1], qc[2], kc, lam, lam_init, subln_g) @ w_out if need_ctx else None
    return out_l, out_c


def chunk_gla(q, k, v, log_a, s0, readout=True):
    bsz, t, nh, _ = k.shape
    dv = v.shape[-1]
    nc = t // CHUNK
    f32 = jnp.float32

    def chunks(a):
        return a.astype(f32).reshape(bsz, nc, CHUNK, nh, a.shape[-1])

    kc, vc = chunks(k), chunks(v)
    b = jnp.cumsum(chunks(log_a), axis=2)
    b_last = b[:, :, -1:]
    kv = jnp.einsum('bnlhk,bnlhv->nbhkv', kc * jnp.exp(b_last - b), vc)
    decay = jnp.moveaxis(jnp.exp(b_last[:, :, 0]), 1, 0)

    def step(s, inp):
        dec, upd = inp
        return dec[..., None] * s + upd, s

    s_final, s_start = lax.scan(step, s0, (decay, kv))
    if not readout:
        return None, s_final
    qc = chunks(q)
    o = jnp.einsum('bnlhk,nbhkv->bnlhv', qc * jnp.exp(b), s_start)
    b_ref = b[:, :, CHUNK // 2:CHUNK // 2 + 1]
    scores = jnp.einsum('bnihk,bnjhk->bnhij', qc * jnp.exp(b - b_ref), kc * jnp.exp(b_ref - b))
    scores = jnp.where(jnp.tril(jnp.ones((CHUNK, CHUNK), dtype=bool)), scores, 0.0)
    o = o + jnp.einsum('bnhij,bnjhv->bnihv', scores, vc)
    return o.reshape(bsz, t, nh, dv).astype(v.dtype), s_final


def bidir_scan(fc, fl, name, need_ctx):
    q_l, v_l = fl[name + '_q'], fl[name + '_v']
    bsz, _, nh, dk = q_l.shape
    s0 = jnp.zeros((bsz, nh, dk, v_l.shape[-1]), jnp.float32)
    outs_l, outs_c = [], []
    for d in range(2):
        rev = (lambda t: jnp.flip(t, axis=1)) if d == 1 else (lambda t: t)
        oc, s_ctx = chunk_gla(rev(fc[name + '_q']), rev(fc[name + '_k'][d]), rev(fc[name + '_v']),
                              rev(fc[name + '_la'][d]), s0, readout=need_ctx)
        ol, _ = chunk_gla(rev(q_l), rev(fl[name + '_k'][d]), rev(v_l), rev(fl[name + '_la'][d]), s_ctx)
        outs_l.append(rev(ol))
        if need_ctx:
            outs_c.append(rev(oc))
    o_l = outs_l[0] + outs_l[1]
    o_c = outs_c[0] + outs_c[1] if need_ctx else None
    return o_l, o_c


def rec_features(h, w_in, gk_up, gk_bias, lb):
    bsz, n, _ = h.shape

    def heads(t, nh):
        return t.reshape(bsz, n, nh, -1)

    gq, gk, gv, gg, gdown, hq, hf, hi, hg = _split(h @ w_in, REC_SPLITS)
    gla_la = [heads(jax.nn.log_sigmoid((gd @ gk_up[d] + gk_bias[d]).astype(jnp.float32)) / GLA_GATE_NORM,
                    GLA_HEADS) for d, gd in enumerate(jnp.split(gdown, 2, axis=-1))]
    gla_k = heads(gk, GLA_HEADS)
    hg_f = [lb[d] + (1.0 - lb[d]) * jax.nn.sigmoid(fd.astype(jnp.float32))
            for d, fd in enumerate(jnp.split(hf, 2, axis=-1))]
    return {
        'gla_q': heads(gq, GLA_HEADS) * GLA_DK ** -0.5,
        'gla_k': [gla_k, gla_k],
        'gla_v': heads(gv, GLA_HEADS),
        'gla_la': gla_la,
        'gla_gate': gg,
        'hg_q': heads(jax.nn.silu(hq), HG_HEADS),
        'hg_k': [heads(1.0 - f, HG_HEADS) for f in hg_f],
        'hg_v': heads(hi, HG_HEADS),
        'hg_la': [heads(jnp.log(f), HG_HEADS) for f in hg_f],
        'hg_gate': hg,
    }


def rec_mixer(hl, hc, w_in, gk_up, gk_bias, gla_norm_g, lb, hg_norm_g, w_out, need_ctx):
    fl = rec_features(hl, w_in, gk_up, gk_bias, lb)
    fc = rec_features(hc, w_in, gk_up, gk_bias, lb)
    gla_l, gla_c = bidir_scan(fc, fl, 'gla', need_ctx)
    hg_l, hg_c = bidir_scan(fc, fl, 'hg', need_ctx)

    def readout(o_gla, o_hg, f):
        bsz, n = o_gla.shape[:2]
        a = rms_norm(o_gla, gla_norm_g).reshape(bsz, n, -1) * jax.nn.silu(f['gla_gate'])
        b = rms_norm(o_hg, hg_norm_g).reshape(bsz, n, -1) * jax.nn.silu(f['hg_gate'])
        return jnp.concatenate([a, b], axis=-1) @ w_out

    out_l = readout(gla_l, hg_l, fl)
    out_c = readout(gla_c, hg_c, fc) if need_ctx else None
    return out_l, out_c


def expert_choice_ffn(h, w_router, w_gate, w_up, w_down):
    bsz, n, d = h.shape
    cap = EC_CAPACITY_FACTOR * n // N_EXPERTS
    aff = jax.nn.softmax((h @ w_router).astype(jnp.float32), axis=-1)
    gate, idx = lax.top_k(jnp.swapaxes(aff, 1, 2), cap)
    xe = jax.vmap(lambda hb, ib: hb[ib])(h, idx)
    hid = jax.nn.silu(jnp.einsum('becd,edf->becf', xe, w_gate)) * jnp.einsum('becd,edf->becf', xe, w_up)
    ye = jnp.einsum('becf,efd->becd', hid, w_down) * gate[..., None].astype(h.dtype)
    return jax.vmap(lambda yb, ib: jnp.zeros((n, d), yb.dtype).at[ib.reshape(-1)].add(yb.reshape(-1, d)))(ye, idx)


def setup_inputs(seed: int = 0) -> dict:
    key = jax.random.key(seed)
    ks = iter(jax.random.split(key, 64))

    def nrm(shape, scale):
        return jax.random.normal(next(ks), shape, jnp.float32) * scale

    def gain(shape):
        return 1.0 + nrm(shape, 0.02)

    D, NA, NR, E, F = D_MODEL, N_ATTN_LAYERS, N_REC_LAYERS, N_EXPERTS, EXPERT_FF
    return {
        'x': nrm((BATCH, SEQ, D), 1.0),
        'c': nrm((BATCH, D), 1.0),
        'ctx': nrm((BATCH, CTX_LEN, D), 1.0),
        'c_ctx': nrm((D,), 1.0),
        'ada_w': nrm((DEPTH, D, 6 * D), 0.5 * D ** -0.5),
        'ada_b': nrm((DEPTH, 6 * D), 0.02),
        'norm1_g': gain((DEPTH, D)),
        'norm2_g': gain((DEPTH, D)),
        'att_w_in': nrm((NA, D, ATT_IN), D ** -0.5),
        'mla_q_norm_g': gain((NA, MLA_Q_RANK)),
        'mla_q_up': nrm((NA, MLA_Q_RANK, MLA_HEADS * (MLA_NOPE + MLA_ROPE)), MLA_Q_RANK ** -0.5),
        'mla_kv_norm_g': gain((NA, MLA_KV_RANK)),
        'mla_kv_up': nrm((NA, MLA_KV_RANK, MLA_HEADS * (MLA_NOPE + MLA_V)), MLA_KV_RANK ** -0.5),
        'da_lam_q1': nrm((NA, DA_QK), 0.1),
        'da_lam_k1': nrm((NA, DA_QK), 0.1),
        'da_lam_q2': nrm((NA, DA_QK), 0.1),
        'da_lam_k2': nrm((NA, DA_QK), 0.1),
        'da_subln_g': gain((NA, DA_V)),
        'att_w_out': nrm((NA, ATT_OUT, D), ATT_OUT ** -0.5),
        'rec_w_in': nrm((NR, D, REC_IN), D ** -0.5),
        'gla_gk_up': nrm((NR, 2, GLA_GATE_RANK, GLA_HEADS * GLA_DK), GLA_GATE_RANK ** -0.5),
        'gla_gk_bias': nrm((NR, 2, GLA_HEADS * GLA_DK), 0.02),
        'gla_norm_g': gain((NR, GLA_DV)),
        'hg_lb_logits': nrm((DEPTH, 2, HG_HEADS * HG_DF), 0.1),
        'hg_norm_g': gain((NR, HG_DV)),
        'rec_w_out': nrm((NR, REC_OUT, D), REC_OUT ** -0.5),
        'moe_router': nrm((DEPTH, D, E), D ** -0.5),
        'moe_w_gate': nrm((DEPTH, E, D, F), D ** -0.5),
        'moe_w_up': nrm((DEPTH, E, D, F), D ** -0.5),
        'moe_w_down': nrm((DEPTH, E, F, D), F ** -0.5),
        'final_norm_g': gain((D,)),
    }


def reference(x, c, ctx, c_ctx, ada_w, ada_b, norm1_g, norm2_g, att_w_in, mla_q_norm_g, mla_q_up,
              mla_kv_norm_g, mla_kv_up, da_lam_q1, da_lam_k1, da_lam_q2, da_lam_k2, da_subln_g, att_w_out,
              rec_w_in, gla_gk_up, gla_gk_bias, gla_norm_g, hg_lb_logits, hg_norm_g, rec_w_out,
              moe_router, moe_w_gate, moe_w_up, moe_w_down, final_norm_g):
    rows = x.shape[1] // GRID_W
    cos_da, sin_da = axial_rope(rows, DA_QK)
    cos_r, sin_r = axial_rope(rows, MLA_ROPE)
    rope = (cos_da, sin_da, cos_r, sin_r)
    lb = jax.nn.softmax(hg_lb_logits.astype(jnp.float32), axis=0)
    lb = jnp.cumsum(lb, axis=0) - lb[0]
    sc_l = jax.nn.silu(c)
    sc_c = jax.nn.silu(c_ctx)
    xl, xc = x, ctx
    for i in range(DEPTH):
        last = i == DEPTH - 1
        j = i // 2
        mod_l = jnp.split((sc_l @ ada_w[i] + ada_b[i])[:, None, :], 6, axis=-1)
        mod_c = jnp.split(sc_c @ ada_w[i] + ada_b[i], 6, axis=-1)
        hl = modulate(xl, norm1_g[i], mod_l[0], mod_l[1])
        hc = modulate(xc, norm1_g[i], mod_c[0], mod_c[1])
        if i % 2 == 0:
            ml, mc = attn_mixer(hl, hc, rope, i, att_w_in[j], mla_q_norm_g[j], mla_q_up[j], mla_kv_norm_g[j],
                                mla_kv_up[j], da_lam_q1[j], da_lam_k1[j], da_lam_q2[j], da_lam_k2[j],
                                da_subln_g[j], att_w_out[j], not last)
        else:
            ml, mc = rec_mixer(hl, hc, rec_w_in[j], gla_gk_up[j], gla_gk_bias[j], gla_norm_g[j], lb[i],
                               hg_norm_g[j], rec_w_out[j], not last)
        xl = xl + mod_l[2] * ml
        hl = modulate(xl, norm2_g[i], mod_l[3], mod_l[4])
        xl = xl + mod_l[5] * expert_choice_ffn(hl, moe_router[i], moe_w_gate[i], moe_w_up[i], moe_w_down[i])
        if not last:
            xc = xc + mod_c[2] * mc
            hc = modulate(xc, norm2_g[i], mod_c[3], mod_c[4])
            xc = xc + mod_c[5] * expert_choice_ffn(hc, moe_router[i], moe_w_gate[i], moe_w_up[i], moe_w_down[i])
    return rms_norm(xl, final_norm_g)
```

```python
from contextlib import ExitStack
import numpy as np
import concourse.bass as bass
import concourse.mybir as mybir
from concourse.alu_op_type import AluOpType as ALU

F32 = mybir.dt.float32
BF16 = mybir.dt.bfloat16
I32 = mybir.dt.int32
U32 = mybir.dt.uint32
U16 = mybir.dt.uint16
AF = mybir.ActivationFunctionType
AX = mybir.AxisListType


class Buf:
    __slots__ = ("t", "w", "r", "name")

    def __init__(self, t, name=""):
        self.t = t
        self.w = None
        self.r = []
        self.name = name

    def __getitem__(self, idx):
        return self.t[idx]


class TileList(list):
    full = None


def tile_views(full, n=None):
    tl = TileList(Buf(full.t[:, :, t * 128:(t + 1) * 128]) for t in range(n or (full.t.shape[2] // 128)))
    tl.full = full
    return tl


class Tracker:
    COMPUTE = ("pe", "dve", "act", "pool")
    NDMA = 6

    def __init__(self, nc, es, same_engine_sync=True):
        self.nc = nc
        self.es = es
        self.same_engine_sync = same_engine_sync
        self.eng = {"pe": nc.tensor, "dve": nc.vector, "act": nc.scalar, "pool": nc.gpsimd, "sp": nc.sync}
        self.sems = {}
        self.cnt = {}
        for k in self.COMPUTE:
            self.sems[k] = es.enter_context(nc.semaphore("s_" + k))
            self.cnt[k] = 0
        self.ring = {}
        self.ring_pos = {}
        for q in ("sp", "act", "pool"):
            keys = []
            for i in range(self.NDMA):
                key = "d_%s%d" % (q, i)
                self.sems[key] = es.enter_context(nc.semaphore(key))
                self.cnt[key] = 0
                keys.append(key)
            self.ring[q] = keys
            self.ring_pos[q] = 0
        self.waited = {e: {} for e in self.eng}
        self.ninstr = 0

    def _wait(self, e, tok):
        if tok is None:
            return
        key, val = tok
        if key == e and (e == "pe" or not self.same_engine_sync):
            return
        if self.waited[e].get(key, 0) >= val:
            return
        self.eng[e].wait_ge(self.sems[key], val)
        self.waited[e][key] = val

    def _deps(self, e, reads, writes):
        for b in reads:
            self._wait(e, b.w)
        for b in writes:
            self._wait(e, b.w)
            for tok in b.r:
                self._wait(e, tok)

    def _commit(self, tok, reads, writes):
        for b in writes:
            b.w = tok
            b.r = []
        for b in reads:
            if b not in writes:
                b.r.append(tok)
                if len(b.r) > 64:
                    best = {}
                    for k, v in b.r:
                        if best.get(k, 0) < v:
                            best[k] = v
                    b.r = list(best.items())

    def op(self, e, fn, reads=(), writes=()):
        self._deps(e, reads, writes)
        ins = fn(self.eng[e])
        self.cnt[e] += 1
        ins.then_inc(self.sems[e], 1)
        tok = (e, self.cnt[e])
        self._commit(tok, reads, writes)
        self.ninstr += 1
        return tok

    def mm(self, out_buf, fn, reads, first, last):
        e = "pe"
        if first:
            self._deps(e, reads, [out_buf])
        else:
            self._deps(e, reads, [])
        ins = fn(self.eng[e])
        self.ninstr += 1
        if last:
            self.cnt[e] += 1
            ins.then_inc(self.sems[e], 1)
            tok = (e, self.cnt[e])
            self._commit(tok, reads, [out_buf])
        else:
            tok = (e, self.cnt[e] + 1)
            for b in reads:
                b.r.append(tok)
        return None

    def dma(self, q, out_ap, in_ap, reads=(), writes=(), **kw):
        e = q
        ring = self.ring[q]
        key = ring[self.ring_pos[q] % len(ring)]
        self.ring_pos[q] += 1
        if self.cnt[key] > 0:
            self._wait(e, (key, self.cnt[key]))
        self._deps(e, reads, writes)
        ins = self.eng[e].dma_start(out=out_ap, in_=in_ap, **kw)
        self.cnt[key] += 16
        ins.then_inc(self.sems[key], 16)
        tok = (key, self.cnt[key])
        self._commit(tok, reads, writes)
        self.ninstr += 1
        return tok

    def barrier(self):
        toks = [(k, v) for k, v in self.cnt.items() if v > 0]
        for e in self.eng:
            for tok in toks:
                self._wait(e, tok)

    def finish(self, bufs):
        for b in bufs:
            self._wait("sp", b.w)

from concourse.bass_utils import run_bass_kernel_spmd
import ml_dtypes

NPBF16 = ml_dtypes.bfloat16
NCORES = 8
D = 1024
SEQ = 2048
CTX = 256
NTOK = SEQ + CTX
NE = 16
FF = 2816
NFC = FF // 128
EPS = 1e-6


def new_nc():
    return bass.Bass("TRN2", target_bir_lowering=False)


class Ctx:
    def __init__(self, nc, es):
        self.nc = nc
        self.es = es
        self.T = Tracker(nc, es)
        self.n = 0

    def sb(self, shape, dt, name=None):
        self.n += 1
        name = name or ("t%d" % self.n)
        return Buf(self.es.enter_context(self.nc.sbuf_tensor(name, list(shape), dt)), name)

    def ps(self, shape, dt=F32, name=None):
        self.n += 1
        name = name or ("p%d" % self.n)
        return Buf(self.es.enter_context(self.nc.psum_tensor(name, list(shape), dt)), name)

    over = None

    def din(self, name, shape, dt):
        if self.over is not None:
            b = self.over[name]
            assert list(b.t.shape) == list(shape) and b.t.dtype == dt, (name, b.t.shape, shape)
            return b
        return Buf(self.nc.dram_tensor(name, list(shape), dt, kind="ExternalInput").ap(), name)

    def dout(self, name, shape, dt):
        if self.over is not None:
            b = self.over[name]
            assert list(b.t.shape) == list(shape) and b.t.dtype == dt, (name, b.t.shape, shape)
            return b
        return Buf(self.nc.dram_tensor(name, list(shape), dt, kind="ExternalOutput").ap(), name)

    def scratch(self, name, shape, dt):
        return Buf(self.nc.dram_tensor(name, list(shape), dt, kind="Internal").ap(), name)


def body_ada_cols(C):
    nc = C.nc
    T = C.T
    ccT_d = C.din("ccT", [D, 9], F32)
    w_d = C.din("adaw", [2, D, 768], F32)
    b_d = C.din("adab", [2, 768], F32)
    out_d = C.dout("mods", [2, 9, 768], F32)
    ccT = C.sb([128, 8, 9], F32)
    scT = C.sb([128, 8, 9], F32)
    w = [C.sb([128, 8, 768], F32) for _ in range(2)]
    bias = C.sb([9, 2, 768], F32)
    res = C.sb([9, 2, 768], F32)
    T.dma("sp", ccT[:], ccT_d.t.rearrange("(k p) s -> p k s", p=128), writes=[ccT])
    for l in range(2):
        T.dma(["sp", "act"][l], w[l][:], w_d.t[l].rearrange("(k p) f -> p k f", p=128), writes=[w[l]])
        T.dma("pool", bias[:, l, :], b_d.t[l].partition_broadcast(9), writes=[bias])
    T.op("act", lambda e: e.activation(out=scT[:], in_=ccT[:], func=AF.Silu), reads=[ccT], writes=[scT])
    pss = [C.ps([9, 384]) for _ in range(4)]
    for l in range(2):
        for h in range(2):
            p = pss[l * 2 + h]
            for k in range(8):
                T.mm(p, lambda e: e.matmul(p[:], lhsT=scT[:, k, :], rhs=w[l][:, k, h * 384:(h + 1) * 384],
                                           start=(k == 0), stop=(k == 7)), [scT, w[l]], k == 0, k == 7)
            T.op("dve", lambda e: e.tensor_tensor(out=res[:, l, h * 384:(h + 1) * 384], in0=p[:],
                                                  in1=bias[:, l, h * 384:(h + 1) * 384], op=ALU.add),
                 reads=[p, bias], writes=[res])
    T.dma("sp", out_d.t.rearrange("l s f -> s l f"), res[:], reads=[res], writes=[out_d])
    T.finish([out_d])


def build_ada():
    nc = new_nc()
    with ExitStack() as es:
        C = Ctx(nc, es)
        body_ada_cols(C)
    return nc

def body_ffn_ep(C, NT):
    nc = C.nc
    HT = NT // 2
    blocks = []
    s = 0
    while s < HT:
        n = min(512, HT - s)
        blocks.append((s, n))
        s += n
    T = C.T
    xT_d = C.din("xT", [2, D, NT], BF16)
    wg_d = C.din("wg", [2, D, FF], F32)
    wu_d = C.din("wu", [2, D, FF], F32)
    wd_d = C.din("wd", [2, FF, D], F32)
    y_d = C.dout("y", [2, NT, D], F32)
    xT = C.sb([128, 8, NT], BF16)
    hid_t = C.sb([128, NFC, HT], BF16)
    hid = [Buf(hid_t.t[:, fc, :]) for fc in range(NFC)]
    wdb_t = C.sb([128, NFC, D], BF16)
    wdb = [Buf(wdb_t.t[:, 2 * g:2 * g + 2, :]) for g in range(NFC // 2)]
    gstg = [C.sb([128, 8, 256], F32) for _ in range(2)]
    ustg = [C.sb([128, 8, 256], F32) for _ in range(2)]
    dstg = [C.sb([128, 2, D], F32) for _ in range(2)]
    wgb = [C.sb([128, 8, 256], BF16) for _ in range(2)]
    wub = [C.sb([128, 8, 256], BF16) for _ in range(2)]
    sg = [C.sb([128, 512], F32) for _ in range(2)]
    ysb = [C.sb([128, D], F32) for _ in range(2)]
    pg = [C.ps([128, 512]) for _ in range(2)]
    pu = [C.ps([128, 512]) for _ in range(2)]
    py = [C.ps([128, 512]) for _ in range(2)]
    it = 0
    yi = 0
    for ex in range(2):
        T.dma("pool", xT[:], xT_d.t[ex].rearrange("(k p) t -> p k t", p=128), reads=[xT_d], writes=[xT])
        for half in range(2):
            t0 = half * HT
            for g in range(NFC // 2):
                b = it % 2
                it += 1
                T.dma("sp", gstg[b][:], wg_d.t[ex].rearrange("(k p) f -> p k f", p=128)[:, :, g * 256:(g + 1) * 256],
                      writes=[gstg[b]])
                T.dma("act", ustg[b][:], wu_d.t[ex].rearrange("(k p) f -> p k f", p=128)[:, :, g * 256:(g + 1) * 256],
                      writes=[ustg[b]])
                T.op("act", lambda e: e.copy(out=wgb[b][:], in_=gstg[b][:]), reads=[gstg[b]], writes=[wgb[b]])
                T.op("pool", lambda e: e.tensor_copy(out=wub[b][:], in_=ustg[b][:]), reads=[ustg[b]], writes=[wub[b]])
                if half == 0:
                    T.dma("sp", dstg[b][:], wd_d.t[ex].rearrange("(c p) d -> p c d", p=128)[:, 2 * g:2 * g + 2, :],
                          writes=[dstg[b]])
                    T.op("dve", lambda e: e.tensor_copy(out=wdb[g][:], in_=dstg[b][:]), reads=[dstg[b]], writes=[wdb[g]])
                for j in range(2):
                    fc = 2 * g + j
                    for (bs, bn) in blocks:
                        pb = (fc * len(blocks) + (bs // 512)) % 2
                        pgb, pub, sgb = pg[pb], pu[pb], sg[pb]
                        for k in range(8):
                            T.mm(pgb, lambda e: e.matmul(pgb[:, :bn], lhsT=wgb[b][:, k, j * 128:(j + 1) * 128],
                                                         rhs=xT[:, k, t0 + bs:t0 + bs + bn], start=(k == 0), stop=(k == 7)),
                                 [wgb[b], xT], k == 0, k == 7)
                        for k in range(8):
                            T.mm(pub, lambda e: e.matmul(pub[:, :bn], lhsT=wub[b][:, k, j * 128:(j + 1) * 128],
                                                         rhs=xT[:, k, t0 + bs:t0 + bs + bn], start=(k == 0), stop=(k == 7)),
                                 [wub[b], xT], k == 0, k == 7)
                        T.op("act", lambda e: e.activation(out=sgb[:, :bn], in_=pgb[:, :bn], func=AF.Silu),
                             reads=[pgb], writes=[sgb])
                        T.op("dve", lambda e: e.tensor_tensor(out=hid[fc][:, bs:bs + bn], in0=sgb[:, :bn], in1=pub[:, :bn],
                                                              op=ALU.mult), reads=[sgb, pub], writes=[hid[fc]])
            for tt in range(HT // 128):
                yb = ysb[yi % 2]
                yi += 1
                for dh in range(2):
                    p = py[dh]
                    for fc in range(NFC):
                        T.mm(p, lambda e: e.matmul(p[:], lhsT=hid[fc][:, tt * 128:(tt + 1) * 128],
                                                   rhs=wdb_t[:, fc, dh * 512:(dh + 1) * 512], start=(fc == 0), stop=(fc == NFC - 1)),
                             [hid[fc], wdb[fc // 2]], fc == 0, fc == NFC - 1)
                    if dh == 0:
                        T.op("act", lambda e: e.copy(out=yb[:, 0:512], in_=p[:]), reads=[p], writes=[yb])
                    else:
                        T.op("dve", lambda e: e.tensor_copy(out=yb[:, 512:1024], in_=p[:]), reads=[p], writes=[yb])
                T.dma("pool", y_d.t[ex, t0 + tt * 128:t0 + (tt + 1) * 128, :], yb[:], reads=[yb], writes=[y_d])
    T.finish([y_d])


def build_ffn(NT):
    nc = new_nc()
    with ExitStack() as es:
        C = Ctx(nc, es)
        body_ffn_ep(C, NT)
    return nc

import math

NT_TILES = NTOK // 128
TOKBLOCKS = [(0, 256)] + [(256 + i * 512, 512) for i in range(4)]


def make_consts(C):
    T = C.T
    k = {}
    dji = C.sb([128, 128], F32)
    T.op("pool", lambda e: e.iota(dji[:], pattern=[[1, 128]], base=0, channel_multiplier=-1,
                                  allow_small_or_imprecise_dtypes=True), writes=[dji])
    k["dji"] = dji
    identf = C.sb([128, 128], F32)
    identb = C.sb([128, 128], BF16)
    utri = C.sb([128, 128], BF16)
    onesb = C.sb([128, 128], BF16)
    T.op("dve", lambda e: e.tensor_single_scalar(out=identf[:], in_=dji[:], scalar=0.0, op=ALU.is_equal), reads=[dji], writes=[identf])
    T.op("dve", lambda e: e.tensor_single_scalar(out=identb[:], in_=dji[:], scalar=0.0, op=ALU.is_equal), reads=[dji], writes=[identb])
    T.op("dve", lambda e: e.tensor_single_scalar(out=utri[:], in_=dji[:], scalar=0.0, op=ALU.is_gt), reads=[dji], writes=[utri])
    T.op("dve", lambda e: e.memset(onesb[:], 1.0), writes=[onesb])
    k.update(identf=identf, identb=identb, utri=utri, onesb=onesb)
    return k


def make_swap(C, k, R):
    T = C.T
    a = C.sb([128, 128], F32)
    b = C.sb([128, 128], F32)
    P = C.sb([128, 128], BF16)
    dji = k["dji"]
    T.op("dve", lambda e: e.tensor_single_scalar(out=a[:R, :R], in_=dji[:R, :R], scalar=float(R // 2), op=ALU.is_equal), reads=[dji], writes=[a])
    T.op("dve", lambda e: e.tensor_single_scalar(out=b[:R, :R], in_=dji[:R, :R], scalar=float(-(R // 2)), op=ALU.is_equal), reads=[dji], writes=[b])
    T.op("dve", lambda e: e.tensor_tensor(out=P[:R, :R], in0=a[:R, :R], in1=b[:R, :R], op=ALU.add), reads=[a, b], writes=[P])
    return P


def make_rope(C, R, n):
    T = C.T
    ln = int(math.log2(n))
    outs_pre = [C.sb([128, 2048], F32), C.sb([128, 2048], F32)]
    return _make_rope_inner(C, R, n, ln, outs_pre)


def _make_rope_inner(C, R, n, ln, outs_pre):
  T = C.T
  with scope(C):
    pi = C.sb([128, 1], I32)
    T.op("pool", lambda e: e.iota(pi[:R, :], pattern=[[0, 1]], base=0, channel_multiplier=1), writes=[pi])

    def ibit(shift, mask):
        t = C.sb([128, 1], I32)
        o = C.sb([128, 1], F32)
        T.op("dve", lambda e: e.tensor_scalar(out=t[:R, :], in0=pi[:R, :], scalar1=shift, scalar2=mask,
                                              op0=ALU.logical_shift_right, op1=ALU.bitwise_and), reads=[pi], writes=[t])
        T.op("dve", lambda e: e.tensor_copy(out=o[:R, :], in_=t[:R, :]), reads=[t], writes=[o])
        return o
    f = ibit(0, n - 1)
    axis = ibit(ln, 1)
    half = ibit(ln + 1, 1)
    inv = C.sb([128, 1], F32)
    T.op("act", lambda e: e.activation(out=inv[:R, :], in_=f[:R, :], func=AF.Exp, scale=-math.log(10000.0) / n), reads=[f], writes=[inv])
    A = C.sb([128, 1], F32)
    B = C.sb([128, 1], F32)
    sgn = C.sb([128, 1], F32)
    T.op("dve", lambda e: e.tensor_tensor(out=B[:R, :], in0=inv[:R, :], in1=axis[:R, :], op=ALU.mult), reads=[inv, axis], writes=[B])
    T.op("dve", lambda e: e.tensor_tensor(out=A[:R, :], in0=inv[:R, :], in1=B[:R, :], op=ALU.subtract), reads=[inv, B], writes=[A])
    T.op("dve", lambda e: e.tensor_scalar(out=sgn[:R, :], in0=half[:R, :], scalar1=2.0, scalar2=-1.0, op0=ALU.mult, op1=ALU.add), reads=[half], writes=[sgn])
    rowp = C.sb([128, 32, 64], F32)
    colp = C.sb([128, 32, 64], F32)
    T.op("pool", lambda e: e.iota(rowp[:R], pattern=[[1, 32], [0, 64]], base=0, channel_multiplier=0, allow_small_or_imprecise_dtypes=True), writes=[rowp])
    T.op("pool", lambda e: e.iota(colp[:R], pattern=[[0, 32], [1, 64]], base=0, channel_multiplier=0, allow_small_or_imprecise_dtypes=True), writes=[colp])
    ang = C.sb([128, 2048], F32)
    rp = rowp.t.rearrange("p a b -> p (a b)")
    cp = colp.t.rearrange("p a b -> p (a b)")
    T.op("dve", lambda e: e.tensor_scalar(out=cp[:R], in0=cp[:R], scalar1=B[:R, 0:1], scalar2=None, op0=ALU.mult), reads=[colp, B], writes=[colp])
    T.op("dve", lambda e: e.scalar_tensor_tensor(out=ang[:R], in0=rp[:R], scalar=A[:R, 0:1], in1=cp[:R], op0=ALU.mult, op1=ALU.add),
         reads=[rowp, colp, A], writes=[ang])
    outs = []
    ki = C.sb([128, 2048], I32)
    kf = C.sb([128, 2048], F32)
    m = C.sb([128, 2048], F32)
    for si, shift in enumerate((math.pi / 2, 0.0)):
        r1 = outs_pre[si]
        T.op("dve", lambda e: e.tensor_scalar(out=kf[:R], in0=ang[:R], scalar1=shift, scalar2=1.0 / (2 * math.pi), op0=ALU.add, op1=ALU.mult),
             reads=[ang], writes=[kf])
        T.op("dve", lambda e: e.tensor_copy(out=ki[:R], in_=kf[:R]), reads=[kf], writes=[ki])
        T.op("dve", lambda e: e.tensor_copy(out=kf[:R], in_=ki[:R]), reads=[ki], writes=[kf])
        T.op("dve", lambda e: e.scalar_tensor_tensor(out=r1[:R], in0=kf[:R], scalar=-2 * math.pi, in1=ang[:R], op0=ALU.mult, op1=ALU.add),
             reads=[kf, ang], writes=[r1])
        if shift != 0.0:
            T.op("dve", lambda e: e.tensor_scalar(out=r1[:R], in0=r1[:R], scalar1=shift, scalar2=None, op0=ALU.add), reads=[r1], writes=[r1])
        T.op("dve", lambda e: e.tensor_single_scalar(out=m[:R], in_=r1[:R], scalar=math.pi, op=ALU.is_gt), reads=[r1], writes=[m])
        T.op("dve", lambda e: e.scalar_tensor_tensor(out=r1[:R], in0=m[:R], scalar=-2 * math.pi, in1=r1[:R], op0=ALU.mult, op1=ALU.add),
             reads=[m, r1], writes=[r1])
        T.op("dve", lambda e: e.tensor_single_scalar(out=m[:R], in_=r1[:R], scalar=-math.pi, op=ALU.is_lt), reads=[r1], writes=[m])
        T.op("dve", lambda e: e.scalar_tensor_tensor(out=r1[:R], in0=m[:R], scalar=2 * math.pi, in1=r1[:R], op0=ALU.mult, op1=ALU.add),
             reads=[m, r1], writes=[r1])
        T.op("dve", lambda e: e.tensor_scalar(out=r1[:R], in0=r1[:R], scalar1=math.pi, scalar2=-math.pi, op0=ALU.min, op1=ALU.max), reads=[r1], writes=[r1])
        T.op("act", lambda e: e.activation(out=r1[:R], in_=r1[:R], func=AF.Sin), reads=[r1], writes=[r1])
        outs.append(r1)
    cos, sin = outs
    T.op("dve", lambda e: e.tensor_scalar(out=sin[:R], in0=sin[:R], scalar1=sgn[:R, 0:1], scalar2=None, op0=ALU.mult), reads=[sin, sgn], writes=[sin])
  return cos, sin


def rstd_from_ss(C, ss, n, P=128, W=1):
    T = C.T
    v = C.sb([128, W], F32)
    T.op("dve", lambda e: e.tensor_scalar(out=v[:P], in0=ss[:P, :W], scalar1=1.0 / n, scalar2=EPS, op0=ALU.mult, op1=ALU.add), reads=[ss], writes=[v])
    T.op("act", lambda e: e.activation(out=v[:P], in_=v[:P], func=AF.Sqrt), reads=[v], writes=[v])
    T.op("dve", lambda e: e.reciprocal(out=v[:P], in_=v[:P]), reads=[v], writes=[v])
    return v


def bcast_load(C, q, dram_ap_1d, n, name=None):
    t = C.sb([128, n], F32, name)
    C.T.dma(q, t[:], dram_ap_1d.partition_broadcast(128), writes=[t])
    return t

from contextlib import contextmanager


@contextmanager
def scope(C):
    old = C.es
    with ExitStack() as es2:
        C.es = es2
        try:
            yield
        finally:
            C.T.barrier()
            C.es = old


def evac(C, i, out_ap, in_ap, reads, writes):
    if i % 2 == 0:
        C.T.op("act", lambda e: e.copy(out=out_ap, in_=in_ap), reads=reads, writes=writes)
    else:
        C.T.op("dve", lambda e: e.tensor_copy(out=out_ap, in_=in_ap), reads=reads, writes=writes)


def norm_mod_transpose(C, k, x_tiles, g_d, shift_l, scale_l, shift_c, scale_c, hT, ps_bf, h_tok=None, tile_cb=None, tiles=None):
    T = C.T
    with scope(C):
        G = bcast_load(C, "sp", g_d, D)
        A = {}
        B = {}
        for nm, sh, sc, q in (("l", shift_l, scale_l, "act"), ("c", shift_c, scale_c, "pool")):
            S = bcast_load(C, q, sc, D)
            A[nm] = C.sb([128, D], F32)
            T.op("dve", lambda e: e.scalar_tensor_tensor(out=A[nm][:], in0=S[:], scalar=1.0, in1=G[:], op0=ALU.add, op1=ALU.mult),
                 reads=[S, G], writes=[A[nm]])
            B[nm] = bcast_load(C, q, sh, D)
        NBF = 3
        xt = [C.sb([128, D], F32) for _ in range(NBF)]
        junk = C.sb([128, D], BF16)
        hb = [C.sb([128, D], BF16) for _ in range(NBF)]
        hf = [C.sb([128, D], F32) for _ in range(NBF)]
        ss = [C.sb([128, 1], F32) for _ in range(NBF)]

        def stage1(i, t):
            nm = "c" if t < 2 else "l"
            x = xt[i % NBF]
            T.dma(["sp", "act"][i % 2], x[:], x_tiles[t].t, reads=[x_tiles[t]], writes=[x])
            s = ss[i % NBF]
            T.op("act", lambda e: e.activation(out=junk[:], in_=x[:], func=AF.Square, accum_out=s[:, 0:1]), reads=[x], writes=[junk, s])
            r = rstd_from_ss(C, s, D)
            h32 = hf[i % NBF]
            T.op("dve", lambda e: e.scalar_tensor_tensor(out=h32[:], in0=x[:], scalar=r[:, 0:1], in1=A[nm][:], op0=ALU.mult, op1=ALU.mult),
                 reads=[x, r, A[nm]], writes=[h32])
            hcur = h_tok[t] if h_tok is not None else hb[i % NBF]
            T.op("dve", lambda e: e.tensor_tensor(out=hcur[:], in0=h32[:], in1=B[nm][:], op=ALU.add), reads=[h32, B[nm]], writes=[hcur])
            if tile_cb is not None:
                T.op("dve", lambda e: e.tensor_tensor(out=h32[:], in0=h32[:], in1=B[nm][:], op=ALU.add), reads=[h32, B[nm]], writes=[h32])
            return (i, t, h32, hcur)

        def stage2(st):
            i, t, h32, hcur = st
            if tile_cb is not None:
                tile_cb(t, h32)
            if hT is not None:
                p = ps_bf[i % 2]
                pv = p.t[:].bitcast(BF16)
                for c in range(8):
                    T.op("pe", lambda e: e.transpose(pv[:, c * 128:(c + 1) * 128], hcur[:, c * 128:(c + 1) * 128], k["identb"][:]),
                         reads=[hcur, k["identb"]], writes=[p])
                evac(C, i, hT[t].t, pv.rearrange("p (c n) -> p c n", c=8), [p], [hT[t]])

        prev = None
        for i, t in enumerate(tiles if tiles is not None else range(NT_TILES)):
            cur = stage1(i, t)
            if prev is not None:
                stage2(prev)
            prev = cur
        stage2(prev)


def proj_fm(C, k, dst, src, nk, w, col0, R, ps, rope=None, scale=None, tmp=None):
    T = C.T

    def mm_block(bi):
        t0, n = TOKBLOCKS[bi]
        p = ps[bi % 2]
        ntile = n // 128
        tiles_ = list(src[t0 // 128:t0 // 128 + ntile])
        for kk in range(nk):
            T.mm(p, lambda e: e.matmul(p[:R, :n], lhsT=w[:, kk, col0:col0 + R], rhs=src.full[:, kk, t0:t0 + n],
                                       start=(kk == 0), stop=(kk == nk - 1)), [w] + tiles_, kk == 0, kk == nk - 1)

    def post_block(bi):
        t0, n = TOKBLOCKS[bi]
        p = ps[bi % 2]
        if rope is not None and bi > 0:
            cos, sin, Pm = rope
            raw, t1, p2 = tmp[0][bi % 2], tmp[1][bi % 2], tmp[2][bi % 2]
            pos0 = t0 - 256
            T.op("act", lambda e: e.copy(out=raw[:R, :n], in_=p[:R, :n]), reads=[p], writes=[raw])
            T.mm(p2, lambda e: e.matmul(p2[:R, :n], lhsT=Pm[:R, :R], rhs=raw[:R, :n], start=True, stop=True), [Pm, raw], True, True)
            T.op("dve", lambda e: e.tensor_tensor(out=t1[:R, :n], in0=raw[:R, :n], in1=cos[:R, pos0:pos0 + n], op=ALU.mult), reads=[raw, cos], writes=[t1])
            T.op("dve", lambda e: e.tensor_tensor(out=raw[:R, :n], in0=p2[:R, :n], in1=sin[:R, pos0:pos0 + n], op=ALU.mult), reads=[p2, sin, raw], writes=[raw])
            T.op("dve", lambda e: e.tensor_tensor(out=dst[:R, t0:t0 + n], in0=t1[:R, :n], in1=raw[:R, :n], op=ALU.add), reads=[t1, raw], writes=[dst])
        else:
            evac(C, bi, dst[:R, t0:t0 + n], p[:R, :n], [p], [dst])

    nb = len(TOKBLOCKS)
    mm_block(0)
    for bi in range(nb):
        if bi + 1 < nb:
            mm_block(bi + 1)
        post_block(bi)


def proj_tm(C, vaug, src, nk, w, col0, W, ps):
    T = C.T
    for t in range(NT_TILES):
        p = ps[t % 2]
        for kk in range(nk):
            T.mm(p, lambda e: e.matmul(p[:, :W], lhsT=src[t][:, kk, :], rhs=w[:, kk, col0:col0 + W], start=(kk == 0), stop=(kk == nk - 1)),
                 [src[t], w], kk == 0, kk == nk - 1)
        evac(C, t, vaug[:, t, 0:W], p[:, :W], [p], [vaug])


def attend(C, pairs, vaug, W, scale, on, psS, psO, pts):
    T = C.T
    rec = C.sb([128, 1], F32)
    cnt = 0
    for (q0, nq, ktiles) in [(0, 2, [0, 1])] + [(256 + i * 512, 4, list(range(18))) for i in range(4)]:
        n = nq * 128

        def issueS(ii):
            kt = ktiles[ii]
            p = psS[ii % 2]
            for pi_, (kT, qT, R) in enumerate(pairs):
                T.mm(p, lambda e: e.matmul(p[:, :n], lhsT=kT[:R, kt * 128:(kt + 1) * 128], rhs=qT[:R, q0:q0 + n],
                                           start=(pi_ == 0), stop=(pi_ == len(pairs) - 1)), [kT, qT], pi_ == 0, pi_ == len(pairs) - 1)
        issueS(0)
        for ii, kt in enumerate(ktiles):
            p = psS[ii % 2]
            pt = pts[cnt % len(pts)]
            cnt += 1
            T.op("act", lambda e: e.activation(out=pt[:, :n], in_=p[:, :n], func=AF.Exp, scale=scale), reads=[p], writes=[pt])
            if ii + 1 < len(ktiles):
                issueS(ii + 1)
            for qs in range(nq):
                po = psO[qs]
                T.mm(po, lambda e: e.matmul(po[:, :W + 1], lhsT=pt[:, qs * 128:(qs + 1) * 128], rhs=vaug[:, kt, :],
                                            start=(ii == 0), stop=(ii == len(ktiles) - 1)), [pt, vaug], ii == 0, ii == len(ktiles) - 1)
        for qs in range(nq):
            po = psO[qs]
            qt = q0 // 128 + qs
            T.op("dve", lambda e: e.reciprocal(out=rec[:], in_=po[:, W:W + 1]), reads=[po], writes=[rec])
            T.op("dve", lambda e: e.tensor_scalar(out=on[:, qt, :], in0=po[:, 0:W], scalar1=rec[:, 0:1], scalar2=None, op0=ALU.mult),
                 reads=[po, rec], writes=[on])


def route_and_gather(C, k, h_tok, logits, xeT_d, meta_d, ps, with_ctx):
    T = C.T
    NS = 288 if with_ctx else 256
    t_first = 0 if with_ctx else 2
    tiles = list(range(t_first, NT_TILES))
    identf = k["identf"]
    with scope(C):
        mx = C.sb([128, 18], F32)
        ez = C.sb([128, 18, 16], F32)
        aff = C.sb([128, 18, 16], F32)
        sm = C.sb([128, 18], F32)
        T.op("dve", lambda e: e.tensor_reduce(out=mx[:], in_=logits[:], axis=AX.X, op=ALU.max), reads=[logits], writes=[mx])
        for t in range(NT_TILES):
            T.op("dve", lambda e: e.tensor_scalar(out=ez[:, t, :], in0=logits[:, t, :], scalar1=mx[:, t:t + 1], scalar2=None, op0=ALU.subtract),
                 reads=[logits, mx], writes=[ez])
        T.op("act", lambda e: e.activation(out=ez[:], in_=ez[:], func=AF.Exp), reads=[ez], writes=[ez])
        T.op("dve", lambda e: e.tensor_reduce(out=sm[:], in_=ez[:], axis=AX.X, op=ALU.add), reads=[ez], writes=[sm])
        T.op("dve", lambda e: e.reciprocal(out=sm[:], in_=sm[:]), reads=[sm], writes=[sm])
        for t in range(NT_TILES):
            T.op("dve", lambda e: e.tensor_scalar(out=aff[:, t, :], in0=ez[:, t, :], scalar1=sm[:, t:t + 1], scalar2=None, op0=ALU.mult),
                 reads=[ez, sm], writes=[aff])
        affT = C.sb([16, NTOK], F32)
        for t in range(NT_TILES):
            p = ps[t % 2]
            T.op("pe", lambda e: e.transpose(p[:16, :128], aff[:, t, :], identf[:]), reads=[aff, identf], writes=[p])
            evac(C, t, affT[:, t * 128:(t + 1) * 128], p[:16, :128], [p], [affT])
        maskT = C.sb([16, NTOK], F32)
        m8 = C.sb([16, 8], F32)

        def thresh(lo, n, cap):
            wk = C.sb([16, n], F32)
            T.op("dve", lambda e: e.tensor_copy(out=wk[:], in_=affT[:, lo:lo + n]), reads=[affT], writes=[wk])
            for r in range(cap // 8):
                T.op("dve", lambda e: e.max(out=m8[:], in_=wk[:]), reads=[wk], writes=[m8])
                if r < cap // 8 - 1:
                    T.op("dve", lambda e: e.match_replace(out=wk[:], in_to_replace=m8[:], in_values=wk[:], imm_value=-1.0), reads=[wk, m8], writes=[wk])
            T.op("dve", lambda e: e.tensor_scalar(out=maskT[:, lo:lo + n], in0=affT[:, lo:lo + n], scalar1=m8[:, 7:8], scalar2=None, op0=ALU.is_ge),
                 reads=[affT, m8], writes=[maskT])
        thresh(256, SEQ, 2 * SEQ // NE)
        if with_ctx:
            thresh(0, CTX, 2 * CTX // NE)
        else:
            T.op("dve", lambda e: e.memset(maskT[:, 0:256], 0.0), writes=[maskT])
        mask = C.sb([128, 18, 16], F32)
        maskb = C.sb([128, 18, 16], BF16)
        for t in range(NT_TILES):
            p = ps[t % 2]
            T.op("pe", lambda e: e.transpose(p[:, :16], maskT[:, t * 128:(t + 1) * 128], identf[:16, :16]), reads=[maskT, identf], writes=[p])
            evac(C, t, mask[:, t, :], p[:, :16], [p], [mask])
        T.op("dve", lambda e: e.tensor_copy(out=maskb[:], in_=mask[:]), reads=[mask], writes=[maskb])
        pre_p, tot_p = ps[0], ps[1]
        mb2 = maskb.t.rearrange("p t e -> p (t e)")
        T.mm(pre_p, lambda e: e.matmul(pre_p[:, :288], lhsT=k["utri"][:], rhs=mb2, start=True, stop=True), [k["utri"], maskb], True, True)
        T.mm(tot_p, lambda e: e.matmul(tot_p[:, :288], lhsT=k["onesb"][:], rhs=mb2, start=True, stop=True), [k["onesb"], maskb], True, True)
        tot = C.sb([128, 18, 16], F32)
        off = C.sb([128, 18, 16], F32)
        slot = C.sb([128, 18, 16], F32)
        T.op("act", lambda e: e.copy(out=tot.t.rearrange("p t e -> p (t e)"), in_=tot_p[:, :288]), reads=[tot_p], writes=[tot])
        T.op("dve", lambda e: e.memset(off[:], 0.0), writes=[off])
        T.op("dve", lambda e: e.memset(off[:, 0:2, :], 256.0), reads=[off], writes=[off])
        T.op("dve", lambda e: e.tensor_tensor(out=off[:, 1, :], in0=off[:, 0, :], in1=tot[:, 0, :], op=ALU.add), reads=[off, tot], writes=[off])
        for t in range(3, NT_TILES):
            T.op("dve", lambda e: e.tensor_tensor(out=off[:, t, :], in0=off[:, t - 1, :], in1=tot[:, t - 1, :], op=ALU.add), reads=[off, tot], writes=[off])
        T.op("dve", lambda e: e.tensor_tensor(out=slot.t.rearrange("p t e -> p (t e)"), in0=pre_p[:, :288],
                                              in1=off.t.rearrange("p t e -> p (t e)"), op=ALU.add), reads=[pre_p, off], writes=[slot])
        T.op("dve", lambda e: e.scalar_tensor_tensor(out=slot.t.rearrange("p t e -> p (t e)"), in0=slot.t.rearrange("p t e -> p (t e)"), scalar=1.0,
                                                     in1=mask.t.rearrange("p t e -> p (t e)"), op0=ALU.add, op1=ALU.mult), reads=[slot, mask], writes=[slot])
        T.op("dve", lambda e: e.tensor_scalar(out=slot[:], in0=slot[:], scalar1=-1.0, scalar2=None, op0=ALU.add), reads=[slot], writes=[slot])
        vals = C.sb([128, 18, 16, 4], BF16)
        tcol = C.sb([128, 18, 16], F32)
        pcol = C.sb([128, 18, 16], F32)
        T.op("pool", lambda e: e.iota(tcol[:], pattern=[[1, 18], [0, 16]], base=0, channel_multiplier=0, allow_small_or_imprecise_dtypes=True), writes=[tcol])
        T.op("pool", lambda e: e.iota(pcol[:], pattern=[[0, 18], [0, 16]], base=0, channel_multiplier=1, allow_small_or_imprecise_dtypes=True), writes=[pcol])
        ahi = C.sb([128, 18, 16], BF16)
        alo = C.sb([128, 18, 16], F32)
        T.op("dve", lambda e: e.tensor_copy(out=ahi[:], in_=aff[:]), reads=[aff], writes=[ahi])
        T.op("dve", lambda e: e.tensor_tensor(out=alo[:], in0=aff[:], in1=ahi[:], op=ALU.subtract), reads=[aff, ahi], writes=[alo])
        for ci, src in enumerate((tcol, pcol, ahi, alo)):
            T.op("dve", lambda e: e.tensor_copy(out=vals[:, :, :, ci], in_=src[:]), reads=[src], writes=[vals])
        iota_s = C.sb([128, 288], U16)
        T.op("pool", lambda e: e.iota(iota_s[:], pattern=[[1, 288]], base=0, channel_multiplier=0, allow_small_or_imprecise_dtypes=True), writes=[iota_s])
        ohl = [C.sb([128, 16, 256], BF16) for _ in range(2)]
        ohc = [C.sb([128, 2, 32], BF16) for _ in range(2)]
        xes = [C.sb([128, 8, NS], BF16) for _ in range(2)]
        meta_sb = C.sb([4, 16, NS], F32)
        pm = ps[2]
        pgs = [ps[3], ps[4]]
        ei = 0

        def build_oh(ex):
            ol, oc = ohl[ex % 2], ohc[ex % 2]
            for t in tiles:
                if t >= 2:
                    T.op("dve", lambda e: e.tensor_scalar(out=ol[:, t - 2, :], in0=iota_s[:, 0:256], scalar1=slot[:, t, ex:ex + 1], scalar2=None, op0=ALU.is_equal),
                         reads=[iota_s, slot], writes=[ol])
                else:
                    T.op("dve", lambda e: e.tensor_scalar(out=oc[:, t, :], in0=iota_s[:, 256:288], scalar1=slot[:, t, ex:ex + 1], scalar2=None, op0=ALU.is_equal),
                         reads=[iota_s, slot], writes=[oc])
        build_oh(0)
        for ex in range(NE):
            ol, oc, xe = ohl[ex % 2], ohc[ex % 2], xes[ex % 2]
            if ex + 1 < NE:
                build_oh(ex + 1)
            for t in range(2, NT_TILES):
                T.mm(pm, lambda e: e.matmul(pm[:4, 0:256], lhsT=vals[:, t, ex, :], rhs=ol[:, t - 2, :], start=(t == 2), stop=(t == NT_TILES - 1)),
                     [vals, ol], t == 2, t == NT_TILES - 1)
            if with_ctx:
                for t in range(2):
                    T.mm(pm, lambda e: e.matmul(pm[:4, 256:288], lhsT=vals[:, t, ex, :], rhs=oc[:, t, :], start=(t == 0), stop=(t == 1)),
                         [vals, oc], t == 0, t == 1)
            T.op("act", lambda e: e.copy(out=meta_sb[:, ex, :], in_=pm[:4, :NS]), reads=[pm], writes=[meta_sb])
            for c in range(8):
                pg = pgs[ei % 2]
                ei += 1
                for t in range(2, NT_TILES):
                    T.mm(pg, lambda e: e.matmul(pg[:, 0:256], lhsT=h_tok[t][:, c * 128:(c + 1) * 128], rhs=ol[:, t - 2, :], start=(t == 2), stop=(t == NT_TILES - 1)),
                         [h_tok[t], ol], t == 2, t == NT_TILES - 1)
                if with_ctx:
                    for t in range(2):
                        T.mm(pg, lambda e: e.matmul(pg[:, 256:288], lhsT=h_tok[t][:, c * 128:(c + 1) * 128], rhs=oc[:, t, :], start=(t == 0), stop=(t == 1)),
                             [h_tok[t], oc], t == 0, t == 1)
                evac(C, c, xe[:, c, :], pg[:, :NS], [pg], [xe])
            T.dma(["sp", "act"][ex % 2], xeT_d.t[ex].rearrange("(c p) s -> p c s", p=128), xe[:], reads=[xe], writes=[xeT_d])
        T.dma("sp", meta_d.t, meta_sb[:], reads=[meta_sb], writes=[meta_d])


DA_SCALE = 64 ** -0.5
MLA_SCALE = 96 ** -0.5


def load_cast(C, q, dram_ap, shape, stage=None):
    out = C.sb(shape, BF16)
    C.T.dma("pool", out[:], dram_ap, writes=[out])
    return out


def router_cb_factory(C, k, router_sb, logits, psT, psL):
    T = C.T
    hfT = [C.sb([128, 8, 128], F32) for _ in range(2)]

    def cb(t, h32):
        hT_ = hfT[t % 2]
        for half in range(2):
            p = psT[half]
            for c in range(4):
                cc = half * 4 + c
                T.op("pe", lambda e: e.transpose(p[:, c * 128:(c + 1) * 128], h32[:, cc * 128:(cc + 1) * 128], k["identf"][:]),
                     reads=[h32, k["identf"]], writes=[p])
            evac(C, half, hT_[:, half * 4:(half + 1) * 4, :], p.t[:].rearrange("p (c n) -> p c n", c=4), [p], [hT_])
        for c in range(8):
            T.mm(psL, lambda e: e.matmul(psL[:, :16], lhsT=hT_[:, c, :], rhs=router_sb[:, c, :], start=(c == 0), stop=(c == 7)),
                 [hT_, router_sb], c == 0, c == 7)
        T.op("act", lambda e: e.copy(out=logits[:, t, :], in_=psL[:, :16]), reads=[psL], writes=[logits])
    return cb


def body_attn(C):
    nc = C.nc
    LAM_INIT = 0.8 - 0.6 * math.exp(-0.3 * 0)
    T = C.T
    x_d = C.din("xin", [NTOK, D], F32)
    mod_d = C.din("mod", [2, 6 * D], F32)
    n1g_d = C.din("n1g", [D], F32)
    n2g_d = C.din("n2g", [D], F32)
    win_d = C.din("w_in", [D, 2208], F32)
    qng_d = C.din("qng", [384], F32)
    qup_d = C.din("q_up", [384, 768], F32)
    kvng_d = C.din("kvng", [256], F32)
    kvup_d = C.din("kv_up", [256, 1024], F32)
    lam_d = C.din("lamv", [4, 64], F32)
    subg_d = C.din("subg", [128], F32)
    wout_d = C.din("w_out", [D, D], F32)
    rt_d = C.din("router", [D, NE], F32)
    xmid_d = C.dout("xmid", [NTOK, D], F32)
    xeT_d = C.dout("xeT", [NE, D, 288], BF16)
    meta_d = C.dout("meta", [4, NE, 288], F32)
    attn_d = Buf(nc.dram_tensor("attn_scr", [NTOK, D], BF16, kind="Internal").ap(), "attn_scr")
    x_tiles = [Buf(x_d.t[t * 128:(t + 1) * 128, :]) for t in range(NT_TILES)]
    xmid_tiles = [Buf(xmid_d.t[t * 128:(t + 1) * 128, :]) for t in range(NT_TILES)]
    attn_cols = {}
    ps = [C.ps([128, 512]) for _ in range(8)]
    k = make_consts(C)
    win_v = win_d.t.rearrange("(k p) f -> p k f", p=128)

    def modv(row, i):
        return mod_d.t[row, i * D:(i + 1) * D]

    with scope(C):
        hT_t = C.sb([128, 8, NTOK], BF16)
        hT = tile_views(hT_t)
        norm_mod_transpose(C, k, x_tiles, n1g_d.t, modv(0, 0), modv(0, 1), modv(1, 0), modv(1, 1), hT, ps[0:2])
        with scope(C):
            cosD, sinD = make_rope(C, 64, 16)
            P64 = make_swap(C, k, 64)
            lamb = C.sb([128, 4, 64], F32)
            T.dma("sp", lamb.t.rearrange("p a b -> p (a b)"), lam_d.t.rearrange("a b -> (a b)").partition_broadcast(128), writes=[lamb])
            lp = C.sb([128, 2, 64], F32)
            ls = C.sb([128, 2], F32)
            neglam = C.sb([128, 1], F32)
            T.op("dve", lambda e: e.tensor_tensor(out=lp[:, 0, :], in0=lamb[:, 0, :], in1=lamb[:, 1, :], op=ALU.mult), reads=[lamb], writes=[lp])
            T.op("dve", lambda e: e.tensor_tensor(out=lp[:, 1, :], in0=lamb[:, 2, :], in1=lamb[:, 3, :], op=ALU.mult), reads=[lamb, lp], writes=[lp])
            T.op("dve", lambda e: e.tensor_reduce(out=ls[:], in_=lp[:], axis=AX.X, op=ALU.add), reads=[lp], writes=[ls])
            T.op("act", lambda e: e.activation(out=ls[:], in_=ls[:], func=AF.Exp), reads=[ls], writes=[ls])
            T.op("dve", lambda e: e.tensor_scalar(out=neglam[:], in0=ls[:, 1:2], scalar1=-LAM_INIT, scalar2=None, op0=ALU.add), reads=[ls], writes=[neglam])
            T.op("dve", lambda e: e.tensor_tensor(out=neglam[:], in0=neglam[:], in1=ls[:, 0:1], op=ALU.subtract), reads=[neglam, ls], writes=[neglam])
            Gs = bcast_load(C, "act", subg_d.t, 128)
            T.op("dve", lambda e: e.tensor_scalar(out=Gs[:], in0=Gs[:], scalar1=1.0 - LAM_INIT, scalar2=None, op0=ALU.mult), reads=[Gs], writes=[Gs])
            wst = C.sb([128, 8, 384], F32)
            wda = C.sb([128, 8, 384], BF16)
            qk = [C.sb([128, NTOK], BF16) for _ in range(4)]
            for u in range(4):
                T.op("dve", lambda e: e.memset(qk[u][64:128, :], 0.0), writes=[qk[u]])
            vaug = C.sb([128, 18, 129], BF16)
            T.op("dve", lambda e: e.memset(vaug[:, :, 128:129], 1.0), writes=[vaug])
            raw = [C.sb([64, 512], BF16) for _ in range(2)]
            t1 = [C.sb([64, 512], F32) for _ in range(2)]
            o1n = C.sb([128, 18, 128], F32)
            o2n = C.sb([128, 18, 128], F32)
            sq = C.sb([128, 18, 128], F32)
            ssq = C.sb([128, 18], F32)
            ob = C.sb([128, 18, 128], BF16)
            pts = [C.sb([128, 512], BF16) for _ in range(3)]
            for h in range(4):
                for j, c0 in enumerate((h * 128, 512 + h * 128, 1024 + h * 128)):
                    T.dma(["sp", "act", "pool"][j], wst[:, :, j * 128:(j + 1) * 128], win_v[:, :, c0:c0 + 128], writes=[wst])
                T.op("dve", lambda e: e.tensor_copy(out=wda[:], in_=wst[:]), reads=[wst], writes=[wda])
                for u in range(4):
                    proj_fm(C, k, qk[u], hT, 8, wda, (u // 2) * 128 + (u % 2) * 64, 64, ps[0:2], rope=(cosD, sinD, P64), tmp=(raw, t1, ps[2:4]))
                proj_tm(C, vaug, hT, 8, wda, 256, 128, ps[0:2])
                attend(C, [(qk[2], qk[0], 128)], vaug, 128, DA_SCALE, o1n, ps[0:2], ps[4:8], pts)
                attend(C, [(qk[3], qk[1], 128)], vaug, 128, DA_SCALE, o2n, ps[0:2], ps[4:8], pts)
                o1f = o1n.t.rearrange("p a b -> p (a b)")
                o2f = o2n.t.rearrange("p a b -> p (a b)")
                T.op("dve", lambda e: e.scalar_tensor_tensor(out=o1f, in0=o2f, scalar=neglam[:, 0:1], in1=o1f, op0=ALU.mult, op1=ALU.add),
                     reads=[o1n, o2n, neglam], writes=[o1n])
                T.op("act", lambda e: e.activation(out=sq[:], in_=o1n[:], func=AF.Square), reads=[o1n], writes=[sq])
                T.op("dve", lambda e: e.tensor_reduce(out=ssq[:], in_=sq[:], axis=AX.X, op=ALU.add), reads=[sq], writes=[ssq])
                r = rstd_from_ss(C, ssq, 128, W=18)
                for t in range(NT_TILES):
                    T.op("dve", lambda e: e.scalar_tensor_tensor(out=ob[:, t, :], in0=o1n[:, t, :], scalar=r[:, t:t + 1], in1=Gs[:], op0=ALU.mult, op1=ALU.mult),
                         reads=[o1n, r, Gs], writes=[ob])
                T.dma("sp", attn_d.t.rearrange("(t p) d -> p t d", p=128)[:, :, h * 128:(h + 1) * 128], ob[:], reads=[ob], writes=[attn_d])
        with scope(C):
            cosR, sinR = make_rope(C, 32, 8)
            P32 = make_swap(C, k, 32)
            wlat = load_cast(C, "sp", win_v[:, :, 1536:2208], [128, 8, 672])
            qupb = load_cast(C, "act", qup_d.t.rearrange("(c p) f -> p c f", p=128), [128, 3, 768])
            kvupb = load_cast(C, "sp", kvup_d.t.rearrange("(c p) f -> p c f", p=128), [128, 2, 1024])
            gq = C.sb([128, 3], F32)
            gkv = C.sb([128, 2], F32)
            T.dma("pool", gq[:], qng_d.t.rearrange("(c p) -> p c", p=128), writes=[gq], allow_slow_non_contiguous=True)
            T.dma("pool", gkv[:], kvng_d.t.rearrange("(c p) -> p c", p=128), writes=[gkv], allow_slow_non_contiguous=True)
            cqn_t = C.sb([128, 3, NTOK], BF16)
            ckvn_t = C.sb([128, 2, NTOK], BF16)
            cqn = tile_views(cqn_t)
            ckvn = tile_views(ckvn_t)
            kr = C.sb([128, NTOK], BF16)
            T.op("dve", lambda e: e.memset(kr[32:64, :], 0.0), writes=[kr])
            T.op("dve", lambda e: e.memset(kr[64:128, :], 0.0), writes=[kr])
            raw = [C.sb([64, 512], BF16) for _ in range(2)]
            t1 = [C.sb([64, 512], F32) for _ in range(2)]
            with scope(C):
                rawf = [C.sb([128, 512], F32) for _ in range(3)]
                sqb = [C.sb([128, 512], BF16) for _ in range(3)]
                rs = C.sb([128, 512], F32)
                for (nchunk, col0, g, dst_t, dst, nfeat) in ((3, 0, gq, cqn_t, cqn, 384), (2, 384, gkv, ckvn_t, ckvn, 256)):
                    for bi, (t0, n) in enumerate(TOKBLOCKS):
                        ntile = n // 128
                        for c in range(nchunk):
                            p = ps[c]
                            tiles_ = list(hT[t0 // 128:t0 // 128 + ntile])
                            for kk in range(8):
                                T.mm(p, lambda e: e.matmul(p[:, :n], lhsT=wlat[:, kk, col0 + c * 128:col0 + (c + 1) * 128], rhs=hT.full[:, kk, t0:t0 + n],
                                                           start=(kk == 0), stop=(kk == 7)), [wlat] + tiles_, kk == 0, kk == 7)
                            T.op("act", lambda e: e.copy(out=rawf[c][:, :n], in_=p[:, :n]), reads=[p], writes=[rawf[c]])
                            T.op("dve", lambda e: e.tensor_tensor(out=sqb[c][:, :n], in0=rawf[c][:, :n], in1=p[:, :n], op=ALU.mult), reads=[rawf[c], p], writes=[sqb[c]])
                        pss = ps[3]
                        for c in range(nchunk):
                            T.mm(pss, lambda e: e.matmul(pss[:, :n], lhsT=k["onesb"][:], rhs=sqb[c][:, :n], start=(c == 0), stop=(c == nchunk - 1)),
                                 [k["onesb"], sqb[c]], c == 0, c == nchunk - 1)
                        T.op("dve", lambda e: e.tensor_scalar(out=rs[:, :n], in0=pss[:, :n], scalar1=1.0 / nfeat, scalar2=EPS, op0=ALU.mult, op1=ALU.add), reads=[pss], writes=[rs])
                        T.op("act", lambda e: e.activation(out=rs[:, :n], in_=rs[:, :n], func=AF.Sqrt), reads=[rs], writes=[rs])
                        T.op("dve", lambda e: e.reciprocal(out=rs[:, :n], in_=rs[:, :n]), reads=[rs], writes=[rs])
                        for c in range(nchunk):
                            T.op("dve", lambda e: e.scalar_tensor_tensor(out=dst_t[:, c, t0:t0 + n], in0=rawf[c][:, :n], scalar=g[:, c:c + 1], in1=rs[:, :n], op0=ALU.mult, op1=ALU.mult),
                                 reads=[rawf[c], g, rs], writes=[dst[t0 // 128 + j] for j in range(ntile)])
            proj_fm(C, k, kr, hT, 8, wlat, 640, 32, ps[0:2], rope=(cosR, sinR, P32), tmp=(raw, t1, ps[2:4]))
            qn = C.sb([128, NTOK], BF16)
            qr = C.sb([128, NTOK], BF16)
            kn = C.sb([128, NTOK], BF16)
            T.op("dve", lambda e: e.memset(qn[64:128, :], 0.0), writes=[qn])
            T.op("dve", lambda e: e.memset(kn[64:128, :], 0.0), writes=[kn])
            T.op("dve", lambda e: e.memset(qr[32:64, :], 0.0), writes=[qr])
            T.op("dve", lambda e: e.memset(qr[64:128, :], 0.0), writes=[qr])
            vaug = C.sb([128, 18, 65], BF16)
            T.op("dve", lambda e: e.memset(vaug[:, :, 64:65], 1.0), writes=[vaug])
            om = C.sb([128, 18, 64], F32)
            omb = C.sb([128, 18, 64], BF16)
            pts = [C.sb([128, 512], BF16) for _ in range(3)]
            for h in range(8):
                proj_fm(C, k, qn, cqn, 3, qupb, h * 96, 64, ps[0:2])
                proj_fm(C, k, qr, cqn, 3, qupb, h * 96 + 64, 32, ps[0:2], rope=(cosR, sinR, P32), tmp=(raw, t1, ps[2:4]))
                proj_fm(C, k, kn, ckvn, 2, kvupb, h * 128, 64, ps[0:2])
                proj_tm(C, vaug, ckvn, 2, kvupb, h * 128 + 64, 64, ps[0:2])
                attend(C, [(kn, qn, 128), (kr, qr, 128)], vaug, 64, MLA_SCALE, om, ps[0:2], ps[4:8], pts)
                T.op("act", lambda e: e.copy(out=omb[:], in_=om[:]), reads=[om], writes=[omb])
                T.dma("sp", attn_d.t.rearrange("(t p) d -> p t d", p=128)[:, :, 512 + h * 64:512 + (h + 1) * 64], omb[:], reads=[omb], writes=[attn_d])
    woutb = load_cast(C, "sp", wout_d.t.rearrange("(c p) f -> p c f", p=128), [128, 8, D])
    with scope(C):
        M2 = {"l": bcast_load(C, "sp", modv(0, 2), D), "c": bcast_load(C, "act", modv(1, 2), D)}
        at = [C.sb([128, D], BF16) for _ in range(2)]
        aT = [C.sb([128, 8, 128], BF16) for _ in range(2)]
        xt = [C.sb([128, D], F32) for _ in range(2)]
        tmp = [C.sb([128, D], F32) for _ in range(2)]
        for t in range(NT_TILES):
            nm = "c" if t < 2 else "l"
            a, aTt, x, tm = at[t % 2], aT[t % 2], xt[t % 2], tmp[t % 2]
            T.dma("sp", a[:], attn_d.t[t * 128:(t + 1) * 128, :], reads=[attn_d], writes=[a])
            T.dma("act", x[:], x_tiles[t].t, reads=[x_tiles[t]], writes=[x])
            p = ps[t % 2]
            pv = p.t[:].bitcast(BF16)
            for c in range(8):
                T.op("pe", lambda e: e.transpose(pv[:, c * 128:(c + 1) * 128], a[:, c * 128:(c + 1) * 128], k["identb"][:]), reads=[a, k["identb"]], writes=[p])
            evac(C, t, aTt[:], pv.rearrange("p (c n) -> p c n", c=8), [p], [aTt])
            for half in range(2):
                po = ps[2 + half]
                for c in range(8):
                    T.mm(po, lambda e: e.matmul(po[:], lhsT=aTt[:, c, :], rhs=woutb[:, c, half * 512:(half + 1) * 512], start=(c == 0), stop=(c == 7)),
                         [aTt, woutb], c == 0, c == 7)
                T.op("dve", lambda e: e.tensor_tensor(out=tm[:, half * 512:(half + 1) * 512], in0=po[:], in1=M2[nm][:, half * 512:(half + 1) * 512], op=ALU.mult),
                     reads=[po, M2[nm]], writes=[tm])
            T.op("dve", lambda e: e.tensor_tensor(out=tm[:], in0=tm[:], in1=x[:], op=ALU.add), reads=[tm, x], writes=[tm])
            T.dma("pool", xmid_tiles[t].t, tm[:], reads=[tm], writes=[xmid_tiles[t]])
    h_tok = [C.sb([128, D], BF16) for _ in range(NT_TILES)]
    logits = C.sb([128, 18, 16], F32)
    with scope(C):
        router_sb = C.sb([128, 8, NE], F32)
        T.dma("sp", router_sb[:], rt_d.t.rearrange("(c p) e -> p c e", p=128), writes=[router_sb])
        cb = router_cb_factory(C, k, router_sb, logits, ps[2:4], ps[4])
        norm_mod_transpose(C, k, xmid_tiles, n2g_d.t, modv(0, 3), modv(0, 4), modv(1, 3), modv(1, 4), None, ps[0:2], h_tok=h_tok, tile_cb=cb)
    route_and_gather(C, k, h_tok, logits, xeT_d, meta_d, ps, True)
    T.finish([xeT_d, meta_d] + xmid_tiles)


def build_attn():
    nc = new_nc()
    with ExitStack() as es:
        C = Ctx(nc, es)
        body_attn(C)
    return nc

def moe_scatter(C, k, xin_tiles, y_d, meta_d, m5_l, m5_c, ps, with_ctx, tile_done):
    T = C.T
    NS = 288 if with_ctx else 256
    t_first = 0 if with_ctx else 2
    with scope(C):
        xres = {}
        for t in range(t_first, NT_TILES):
            xres[t] = C.sb([128, D], F32)
            T.dma(["sp", "act"][t % 2], xres[t][:], xin_tiles[t].t, reads=[xin_tiles[t]], writes=[xres[t]])
        M5l = bcast_load(C, "sp", m5_l, D)
        M5c = bcast_load(C, "act", m5_c, D) if with_ctx else None
        ml = C.sb([128, 4, 2, 16], F32)
        for st in range(2):
            for c in range(4):
                T.dma(["sp", "act"][c % 2], ml[:, c, st, :], meta_d.t[c, :, st * 128:(st + 1) * 128].rearrange("e p -> p e"), writes=[ml], allow_slow_non_contiguous=True)
        idxl = C.sb([128, 2, 16], F32)
        gl = C.sb([128, 2, 16], F32)
        T.op("dve", lambda e: e.scalar_tensor_tensor(out=idxl.t.rearrange("p a b -> p (a b)"), in0=ml.t[:, 0].rearrange("p a b -> p (a b)"), scalar=128.0,
                                                     in1=ml.t[:, 1].rearrange("p a b -> p (a b)"), op0=ALU.mult, op1=ALU.add), reads=[ml], writes=[idxl])
        T.op("dve", lambda e: e.tensor_tensor(out=gl[:], in0=ml[:, 2], in1=ml[:, 3], op=ALU.add), reads=[ml], writes=[gl])
        if with_ctx:
            mc = C.sb([32, 4, 16], F32)
            for c in range(4):
                T.dma(["sp", "act"][c % 2], mc[:, c, :], meta_d.t[c, :, 256:288].rearrange("e p -> p e"), writes=[mc], allow_slow_non_contiguous=True)
            idxc = C.sb([32, 16], F32)
            gc = C.sb([32, 16], F32)
            T.op("dve", lambda e: e.scalar_tensor_tensor(out=idxc[:], in0=mc[:, 0, :], scalar=128.0, in1=mc[:, 1, :], op0=ALU.mult, op1=ALU.add), reads=[mc], writes=[idxc])
            T.op("dve", lambda e: e.tensor_tensor(out=gc[:], in0=mc[:, 2, :], in1=mc[:, 3, :], op=ALU.add), reads=[mc], writes=[gc])
        iota_t = C.sb([128, NTOK], U16)
        T.op("pool", lambda e: e.iota(iota_t[:], pattern=[[1, NTOK]], base=0, channel_multiplier=0, allow_small_or_imprecise_dtypes=True), writes=[iota_t])
        GE = 2
        ohs = [[C.sb([128, SEQ], BF16) for _ in range(2 * GE)] for _ in range(2)]
        yss = [[C.sb([128, D], BF16) for _ in range(2 * GE)] for _ in range(2)]
        yst = [C.sb([128, D], F32) for _ in range(3)]
        ohc = [[C.sb([32, CTX], BF16) for _ in range(GE)] for _ in range(2)]
        ysc = [[C.sb([32, D], BF16) for _ in range(GE)] for _ in range(2)]
        li = 0
        pi_ = 0

        def prep(g):
            nonlocal li
            gb = g % 2
            for ee in range(GE):
                ex = g * GE + ee
                for st in range(2):
                    oh, ys, yt = ohs[gb][ee * 2 + st], yss[gb][ee * 2 + st], yst[li % 3]
                    li += 1
                    T.dma(["sp", "act"][li % 2], yt[:], y_d.t[ex, st * 128:(st + 1) * 128, :], reads=[y_d], writes=[yt])
                    T.op("dve", lambda e: e.scalar_tensor_tensor(out=ys[:], in0=yt[:], scalar=gl[:, st, ex:ex + 1], in1=M5l[:], op0=ALU.mult, op1=ALU.mult),
                         reads=[yt, gl, M5l], writes=[ys])
                    T.op("dve", lambda e: e.tensor_scalar(out=oh[:], in0=iota_t[:, 256:NTOK], scalar1=idxl[:, st, ex:ex + 1], scalar2=None, op0=ALU.is_equal),
                         reads=[iota_t, idxl], writes=[oh])
                if with_ctx:
                    oh, ys, yt = ohc[gb][ee], ysc[gb][ee], yst[li % 3]
                    li += 1
                    T.dma("sp", yt[:32, :], y_d.t[ex, 256:288, :], reads=[y_d], writes=[yt])
                    T.op("dve", lambda e: e.scalar_tensor_tensor(out=ys[:], in0=yt[:32, :], scalar=gc[:, ex:ex + 1], in1=M5c[:32, :], op0=ALU.mult, op1=ALU.mult),
                         reads=[yt, gc, M5c], writes=[ys])
                    T.op("dve", lambda e: e.tensor_scalar(out=oh[:], in0=iota_t[:32, 0:CTX], scalar1=idxc[:, ex:ex + 1], scalar2=None, op0=ALU.is_equal),
                         reads=[iota_t, idxc], writes=[oh])
        def accum(g):
            nonlocal pi_
            gb = g % 2
            for t in range(t_first, NT_TILES):
                for half in range(2):
                    p = ps[pi_ % 4]
                    pi_ += 1
                    if t >= 2:
                        n = 2 * GE
                        for j in range(n):
                            T.mm(p, lambda e: e.matmul(p[:], lhsT=ohs[gb][j][:, (t - 2) * 128:(t - 1) * 128], rhs=yss[gb][j][:, half * 512:(half + 1) * 512],
                                                       start=(j == 0), stop=(j == n - 1)), [ohs[gb][j], yss[gb][j]], j == 0, j == n - 1)
                    else:
                        for j in range(GE):
                            T.mm(p, lambda e: e.matmul(p[:], lhsT=ohc[gb][j][:, t * 128:(t + 1) * 128], rhs=ysc[gb][j][:, half * 512:(half + 1) * 512],
                                                       start=(j == 0), stop=(j == GE - 1)), [ohc[gb][j], ysc[gb][j]], j == 0, j == GE - 1)
                    T.op("dve", lambda e: e.tensor_tensor(out=xres[t][:, half * 512:(half + 1) * 512], in0=p[:], in1=xres[t][:, half * 512:(half + 1) * 512], op=ALU.add),
                         reads=[p, xres[t]], writes=[xres[t]])
        NG = NE // GE
        prep(0)
        for g in range(NG):
            if g + 1 < NG:
                prep(g + 1)
            accum(g)
        for t in range(t_first, NT_TILES):
            tile_done(t, xres[t])


NCH = NTOK // 64
O_GQ, O_GK, O_GV, O_GG, O_GD, O_HQ, O_HF, O_HI, O_HG = 0, 256, 512, 1024, 1536, 1568, 2080, 3104, 3616
REC_IN = 4128


def body_rec(C):
    nc = C.nc
    T = C.T
    xmid_d = C.din("xmid", [NTOK, D], F32)
    y_d = C.din("y", [NE, 288, D], F32)
    meta_in = C.din("meta_in", [4, NE, 288], F32)
    mod0_d = C.din("mod0", [2, 6 * D], F32)
    mod_d = C.din("mod", [2, 6 * D], F32)
    n1g_d = C.din("n1g", [D], F32)
    n2g_d = C.din("n2g", [D], F32)
    win_d = C.din("w_in", [D, REC_IN], F32)
    gkup_d = C.din("gk_up", [2, 16, 256], F32)
    gkb_d = C.din("gk_bias", [2, 256], F32)
    glag_d = C.din("gla_g", [128], F32)
    hgg_d = C.din("hg_g", [128], F32)
    lbl_d = C.din("lb_logits", [2, 2, 512], F32)
    wout_d = C.din("w_out", [D, D], F32)
    rt_d = C.din("router", [D, NE], F32)
    xmid1_d = C.dout("xmid1", [NTOK, D], F32)
    xeT_d = C.dout("xeT", [NE, D, 256], BF16)
    meta_d = C.dout("meta", [4, NE, 256], F32)
    x1_d = Buf(nc.dram_tensor("x1_scr", [NTOK, D], F32, kind="Internal").ap())
    a_d = Buf(nc.dram_tensor("a_scr", [NTOK, D], BF16, kind="Internal").ap())
    xmid_tiles = [Buf(xmid_d.t[t * 128:(t + 1) * 128, :]) for t in range(NT_TILES)]
    x1_tiles = [Buf(x1_d.t[t * 128:(t + 1) * 128, :]) for t in range(NT_TILES)]
    xmid1_tiles = [Buf(xmid1_d.t[t * 128:(t + 1) * 128, :]) for t in range(NT_TILES)]
    ps = [C.ps([128, 512]) for _ in range(8)]
    k = make_consts(C)
    win_v = win_d.t.rearrange("(k p) f -> p k f", p=128)

    def modv(row, i):
        return mod_d.t[row, i * D:(i + 1) * D]

    def done0(t, xb):
        T.dma(["sp", "act"][t % 2], x1_tiles[t].t, xb[:], reads=[xb], writes=[x1_tiles[t]])
    moe_scatter(C, k, xmid_tiles, y_d, meta_in, mod0_d.t[0, 5 * D:6 * D], mod0_d.t[1, 5 * D:6 * D], ps, True, done0)

    with scope(C):
        hT_t = C.sb([128, 8, NTOK], BF16)
        hT = tile_views(hT_t)
        norm_mod_transpose(C, k, x1_tiles, n1g_d.t, modv(0, 0), modv(0, 1), modv(1, 0), modv(1, 1), hT, ps[0:2])
        dji = k["dji"]
        fb = C.sb([128, 128], F32)
        pb = C.sb([128, 1], F32)
        eq = C.sb([128, 128], F32)
        Mf = C.sb([128, 128], F32)
        Mb = C.sb([128, 128], F32)
        T.op("pool", lambda e: e.iota(fb[:], pattern=[[1, 128]], base=0, channel_multiplier=0, allow_small_or_imprecise_dtypes=True), writes=[fb])
        T.op("pool", lambda e: e.iota(pb[:], pattern=[[0, 1]], base=0, channel_multiplier=1, allow_small_or_imprecise_dtypes=True), writes=[pb])
        T.op("dve", lambda e: e.tensor_single_scalar(out=fb[:], in_=fb[:], scalar=64.0, op=ALU.is_ge), reads=[fb], writes=[fb])
        T.op("dve", lambda e: e.tensor_single_scalar(out=pb[:], in_=pb[:], scalar=64.0, op=ALU.is_ge), reads=[pb], writes=[pb])
        T.op("dve", lambda e: e.tensor_scalar(out=eq[:], in0=fb[:], scalar1=pb[:, 0:1], scalar2=None, op0=ALU.is_equal), reads=[fb, pb], writes=[eq])
        T.op("dve", lambda e: e.tensor_single_scalar(out=Mf[:], in_=dji[:], scalar=0.0, op=ALU.is_ge), reads=[dji], writes=[Mf])
        T.op("dve", lambda e: e.tensor_tensor(out=Mf[:], in0=Mf[:], in1=eq[:], op=ALU.mult), reads=[Mf, eq], writes=[Mf])
        T.op("dve", lambda e: e.tensor_single_scalar(out=Mb[:], in_=dji[:], scalar=0.0, op=ALU.is_le), reads=[dji], writes=[Mb])
        T.op("dve", lambda e: e.tensor_tensor(out=Mb[:], in0=Mb[:], in1=eq[:], op=ALU.mult), reads=[Mb, eq], writes=[Mb])
        rm = C.sb([128, NCH, 64], BF16)
        mA = C.sb([128, NT_TILES, 128], BF16)
        with scope(C):
            tmpi = C.sb([128, NTOK], F32)
            T.op("pool", lambda e: e.iota(tmpi.t.rearrange("p (a b) -> p a b", b=64), pattern=[[0, NCH], [1, 64]], base=0, channel_multiplier=0, allow_small_or_imprecise_dtypes=True), writes=[tmpi])
            T.op("dve", lambda e: e.tensor_single_scalar(out=rm.t.rearrange("p a b -> p (a b)"), in_=tmpi[:], scalar=0.0, op=ALU.is_gt), reads=[tmpi], writes=[rm])
            T.op("pool", lambda e: e.iota(tmpi.t.rearrange("p (a b) -> p a b", b=128), pattern=[[0, NT_TILES], [1, 128]], base=0, channel_multiplier=0, allow_small_or_imprecise_dtypes=True), reads=[tmpi], writes=[tmpi])
            T.op("dve", lambda e: e.tensor_single_scalar(out=mA.t.rearrange("p a b -> p (a b)"), in_=tmpi[:], scalar=64.0, op=ALU.is_lt), reads=[tmpi], writes=[mA])
        rmf = rm.t.rearrange("p a b -> p (a b)")
        mAf = mA.t.rearrange("p a b -> p (a b)")
        wgd = load_cast(C, "sp", win_v[:, :, O_GD:O_GD + 32], [128, 8, 32])
        gd = [C.sb([16, NTOK], BF16) for _ in range(2)]
        for d in range(2):
            proj_fm(C, k, gd[d], hT, 8, wgd, d * 16, 16, ps[0:2])
        gkupb = C.sb([16, 2, 256], BF16)
        with scope(C):
            st = C.sb([16, 2, 256], F32)
            T.dma("sp", st[:], gkup_d.t.rearrange("d r f -> r d f"), writes=[st])
            T.op("dve", lambda e: e.tensor_copy(out=gkupb[:], in_=st[:]), reads=[st], writes=[gkupb])
        ngkb = C.sb([64, 2, 4], F32)
        T.dma("pool", ngkb[:], gkb_d.t.rearrange("d (h p) -> p d h", p=64), writes=[ngkb], allow_slow_non_contiguous=True)
        T.op("dve", lambda e: e.tensor_scalar(out=ngkb[:], in0=ngkb[:], scalar1=-1.0, scalar2=None, op0=ALU.mult), reads=[ngkb], writes=[ngkb])
        lbt = C.sb([128, 2, 2, 4], F32)
        T.dma("pool", lbt[:], lbl_d.t.rearrange("l d (h p) -> p l d h", p=128), writes=[lbt], allow_slow_non_contiguous=True)
        lb = C.sb([128, 2, 4], F32)
        oml = C.sb([128, 2, 4], F32)
        T.op("dve", lambda e: e.tensor_tensor(out=lb[:], in0=lbt[:, 1], in1=lbt[:, 0], op=ALU.subtract), reads=[lbt], writes=[lb])
        T.op("act", lambda e: e.activation(out=lb[:], in_=lb[:], func=AF.Sigmoid), reads=[lb], writes=[lb])
        T.op("dve", lambda e: e.tensor_scalar(out=oml[:], in0=lb[:], scalar1=-1.0, scalar2=1.0, op0=ALU.mult, op1=ALU.add), reads=[lb], writes=[oml])
        Gg = {"gla": bcast_load(C, "sp", glag_d.t, 128), "hg": bcast_load(C, "act", hgg_d.t, 128)}
        wsts = [C.sb([128, 8, 128], F32) for _ in range(2)]
        whb = C.sb([128, 8, 640], BF16)
        qf = C.sb([128, NTOK], F32)
        kf = C.sb([128, NTOK], F32)
        la = C.sb([128, NTOK], F32)
        bb = C.sb([128, NTOK], F32)
        tA = C.sb([128, NTOK], F32)
        tB = C.sb([128, NTOK], F32)
        qe = C.sb([128, NTOK], BF16)
        qeA = C.sb([128, NTOK], BF16)
        qeB = C.sb([128, NTOK], BF16)
        qt = C.sb([128, NTOK], BF16)
        kt = C.sb([128, NTOK], BF16)
        kh = C.sb([128, NTOK], BF16)
        dec = C.sb([128, NCH], F32)
        khtm = C.sb([128, NT_TILES, 128], BF16)
        vtm = C.sb([128, NT_TILES, 128], BF16)
        sgate = C.sb([128, 16, 128], BF16)
        ofw = C.sb([128, 16, 128], F32)
        ssq = C.sb([128, 16], F32)
        ab = C.sb([128, 16, 128], BF16)
        Sf = [C.sb([128, 128], F32) for _ in range(2)]
        Sall_t = C.sb([128, NCH, 128], BF16)
        T.op("dve", lambda e: e.memset(Sall_t[64:128], 0.0), writes=[Sall_t])
        for zb in (qeA, qeB, qt, kt):
            T.op("dve", lambda e: e.memset(zb[64:128, :], 0.0), writes=[zb])
        Sall = [Buf(Sall_t.t[:, c, :]) for c in range(NCH)]
        scm = [C.sb([128, 128], BF16) for _ in range(2)]

        def v3(buf):
            return buf.t.rearrange("p (c l) -> p c l", l=64)

        for head in range(8):
            gla = head < 4
            h = head % 4
            dk = 64 if gla else 128
            if gla:
                cols = [(O_GQ + h * 64, 64), (O_GK + h * 64, 64), (O_GV + h * 128, 128), (O_GG + h * 128, 128)]
            else:
                cols = [(O_HQ + h * 128, 128), (O_HF + h * 128, 128), (O_HF + 512 + h * 128, 128), (O_HI + h * 128, 128), (O_HG + h * 128, 128)]
            offs = []
            o = 0
            for j, (c0, n) in enumerate(cols):
                wst = wsts[j % 2]
                T.dma(["sp", "act"][j % 2], wst[:, :, :n], win_v[:, :, c0:c0 + n], writes=[wst])
                T.op("dve", lambda e: e.tensor_copy(out=whb[:, :, o:o + n], in_=wst[:, :, :n]), reads=[wst], writes=[whb])
                offs.append(o)
                o += n
            vo, go = offs[-2], offs[-1]
            proj_tm(C, vtm, hT, 8, whb, vo, 128, ps[0:2])
            for t in range(2, NT_TILES):
                p = ps[t % 2]
                for kk in range(8):
                    T.mm(p, lambda e: e.matmul(p[:, :128], lhsT=hT[t][:, kk, :], rhs=whb[:, kk, go:go + 128], start=(kk == 0), stop=(kk == 7)),
                         [hT[t], whb], kk == 0, kk == 7)
                T.op("act", lambda e: e.activation(out=sgate[:, t - 2, :], in_=p[:, :128], func=AF.Silu), reads=[p], writes=[sgate])

            def fm_raw(dst, col0, func=None, scale=1.0):
                for bi, (t0, n) in enumerate(TOKBLOCKS):
                    p = ps[bi % 2]
                    ntile = n // 128
                    tiles_ = list(hT[t0 // 128:t0 // 128 + ntile])
                    for kk in range(8):
                        T.mm(p, lambda e: e.matmul(p[:dk, :n], lhsT=whb[:, kk, col0:col0 + dk], rhs=hT.full[:, kk, t0:t0 + n],
                                                   start=(kk == 0), stop=(kk == 7)), [whb] + tiles_, kk == 0, kk == 7)
                    T.op("act", lambda e: e.activation(out=dst[:dk, t0:t0 + n], in_=p[:dk, :n], func=(func or AF.Copy), scale=scale), reads=[p], writes=[dst])
            if gla:
                fm_raw(qf, offs[0], None, 64 ** -0.5)
                fm_raw(kf, offs[1])
            else:
                fm_raw(qf, offs[0], AF.Silu)
            for d in range(2):
                if gla:
                    for bi, (t0, n) in enumerate(TOKBLOCKS):
                        p = ps[bi % 2]
                        T.mm(p, lambda e: e.matmul(p[:64, :n], lhsT=gkupb[:, d, h * 64:(h + 1) * 64], rhs=gd[d][:, t0:t0 + n], start=True, stop=True),
                             [gkupb, gd[d]], True, True)
                        T.op("act", lambda e: e.activation(out=la[:64, t0:t0 + n], in_=p[:64, :n], func=AF.Exp, scale=-1.0, bias=ngkb[:, d, h:h + 1]),
                             reads=[p, ngkb], writes=[la])
                    T.op("act", lambda e: e.activation(out=la[:64], in_=la[:64], func=AF.Ln, bias=1.0), reads=[la], writes=[la])
                    T.op("dve", lambda e: e.tensor_scalar(out=la[:64], in0=la[:64], scalar1=-1.0 / 16.0, scalar2=None, op0=ALU.mult), reads=[la], writes=[la])
                else:
                    fm_raw(kf, offs[1 + d], AF.Sigmoid)
                    T.op("dve", lambda e: e.tensor_scalar(out=kf[:], in0=kf[:], scalar1=oml[:, d, h:h + 1], scalar2=lb[:, d, h:h + 1], op0=ALU.mult, op1=ALU.add),
                         reads=[kf, oml, lb], writes=[kf])
                    T.op("act", lambda e: e.activation(out=la[:], in_=kf[:], func=AF.Ln), reads=[kf], writes=[la])
                    T.op("dve", lambda e: e.tensor_scalar(out=kf[:], in0=kf[:], scalar1=-1.0, scalar2=1.0, op0=ALU.mult, op1=ALU.add), reads=[kf], writes=[kf])
                T.op("dve", lambda e: e.tensor_tensor_scan(out=bb[:dk], data0=rmf[:dk], data1=la[:dk], initial=0.0, op0=ALU.mult, op1=ALU.add),
                     reads=[rm, la], writes=[bb])
                b3 = v3(bb)
                T.op("act", lambda e: e.copy(out=dec[:dk], in_=b3[:dk, :, 63]), reads=[bb], writes=[dec])
                decb = dec[:dk].unsqueeze(2).to_broadcast([dk, NCH, 64])
                if d == 1:
                    T.op("dve", lambda e: e.tensor_tensor(out=tB[:dk], in0=bb[:dk], in1=la[:dk], op=ALU.subtract), reads=[bb, la], writes=[tB])
                    T.op("dve", lambda e: e.tensor_tensor(out=b3[:dk], in0=decb, in1=v3(tB)[:dk], op=ALU.subtract), reads=[dec, tB], writes=[bb])
                    iref = 31
                else:
                    T.op("dve", lambda e: e.tensor_tensor(out=v3(tB)[:dk], in0=decb, in1=b3[:dk], op=ALU.subtract), reads=[dec, bb], writes=[tB])
                    iref = 32
                T.op("dve", lambda e: e.tensor_tensor(out=v3(tA)[:dk], in0=b3[:dk], in1=b3[:dk, :, iref:iref + 1].to_broadcast([dk, NCH, 64]), op=ALU.subtract),
                     reads=[bb], writes=[tA])
                T.op("act", lambda e: e.activation(out=la[:dk], in_=bb[:dk], func=AF.Exp), reads=[bb], writes=[la])
                T.op("act", lambda e: e.activation(out=bb[:dk], in_=tA[:dk], func=AF.Exp), reads=[tA], writes=[bb])
                T.op("act", lambda e: e.activation(out=tB[:dk], in_=tB[:dk], func=AF.Exp), reads=[tB], writes=[tB])
                T.op("dve", lambda e: e.tensor_tensor(out=qe[:dk], in0=qf[:dk], in1=la[:dk], op=ALU.mult), reads=[qf, la], writes=[qe])
                T.op("act", lambda e: e.activation(out=la[:dk], in_=tA[:dk], func=AF.Exp, scale=-1.0), reads=[tA], writes=[la])
                T.op("dve", lambda e: e.tensor_tensor(out=qt[:dk], in0=qf[:dk], in1=bb[:dk], op=ALU.mult), reads=[qf, bb], writes=[qt])
                T.op("dve", lambda e: e.tensor_tensor(out=kh[:dk], in0=kf[:dk], in1=tB[:dk], op=ALU.mult), reads=[kf, tB], writes=[kh])
                T.op("dve", lambda e: e.tensor_tensor(out=qeA[:dk], in0=qe[:dk], in1=mAf[:dk], op=ALU.mult), reads=[qe, mA], writes=[qeA])
                T.op("dve", lambda e: e.tensor_tensor(out=qeB[:dk], in0=qe[:dk], in1=qeA[:dk], op=ALU.subtract), reads=[qe, qeA], writes=[qeB])
                T.op("dve", lambda e: e.tensor_tensor(out=kt[:dk], in0=kf[:dk], in1=la[:dk], op=ALU.mult), reads=[kf, la], writes=[kt])
                T.op("act", lambda e: e.activation(out=dec[:dk], in_=dec[:dk], func=AF.Exp), reads=[dec], writes=[dec])
                for t in range(NT_TILES):
                    p = ps[t % 2]
                    pv = p.t[:].bitcast(BF16)
                    T.op("pe", lambda e: e.transpose(pv[:, :dk], kh[:dk, t * 128:(t + 1) * 128], k["identb"][:dk, :dk]), reads=[kh, k["identb"]], writes=[p])
                    evac(C, t, khtm[:, t, :dk], pv[:, :dk], [p], [khtm])
                order = list(range(NCH)) if d == 0 else [3, 2, 1, 0] + list(range(NCH - 1, 3, -1))
                Mm = Mf if d == 0 else Mb
                T.op("dve", lambda e: e.memset(Sf[0][:], 0.0), writes=[Sf[0]])
                for ci, c in enumerate(order):
                    t, half = c // 2, c % 2
                    r0 = half * 64
                    Scur, Snew = Sf[ci % 2], Sf[(ci + 1) % 2]
                    if c >= 4:
                        T.op("act", lambda e: e.copy(out=Sall[c][:dk], in_=Scur[:dk]), reads=[Scur], writes=[Sall[c]])
                    if ci == len(order) - 1:
                        break
                    pk = ps[ci % 4]
                    T.mm(pk, lambda e: e.matmul(pk[:dk, :128], lhsT=khtm[r0:r0 + 64, t, :dk], rhs=vtm[r0:r0 + 64, t, :], start=True, stop=True),
                         [khtm, vtm], True, True)
                    T.op("dve", lambda e: e.scalar_tensor_tensor(out=Snew[:dk], in0=Scur[:dk], scalar=dec[:dk, c:c + 1], in1=pk[:dk, :128], op0=ALU.mult, op1=ALU.add),
                         reads=[Scur, dec, pk], writes=[Snew])
                def p2_scores(t):
                    pS = ps[4 + t % 2]
                    T.mm(pS, lambda e: e.matmul(pS[:, :128], lhsT=kt[:, t * 128:(t + 1) * 128], rhs=qt[:, t * 128:(t + 1) * 128], start=True, stop=True),
                         [kt, qt], True, True)
                    sc = scm[t % 2]
                    T.op("dve", lambda e: e.tensor_tensor(out=sc[:], in0=pS[:, :128], in1=Mm[:], op=ALU.mult), reads=[pS, Mm], writes=[sc])

                def p2_readout(t):
                    sc = scm[t % 2]
                    po = ps[6 + t % 2]
                    SA, SB_ = Sall[2 * t], Sall[2 * t + 1]
                    T.mm(po, lambda e: e.matmul(po[:, :128], lhsT=sc[:], rhs=vtm[:, t, :], start=True, stop=False), [sc, vtm], True, False)
                    T.mm(po, lambda e: e.matmul(po[:, :128], lhsT=qeA[:, t * 128:(t + 1) * 128], rhs=SA[:], start=False, stop=False), [qeA, SA], False, False)
                    T.mm(po, lambda e: e.matmul(po[:, :128], lhsT=qeB[:, t * 128:(t + 1) * 128], rhs=SB_[:], start=False, stop=True), [qeB, SB_], False, True)
                    if d == 0:
                        T.op("act", lambda e: e.copy(out=ofw[:, t - 2, :], in_=po[:, :128]), reads=[po], writes=[ofw])
                    else:
                        T.op("dve", lambda e: e.tensor_tensor(out=ofw[:, t - 2, :], in0=po[:, :128], in1=ofw[:, t - 2, :], op=ALU.add), reads=[po, ofw], writes=[ofw])

                p2_scores(2)
                for t in range(2, NT_TILES):
                    if t + 1 < NT_TILES:
                        p2_scores(t + 1)
                    p2_readout(t)
            sqv = tA.t[:, 0:2048].rearrange("p (a b) -> p a b", b=128)
            T.op("act", lambda e: e.activation(out=sqv, in_=ofw[:], func=AF.Square), reads=[ofw], writes=[tA])
            T.op("dve", lambda e: e.tensor_reduce(out=ssq[:], in_=sqv, axis=AX.X, op=ALU.add), reads=[tA], writes=[ssq])
            r = rstd_from_ss(C, ssq, 128, W=16)
            G = Gg["gla" if gla else "hg"]
            for t in range(16):
                T.op("dve", lambda e: e.scalar_tensor_tensor(out=sqv[:, t, :], in0=ofw[:, t, :], scalar=r[:, t:t + 1], in1=G[:], op0=ALU.mult, op1=ALU.mult),
                     reads=[ofw, r, G], writes=[tA])
            T.op("dve", lambda e: e.tensor_tensor(out=ab[:], in0=sqv, in1=sgate[:], op=ALU.mult), reads=[tA, sgate], writes=[ab])
            T.dma("sp", a_d.t.rearrange("(t p) d -> p t d", p=128)[:, 2:, head * 128:(head + 1) * 128], ab[:], reads=[ab], writes=[a_d])
    woutb = load_cast(C, "sp", wout_d.t.rearrange("(c p) f -> p c f", p=128), [128, 8, D])
    with scope(C):
        M2 = bcast_load(C, "sp", modv(0, 2), D)
        at = [C.sb([128, D], BF16) for _ in range(2)]
        aT = [C.sb([128, 8, 128], BF16) for _ in range(2)]
        xt = [C.sb([128, D], F32) for _ in range(2)]
        tmp = [C.sb([128, D], F32) for _ in range(2)]
        for t in range(2, NT_TILES):
            a, aTt, x, tm = at[t % 2], aT[t % 2], xt[t % 2], tmp[t % 2]
            T.dma("sp", a[:], a_d.t[t * 128:(t + 1) * 128, :], reads=[a_d], writes=[a])
            T.dma("act", x[:], x1_tiles[t].t, reads=[x1_tiles[t]], writes=[x])
            p = ps[t % 2]
            pv = p.t[:].bitcast(BF16)
            for c in range(8):
                T.op("pe", lambda e: e.transpose(pv[:, c * 128:(c + 1) * 128], a[:, c * 128:(c + 1) * 128], k["identb"][:]), reads=[a, k["identb"]], writes=[p])
            evac(C, t, aTt[:], pv.rearrange("p (c n) -> p c n", c=8), [p], [aTt])
            for half in range(2):
                po = ps[2 + half]
                for c in range(8):
                    T.mm(po, lambda e: e.matmul(po[:], lhsT=aTt[:, c, :], rhs=woutb[:, c, half * 512:(half + 1) * 512], start=(c == 0), stop=(c == 7)),
                         [aTt, woutb], c == 0, c == 7)
                T.op("dve", lambda e: e.tensor_tensor(out=tm[:, half * 512:(half + 1) * 512], in0=po[:], in1=M2[:, half * 512:(half + 1) * 512], op=ALU.mult),
                     reads=[po, M2], writes=[tm])
            T.op("dve", lambda e: e.tensor_tensor(out=tm[:], in0=tm[:], in1=x[:], op=ALU.add), reads=[tm, x], writes=[tm])
            T.dma("pool", xmid1_tiles[t].t, tm[:], reads=[tm], writes=[xmid1_tiles[t]])
    h_tok = [C.sb([128, D], BF16) for _ in range(NT_TILES)]
    logits = C.sb([128, 18, 16], F32)
    T.op("dve", lambda e: e.memset(logits[:], 0.0), writes=[logits])
    with scope(C):
        router_sb = C.sb([128, 8, NE], F32)
        T.dma("sp", router_sb[:], rt_d.t.rearrange("(c p) e -> p c e", p=128), writes=[router_sb])
        cb = router_cb_factory(C, k, router_sb, logits, ps[2:4], ps[4])
        norm_mod_transpose(C, k, xmid1_tiles, n2g_d.t, modv(0, 3), modv(0, 4), modv(1, 3), modv(1, 4), None, ps[0:2], h_tok=h_tok, tile_cb=cb,
                           tiles=list(range(2, NT_TILES)))
    route_and_gather(C, k, h_tok, logits, xeT_d, meta_d, ps, False)
    T.finish([xeT_d, meta_d] + xmid1_tiles[2:])


def build_rec():
    nc = new_nc()
    with ExitStack() as es:
        C = Ctx(nc, es)
        body_rec(C)
    return nc

def body_final(C):
    nc = C.nc
    T = C.T
    xmid_d = C.din("xmid", [NTOK, D], F32)
    y_d = C.din("y", [NE, 256, D], F32)
    meta_in = C.din("meta_in", [4, NE, 256], F32)
    mod_d = C.din("mod", [2, 6 * D], F32)
    fg_d = C.din("fg", [D], F32)
    out_d = C.dout("out", [SEQ, D], F32)
    xmid_tiles = [Buf(xmid_d.t[t * 128:(t + 1) * 128, :]) for t in range(NT_TILES)]
    out_tiles = [Buf(out_d.t[t * 128:(t + 1) * 128, :]) for t in range(16)]
    ps = [C.ps([128, 512]) for _ in range(4)]
    k = make_consts(C)
    G = bcast_load(C, "sp", fg_d.t, D)
    junk = C.sb([128, D], BF16)
    ss = [C.sb([128, 1], F32) for _ in range(2)]
    ob = [C.sb([128, D], F32) for _ in range(2)]

    def done(t, xb):
        s = ss[t % 2]
        o = ob[t % 2]
        T.op("act", lambda e: e.activation(out=junk[:], in_=xb[:], func=AF.Square, accum_out=s[:, 0:1]), reads=[xb], writes=[junk, s])
        r = rstd_from_ss(C, s, D)
        T.op("dve", lambda e: e.scalar_tensor_tensor(out=o[:], in0=xb[:], scalar=r[:, 0:1], in1=G[:], op0=ALU.mult, op1=ALU.mult), reads=[xb, r, G], writes=[o])
        T.dma(["sp", "act"][t % 2], out_tiles[t - 2].t, o[:], reads=[o], writes=[out_tiles[t - 2]])
    moe_scatter(C, k, xmid_tiles, y_d, meta_in, mod_d.t[0, 5 * D:6 * D], None, ps, False, done)
    T.finish(out_tiles)


def build_final():
    nc = new_nc()
    with ExitStack() as es:
        C = Ctx(nc, es)
        body_final(C)
    return nc

DEBUG = {}
_NC_CACHE = {}


def _get(name, fn, *a):
    key = (name,) + a
    if key not in _NC_CACHE:
        _NC_CACHE[key] = fn(*a)
    return _NC_CACHE[key]


def _run(nc, ims):
    ims = [{k: np.ascontiguousarray(v) for k, v in im.items()} for im in ims]
    return run_bass_kernel_spmd(nc, ims, core_ids=list(range(NCORES))).results


def _ffn_launch(xeTs, wg, wu, wd, NS):
    nc = _get("ffn", build_ffn, NS * NCORES)
    ims = []
    for c in range(NCORES):
        xT = np.stack([np.concatenate([xeTs[b][2 * c + j] for b in range(NCORES)], axis=1) for j in range(2)])
        ims.append({"xT": xT, "wg": wg[2 * c:2 * c + 2], "wu": wu[2 * c:2 * c + 2], "wd": wd[2 * c:2 * c + 2]})
    res = _run(nc, ims)
    ys = []
    for b in range(NCORES):
        ys.append(np.stack([res[e // 2]["y"][e % 2, b * NS:(b + 1) * NS, :] for e in range(NE)]))
    return ys


def kernel_unfused(x, c, ctx, c_ctx, ada_w, ada_b, norm1_g, norm2_g, att_w_in, mla_q_norm_g, mla_q_up,
           mla_kv_norm_g, mla_kv_up, da_lam_q1, da_lam_k1, da_lam_q2, da_lam_k2, da_subln_g, att_w_out,
           rec_w_in, gla_gk_up, gla_gk_bias, gla_norm_g, hg_lb_logits, hg_norm_g, rec_w_out,
           moe_router, moe_w_gate, moe_w_up, moe_w_down, final_norm_g):
    f = lambda a: np.asarray(a, dtype=np.float32)
    x, c, ctx, c_ctx = f(x), f(c), f(ctx), f(c_ctx)
    cc = np.concatenate([c, c_ctx[None, :]], axis=0)
    res = _run(_get("ada", build_ada), [{"ccT": cc.T, "adaw": f(ada_w)[:, :, i * 768:(i + 1) * 768], "adab": f(ada_b)[:, i * 768:(i + 1) * 768]}
                                        for i in range(NCORES)])
    mods = np.concatenate([r["mods"] for r in res], axis=-1)
    DEBUG["mods"] = mods
    modrows = lambda l, b: np.stack([mods[l, b], mods[l, 8]])
    lamv = np.stack([f(da_lam_q1)[0], f(da_lam_k1)[0], f(da_lam_q2)[0], f(da_lam_k2)[0]])
    ims = []
    for b in range(NCORES):
        ims.append({"xin": np.concatenate([ctx[b], x[b]], axis=0), "mod": modrows(0, b), "n1g": f(norm1_g)[0], "n2g": f(norm2_g)[0],
                    "w_in": f(att_w_in)[0], "qng": f(mla_q_norm_g)[0], "q_up": f(mla_q_up)[0], "kvng": f(mla_kv_norm_g)[0], "kv_up": f(mla_kv_up)[0],
                    "lamv": lamv, "subg": f(da_subln_g)[0], "w_out": f(att_w_out)[0], "router": f(moe_router)[0]})
    r1 = _run(_get("attn", build_attn), ims)
    DEBUG["r1"] = r1
    y0 = _ffn_launch([r["xeT"] for r in r1], f(moe_w_gate)[0], f(moe_w_up)[0], f(moe_w_down)[0], 288)
    DEBUG["y0"] = y0
    ims = []
    for b in range(NCORES):
        ims.append({"xmid": r1[b]["xmid"], "y": y0[b], "meta_in": r1[b]["meta"], "mod0": modrows(0, b), "mod": modrows(1, b),
                    "n1g": f(norm1_g)[1], "n2g": f(norm2_g)[1], "w_in": f(rec_w_in)[0], "gk_up": f(gla_gk_up)[0], "gk_bias": f(gla_gk_bias)[0],
                    "gla_g": f(gla_norm_g)[0], "hg_g": f(hg_norm_g)[0], "lb_logits": f(hg_lb_logits), "w_out": f(rec_w_out)[0], "router": f(moe_router)[1]})
    r3 = _run(_get("rec", build_rec), ims)
    DEBUG["r3"] = r3
    y1 = _ffn_launch([r["xeT"] for r in r3], f(moe_w_gate)[1], f(moe_w_up)[1], f(moe_w_down)[1], 256)
    DEBUG["y1"] = y1
    ims = [{"xmid": r3[b]["xmid1"], "y": y1[b], "meta_in": r3[b]["meta"], "mod": modrows(1, b), "fg": f(final_norm_g)} for b in range(NCORES)]
    r5 = _run(_get("final", build_final), ims)
    return np.stack([r["out"] for r in r5]).astype(np.float32)


def body_ada_full(C, ccT_d, w_d, b_d, mods_d):
    T = C.T
    ccT = C.sb([128, 8, 2], F32)
    scT = C.sb([128, 8, 2], F32)
    T.dma("sp", ccT[:], ccT_d.t.rearrange("(k p) s -> p k s", p=128), writes=[ccT])
    T.op("act", lambda e: e.activation(out=scT[:], in_=ccT[:], func=AF.Silu), reads=[ccT], writes=[scT])
    bias = C.sb([2, 2, 6 * D], F32)
    res = C.sb([2, 2, 6 * D], F32)
    for l in range(2):
        T.dma("pool", bias[:, l, :], b_d.t[l].partition_broadcast(2), writes=[bias])
    wb = [C.sb([128, 8, 1536], F32) for _ in range(2)]
    pss = [C.ps([128, 512]) for _ in range(2)]
    i = 0
    for l in range(2):
        for blk in range(4):
            w = wb[i % 2]
            T.dma(["sp", "act"][i % 2], w[:], w_d.t[l].rearrange("(k p) f -> p k f", p=128)[:, :, blk * 1536:(blk + 1) * 1536], writes=[w])
            for n in range(3):
                p = pss[(i * 3 + n) % 2]
                c0 = blk * 1536 + n * 512
                for k_ in range(8):
                    T.mm(p, lambda e: e.matmul(p[:2, :], lhsT=scT[:, k_, :], rhs=w[:, k_, n * 512:(n + 1) * 512], start=(k_ == 0), stop=(k_ == 7)),
                         [scT, w], k_ == 0, k_ == 7)
                T.op("dve", lambda e: e.tensor_tensor(out=res[:, l, c0:c0 + 512], in0=p[:2, :], in1=bias[:, l, c0:c0 + 512], op=ALU.add),
                     reads=[p, bias], writes=[res])
            i += 1
    T.dma("sp", mods_d.t.rearrange("l s f -> s l f"), res[:], reads=[res], writes=[mods_d])


def body_ffn_bp(C, xeT_d, wg_d, wu_d, wd_d, y_d, NS):
    T = C.T
    ttiles = [(s, min(128, NS - s)) for s in range(0, NS, 128)]
    NB = 4
    xTs = [C.sb([128, 8, NS], BF16) for _ in range(2)]
    hid_t = [C.sb([128, NFC, NS], BF16) for _ in range(2)]
    wdb_t = [C.sb([128, NFC, D], BF16) for _ in range(2)]
    wgb = [C.sb([128, 8, 256], BF16) for _ in range(NB)]
    wub = [C.sb([128, 8, 256], BF16) for _ in range(NB)]
    sg = [C.sb([128, 512], F32) for _ in range(2)]
    ysb = [C.sb([128, D], F32) for _ in range(2)]
    pg = [C.ps([128, 512]) for _ in range(2)]
    pu = [C.ps([128, 512]) for _ in range(2)]
    py = [C.ps([128, 512]) for _ in range(2)]
    it = 0
    yi = 0
    for ex in range(NE):
        xT = xTs[ex % 2]
        hid = [Buf(hid_t[ex % 2].t[:, fc, :]) for fc in range(NFC)]
        wdbt = wdb_t[ex % 2]
        wdb = [Buf(wdbt.t[:, 2 * g:2 * g + 2, :]) for g in range(NFC // 2)]
        T.dma("act", xT[:], xeT_d.t[ex].rearrange("(k p) t -> p k t", p=128), reads=[xeT_d], writes=[xT])
        wgv = wg_d.t[ex].rearrange("(k p) f -> p k f", p=128)
        wuv = wu_d.t[ex].rearrange("(k p) f -> p k f", p=128)
        wdv = wd_d.t[ex].rearrange("(c p) d -> p c d", p=128)
        for g in range(NFC // 2):
            b = it % NB
            it += 1
            T.dma("pool", wgb[b][:], wgv[:, :, g * 256:(g + 1) * 256], writes=[wgb[b]])
            T.dma("pool", wub[b][:], wuv[:, :, g * 256:(g + 1) * 256], writes=[wub[b]])
            T.dma("pool", wdb[g].t, wdv[:, 2 * g:2 * g + 2, :], writes=[wdb[g]])
            for j in range(2):
                fc = 2 * g + j
                pgb, pub, sgb = pg[fc % 2], pu[fc % 2], sg[fc % 2]
                for k_ in range(8):
                    T.mm(pgb, lambda e: e.matmul(pgb[:, :NS], lhsT=wgb[b][:, k_, j * 128:(j + 1) * 128], rhs=xT[:, k_, :], start=(k_ == 0), stop=(k_ == 7)),
                         [wgb[b], xT], k_ == 0, k_ == 7)
                for k_ in range(8):
                    T.mm(pub, lambda e: e.matmul(pub[:, :NS], lhsT=wub[b][:, k_, j * 128:(j + 1) * 128], rhs=xT[:, k_, :], start=(k_ == 0), stop=(k_ == 7)),
                         [wub[b], xT], k_ == 0, k_ == 7)
                T.op("act", lambda e: e.activation(out=sgb[:, :NS], in_=pgb[:, :NS], func=AF.Silu), reads=[pgb], writes=[sgb])
                T.op("dve", lambda e: e.tensor_tensor(out=hid[fc][:], in0=sgb[:, :NS], in1=pub[:, :NS], op=ALU.mult), reads=[sgb, pub], writes=[hid[fc]])
        for (ts, tn) in ttiles:
            yb = ysb[yi % 2]
            yi += 1
            for dh in range(2):
                p = py[dh]
                for fc in range(NFC):
                    T.mm(p, lambda e: e.matmul(p[:tn, :], lhsT=hid[fc][:, ts:ts + tn], rhs=wdbt[:, fc, dh * 512:(dh + 1) * 512], start=(fc == 0), stop=(fc == NFC - 1)),
                         [hid[fc], wdb[fc // 2]], fc == 0, fc == NFC - 1)
                if dh == 0:
                    T.op("act", lambda e: e.copy(out=yb[:tn, 0:512], in_=p[:tn, :]), reads=[p], writes=[yb])
                else:
                    T.op("dve", lambda e: e.tensor_copy(out=yb[:tn, 512:1024], in_=p[:tn, :]), reads=[p], writes=[yb])
            T.dma("sp", y_d.t[ex, ts:ts + tn, :], yb[:tn, :], reads=[yb], writes=[y_d])


def build_fused():
    nc = new_nc()
    with ExitStack() as es:
        C = Ctx(nc, es)
        T = C.T
        I = {}

        def inp(name, shape, dt=F32):
            I[name] = Buf(nc.dram_tensor(name, list(shape), dt, kind="ExternalInput").ap(), name)
            return I[name]
        xin = inp("xin", [NTOK, D])
        ccT = inp("ccT", [D, 2])
        inp("ada_w", [2, D, 6 * D]); inp("ada_b", [2, 6 * D]); inp("norm1_g", [2, D]); inp("norm2_g", [2, D])
        inp("att_w_in", [D, 2208]); inp("qng", [384]); inp("q_up", [384, 768]); inp("kvng", [256]); inp("kv_up", [256, 1024])
        inp("lamv", [4, 64]); inp("subg", [128]); inp("att_w_out", [D, D])
        inp("rec_w_in", [D, REC_IN]); inp("gk_up", [2, 16, 256]); inp("gk_bias", [2, 256]); inp("gla_g", [128]); inp("hg_g", [128])
        inp("lb_logits", [2, 2, 512]); inp("rec_w_out", [D, D]); inp("router", [2, D, NE])
        inp("wg", [2, NE, D, FF]); inp("wu", [2, NE, D, FF]); inp("wd", [2, NE, FF, D]); inp("fg", [D])
        out_d = Buf(nc.dram_tensor("out", [SEQ, D], F32, kind="ExternalOutput").ap(), "out")
        mods = C.scratch("mods_scr", [2, 2, 6 * D], F32)
        xmid0 = C.scratch("xmid0_scr", [NTOK, D], F32)
        xe0 = C.scratch("xe0_scr", [NE, D, 288], BF16)
        meta0 = C.scratch("meta0_scr", [4, NE, 288], F32)
        y0 = C.scratch("y0_scr", [NE, 288, D], F32)
        xmid1 = C.scratch("xmid1_scr", [NTOK, D], F32)
        xe1 = C.scratch("xe1_scr", [NE, D, 256], BF16)
        meta1 = C.scratch("meta1_scr", [4, NE, 256], F32)
        y1 = C.scratch("y1_scr", [NE, 256, D], F32)
        sub = lambda b, i: Buf(b.t[i])
        with scope(C):
            body_ada_full(C, ccT, I["ada_w"], I["ada_b"], mods)
        with scope(C):
            C.over = {"xin": xin, "mod": sub(mods, 0), "n1g": sub(I["norm1_g"], 0), "n2g": sub(I["norm2_g"], 0), "w_in": I["att_w_in"],
                      "qng": I["qng"], "q_up": I["q_up"], "kvng": I["kvng"], "kv_up": I["kv_up"], "lamv": I["lamv"], "subg": I["subg"],
                      "w_out": I["att_w_out"], "router": sub(I["router"], 0), "xmid": xmid0, "xeT": xe0, "meta": meta0}
            body_attn(C)
        with scope(C):
            body_ffn_bp(C, xe0, sub(I["wg"], 0), sub(I["wu"], 0), sub(I["wd"], 0), y0, 288)
        with scope(C):
            C.over = {"xmid": xmid0, "y": y0, "meta_in": meta0, "mod0": sub(mods, 0), "mod": sub(mods, 1), "n1g": sub(I["norm1_g"], 1),
                      "n2g": sub(I["norm2_g"], 1), "w_in": I["rec_w_in"], "gk_up": I["gk_up"], "gk_bias": I["gk_bias"], "gla_g": I["gla_g"],
                      "hg_g": I["hg_g"], "lb_logits": I["lb_logits"], "w_out": I["rec_w_out"], "router": sub(I["router"], 1),
                      "xmid1": xmid1, "xeT": xe1, "meta": meta1}
            body_rec(C)
        with scope(C):
            body_ffn_bp(C, xe1, sub(I["wg"], 1), sub(I["wu"], 1), sub(I["wd"], 1), y1, 256)
        with scope(C):
            C.over = {"xmid": xmid1, "y": y1, "meta_in": meta1, "mod": sub(mods, 1), "fg": I["fg"], "out": out_d}
            body_final(C)
        C.over = None
    return nc


def kernel_fused(x, c, ctx, c_ctx, ada_w, ada_b, norm1_g, norm2_g, att_w_in, mla_q_norm_g, mla_q_up,
                 mla_kv_norm_g, mla_kv_up, da_lam_q1, da_lam_k1, da_lam_q2, da_lam_k2, da_subln_g, att_w_out,
                 rec_w_in, gla_gk_up, gla_gk_bias, gla_norm_g, hg_lb_logits, hg_norm_g, rec_w_out,
                 moe_router, moe_w_gate, moe_w_up, moe_w_down, final_norm_g, cores=None):
    f = lambda a: np.ascontiguousarray(np.asarray(a, dtype=np.float32))
    x, c, ctx, c_ctx = f(x), f(c), f(ctx), f(c_ctx)
    shared = {
        "ada_w": f(ada_w), "ada_b": f(ada_b), "norm1_g": f(norm1_g), "norm2_g": f(norm2_g), "att_w_in": f(att_w_in)[0],
        "qng": f(mla_q_norm_g)[0], "q_up": f(mla_q_up)[0], "kvng": f(mla_kv_norm_g)[0], "kv_up": f(mla_kv_up)[0],
        "lamv": np.stack([f(da_lam_q1)[0], f(da_lam_k1)[0], f(da_lam_q2)[0], f(da_lam_k2)[0]]), "subg": f(da_subln_g)[0],
        "att_w_out": f(att_w_out)[0], "rec_w_in": f(rec_w_in)[0], "gk_up": f(gla_gk_up)[0], "gk_bias": f(gla_gk_bias)[0],
        "gla_g": f(gla_norm_g)[0], "hg_g": f(hg_norm_g)[0], "lb_logits": f(hg_lb_logits), "rec_w_out": f(rec_w_out)[0],
        "router": f(moe_router), "wg": f(moe_w_gate), "wu": f(moe_w_up), "wd": f(moe_w_down), "fg": f(final_norm_g),
    }
    cores = list(range(NCORES)) if cores is None else cores
    ims = []
    for b in cores:
        im = dict(shared)
        im["xin"] = np.concatenate([ctx[b], x[b]], axis=0)
        im["ccT"] = np.ascontiguousarray(np.stack([c[b], c_ctx], axis=1))
        ims.append(im)
    nc = _get("fused", build_fused)
    res = run_bass_kernel_spmd(nc, ims, core_ids=list(range(len(cores)))).results
    return np.stack([r["out"] for r in res]).astype(np.float32)


def kernel(**inputs):
    return kernel_fused(**inputs)
```

```python
from contextlib import ExitStack
import numpy as np
import concourse.bass as bass
import concourse.mybir as mybir
from concourse.alu_op_type import AluOpType as ALU

F32 = mybir.dt.float32
BF16 = mybir.dt.bfloat16
I32 = mybir.dt.int32
U32 = mybir.dt.uint32
U16 = mybir.dt.uint16
AF = mybir.ActivationFunctionType
AX = mybir.AxisListType


class Buf:
    __slots__ = ("t", "w", "r", "name")

    def __init__(self, t, name=""):
        self.t = t
        self.w = None
        self.r = []
        self.name = name

    def __getitem__(self, idx):
        return self.t[idx]


class TileList(list):
    full = None


def tile_views(full, n=None):
    tl = TileList(Buf(full.t[:, :, t * 128:(t + 1) * 128]) for t in range(n or (full.t.shape[2] // 128)))
    tl.full = full
    return tl


class Tracker:
    COMPUTE = ("pe", "dve", "act", "pool")
    NDMA = 6

    def __init__(self, nc, es, same_engine_sync=True):
        self.nc = nc
        self.es = es
        self.same_engine_sync = same_engine_sync
        self.eng = {"pe": nc.tensor, "dve": nc.vector, "act": nc.scalar, "pool": nc.gpsimd, "sp": nc.sync}
        self.sems = {}
        self.cnt = {}
        for k in self.COMPUTE:
            self.sems[k] = es.enter_context(nc.semaphore("s_" + k))
            self.cnt[k] = 0
        self.ring = {}
        self.ring_pos = {}
        for q in ("sp", "act", "pool"):
            keys = []
            for i in range(self.NDMA):
                key = "d_%s%d" % (q, i)
                self.sems[key] = es.enter_context(nc.semaphore(key))
                self.cnt[key] = 0
                keys.append(key)
            self.ring[q] = keys
            self.ring_pos[q] = 0
        self.waited = {e: {} for e in self.eng}
        self.ninstr = 0

    def _wait(self, e, tok):
        if tok is None:
            return
        key, val = tok
        if key == e and (e == "pe" or not self.same_engine_sync):
            return
        if self.waited[e].get(key, 0) >= val:
            return
        self.eng[e].wait_ge(self.sems[key], val)
        self.waited[e][key] = val

    def _deps(self, e, reads, writes):
        for b in reads:
            self._wait(e, b.w)
        for b in writes:
            self._wait(e, b.w)
            for tok in b.r:
                self._wait(e, tok)

    def _commit(self, tok, reads, writes):
        for b in writes:
            b.w = tok
            b.r = []
        for b in reads:
            if b not in writes:
                b.r.append(tok)
                if len(b.r) > 64:
                    best = {}
                    for k, v in b.r:
                        if best.get(k, 0) < v:
                            best[k] = v
                    b.r = list(best.items())

    def op(self, e, fn, reads=(), writes=()):
        self._deps(e, reads, writes)
        ins = fn(self.eng[e])
        self.cnt[e] += 1
        ins.then_inc(self.sems[e], 1)
        tok = (e, self.cnt[e])
        self._commit(tok, reads, writes)
        self.ninstr += 1
        return tok

    def mm(self, out_buf, fn, reads, first, last):
        e = "pe"
        if first:
            self._deps(e, reads, [out_buf])
        else:
            self._deps(e, reads, [])
        ins = fn(self.eng[e])
        self.ninstr += 1
        if last:
            self.cnt[e] += 1
            ins.then_inc(self.sems[e], 1)
            tok = (e, self.cnt[e])
            self._commit(tok, reads, [out_buf])
        else:
            tok = (e, self.cnt[e] + 1)
            for b in reads:
                b.r.append(tok)
        return None

    def dma(self, q, out_ap, in_ap, reads=(), writes=(), **kw):
        e = q
        ring = self.ring[q]
        key = ring[self.ring_pos[q] % len(ring)]
        self.ring_pos[q] += 1
        if self.cnt[key] > 0:
            self._wait(e, (key, self.cnt[key]))
        self._deps(e, reads, writes)
        ins = self.eng[e].dma_start(out=out_ap, in_=in_ap, **kw)
        self.cnt[key] += 16
        ins.then_inc(self.sems[key], 16)
        tok = (key, self.cnt[key])
        self._commit(tok, reads, writes)
        self.ninstr += 1
        return tok

    def barrier(self):
        toks = [(k, v) for k, v in self.cnt.items() if v > 0]
        for e in self.eng:
            for tok in toks:
                self._wait(e, tok)

    def finish(self, bufs):
        for b in bufs:
            self._wait("sp", b.w)

from concourse.bass_utils import run_bass_kernel_spmd
import ml_dtypes

NPBF16 = ml_dtypes.bfloat16
NCORES = 8
D = 1024
SEQ = 2048
CTX = 256
NTOK = SEQ + CTX
NE = 16
FF = 2816
NFC = FF // 128
EPS = 1e-6


def new_nc():
    return bass.Bass("TRN2", target_bir_lowering=False)


class Ctx:
    def __init__(self, nc, es):
        self.nc = nc
        self.es = es
        self.T = Tracker(nc, es)
        self.n = 0

    def sb(self, shape, dt, name=None):
        self.n += 1
        name = name or ("t%d" % self.n)
        return Buf(self.es.enter_context(self.nc.sbuf_tensor(name, list(shape), dt)), name)

    def ps(self, shape, dt=F32, name=None):
        self.n += 1
        name = name or ("p%d" % self.n)
        return Buf(self.es.enter_context(self.nc.psum_tensor(name, list(shape), dt)), name)

    over = None

    def din(self, name, shape, dt):
        if self.over is not None:
            b = self.over[name]
            assert list(b.t.shape) == list(shape) and b.t.dtype == dt, (name, b.t.shape, shape)
            return b
        return Buf(self.nc.dram_tensor(name, list(shape), dt, kind="ExternalInput").ap(), name)

    def dout(self, name, shape, dt):
        if self.over is not None:
            b = self.over[name]
            assert list(b.t.shape) == list(shape) and b.t.dtype == dt, (name, b.t.shape, shape)
            return b
        return Buf(self.nc.dram_tensor(name, list(shape), dt, kind="ExternalOutput").ap(), name)

    def scratch(self, name, shape, dt):
        return Buf(self.nc.dram_tensor(name, list(shape), dt, kind="Internal").ap(), name)


def body_ada_cols(C):
    nc = C.nc
    T = C.T
    ccT_d = C.din("ccT", [D, 9], F32)
    w_d = C.din("adaw", [2, D, 768], F32)
    b_d = C.din("adab", [2, 768], F32)
    out_d = C.dout("mods", [2, 9, 768], F32)
    ccT = C.sb([128, 8, 9], F32)
    scT = C.sb([128, 8, 9], F32)
    w = [C.sb([128, 8, 768], F32) for _ in range(2)]
    bias = C.sb([9, 2, 768], F32)
    res = C.sb([9, 2, 768], F32)
    T.dma("sp", ccT[:], ccT_d.t.rearrange("(k p) s -> p k s", p=128), writes=[ccT])
    for l in range(2):
        T.dma(["sp", "act"][l], w[l][:], w_d.t[l].rearrange("(k p) f -> p k f", p=128), writes=[w[l]])
        T.dma("pool", bias[:, l, :], b_d.t[l].partition_broadcast(9), writes=[bias])
    T.op("act", lambda e: e.activation(out=scT[:], in_=ccT[:], func=AF.Silu), reads=[ccT], writes=[scT])
    pss = [C.ps([9, 384]) for _ in range(4)]
    for l in range(2):
        for h in range(2):
            p = pss[l * 2 + h]
            for k in range(8):
                T.mm(p, lambda e: e.matmul(p[:], lhsT=scT[:, k, :], rhs=w[l][:, k, h * 384:(h + 1) * 384],
                                           start=(k == 0), stop=(k == 7)), [scT, w[l]], k == 0, k == 7)
            T.op("dve", lambda e: e.tensor_tensor(out=res[:, l, h * 384:(h + 1) * 384], in0=p[:],
                                                  in1=bias[:, l, h * 384:(h + 1) * 384], op=ALU.add),
                 reads=[p, bias], writes=[res])
    T.dma("sp", out_d.t.rearrange("l s f -> s l f"), res[:], reads=[res], writes=[out_d])
    T.finish([out_d])


def build_ada():
    nc = new_nc()
    with ExitStack() as es:
        C = Ctx(nc, es)
        body_ada_cols(C)
    return nc

def body_ffn_ep(C, NT):
    nc = C.nc
    HT = NT // 2
    blocks = []
    s = 0
    while s < HT:
        n = min(512, HT - s)
        blocks.append((s, n))
        s += n
    T = C.T
    xT_d = C.din("xT", [2, D, NT], BF16)
    wg_d = C.din("wg", [2, D, FF], F32)
    wu_d = C.din("wu", [2, D, FF], F32)
    wd_d = C.din("wd", [2, FF, D], F32)
    y_d = C.dout("y", [2, NT, D], F32)
    xT = C.sb([128, 8, NT], BF16)
    hid_t = C.sb([128, NFC, HT], BF16)
    hid = [Buf(hid_t.t[:, fc, :]) for fc in range(NFC)]
    wdb_t = C.sb([128, NFC, D], BF16)
    wdb = [Buf(wdb_t.t[:, 2 * g:2 * g + 2, :]) for g in range(NFC // 2)]
    gstg = [C.sb([128, 8, 256], F32) for _ in range(2)]
    ustg = [C.sb([128, 8, 256], F32) for _ in range(2)]
    dstg = [C.sb([128, 2, D], F32) for _ in range(2)]
    wgb = [C.sb([128, 8, 256], BF16) for _ in range(2)]
    wub = [C.sb([128, 8, 256], BF16) for _ in range(2)]
    sg = [C.sb([128, 512], F32) for _ in range(2)]
    ysb = [C.sb([128, D], F32) for _ in range(2)]
    pg = [C.ps([128, 512]) for _ in range(2)]
    pu = [C.ps([128, 512]) for _ in range(2)]
    py = [C.ps([128, 512]) for _ in range(2)]
    it = 0
    yi = 0
    for ex in range(2):
        T.dma("pool", xT[:], xT_d.t[ex].rearrange("(k p) t -> p k t", p=128), reads=[xT_d], writes=[xT])
        for half in range(2):
            t0 = half * HT
            for g in range(NFC // 2):
                b = it % 2
                it += 1
                T.dma("sp", gstg[b][:], wg_d.t[ex].rearrange("(k p) f -> p k f", p=128)[:, :, g * 256:(g + 1) * 256],
                      writes=[gstg[b]])
                T.dma("act", ustg[b][:], wu_d.t[ex].rearrange("(k p) f -> p k f", p=128)[:, :, g * 256:(g + 1) * 256],
                      writes=[ustg[b]])
                T.op("act", lambda e: e.copy(out=wgb[b][:], in_=gstg[b][:]), reads=[gstg[b]], writes=[wgb[b]])
                T.op("pool", lambda e: e.tensor_copy(out=wub[b][:], in_=ustg[b][:]), reads=[ustg[b]], writes=[wub[b]])
                if half == 0:
                    T.dma("sp", dstg[b][:], wd_d.t[ex].rearrange("(c p) d -> p c d", p=128)[:, 2 * g:2 * g + 2, :],
                          writes=[dstg[b]])
                    T.op("dve", lambda e: e.tensor_copy(out=wdb[g][:], in_=dstg[b][:]), reads=[dstg[b]], writes=[wdb[g]])
                for j in range(2):
                    fc = 2 * g + j
                    for (bs, bn) in blocks:
                        pb = (fc * len(blocks) + (bs // 512)) % 2
                        pgb, pub, sgb = pg[pb], pu[pb], sg[pb]
                        for k in range(8):
                            T.mm(pgb, lambda e: e.matmul(pgb[:, :bn], lhsT=wgb[b][:, k, j * 128:(j + 1) * 128],
                                                         rhs=xT[:, k, t0 + bs:t0 + bs + bn], start=(k == 0), stop=(k == 7)),
                                 [wgb[b], xT], k == 0, k == 7)
                        for k in range(8):
                            T.mm(pub, lambda e: e.matmul(pub[:, :bn], lhsT=wub[b][:, k, j * 128:(j + 1) * 128],
                                                         rhs=xT[:, k, t0 + bs:t0 + bs + bn], start=(k == 0), stop=(k == 7)),
                                 [wub[b], xT], k == 0, k == 7)
                        T.op("act", lambda e: e.activation(out=sgb[:, :bn], in_=pgb[:, :bn], func=AF.Silu),
                             reads=[pgb], writes=[sgb])
                        T.op("dve", lambda e: e.tensor_tensor(out=hid[fc][:, bs:bs + bn], in0=sgb[:, :bn], in1=pub[:, :bn],
                                                              op=ALU.mult), reads=[sgb, pub], writes=[hid[fc]])
            for tt in range(HT // 128):
                yb = ysb[yi % 2]
                yi += 1
                for dh in range(2):
                    p = py[dh]
                    for fc in range(NFC):
                        T.mm(p, lambda e: e.matmul(p[:], lhsT=hid[fc][:, tt * 128:(tt + 1) * 128],
                                                   rhs=wdb_t[:, fc, dh * 512:(dh + 1) * 512], start=(fc == 0), stop=(fc == NFC - 1)),
                             [hid[fc], wdb[fc // 2]], fc == 0, fc == NFC - 1)
                    if dh == 0:
                        T.op("act", lambda e: e.copy(out=yb[:, 0:512], in_=p[:]), reads=[p], writes=[yb])
                    else:
                        T.op("dve", lambda e: e.tensor_copy(out=yb[:, 512:1024], in_=p[:]), reads=[p], writes=[yb])
                T.dma("pool", y_d.t[ex, t0 + tt * 128:t0 + (tt + 1) * 128, :], yb[:], reads=[yb], writes=[y_d])
    T.finish([y_d])


def build_ffn(NT):
    nc = new_nc()
    with ExitStack() as es:
        C = Ctx(nc, es)
        body_ffn_ep(C, NT)
    return nc

import math

NT_TILES = NTOK // 128
TOKBLOCKS = [(0, 256)] + [(256 + i * 512, 512) for i in range(4)]


def make_consts(C):
    T = C.T
    k = {}
    dji = C.sb([128, 128], F32)
    T.op("pool", lambda e: e.iota(dji[:], pattern=[[1, 128]], base=0, channel_multiplier=-1,
                                  allow_small_or_imprecise_dtypes=True), writes=[dji])
    k["dji"] = dji
    identf = C.sb([128, 128], F32)
    identb = C.sb([128, 128], BF16)
    utri = C.sb([128, 128], BF16)
    onesb = C.sb([128, 128], BF16)
    T.op("dve", lambda e: e.tensor_single_scalar(out=identf[:], in_=dji[:], scalar=0.0, op=ALU.is_equal), reads=[dji], writes=[identf])
    T.op("dve", lambda e: e.tensor_single_scalar(out=identb[:], in_=dji[:], scalar=0.0, op=ALU.is_equal), reads=[dji], writes=[identb])
    T.op("dve", lambda e: e.tensor_single_scalar(out=utri[:], in_=dji[:], scalar=0.0, op=ALU.is_gt), reads=[dji], writes=[utri])
    T.op("dve", lambda e: e.memset(onesb[:], 1.0), writes=[onesb])
    k.update(identf=identf, identb=identb, utri=utri, onesb=onesb)
    return k


def make_swap(C, k, R):
    T = C.T
    a = C.sb([128, 128], F32)
    b = C.sb([128, 128], F32)
    P = C.sb([128, 128], BF16)
    dji = k["dji"]
    T.op("dve", lambda e: e.tensor_single_scalar(out=a[:R, :R], in_=dji[:R, :R], scalar=float(R // 2), op=ALU.is_equal), reads=[dji], writes=[a])
    T.op("dve", lambda e: e.tensor_single_scalar(out=b[:R, :R], in_=dji[:R, :R], scalar=float(-(R // 2)), op=ALU.is_equal), reads=[dji], writes=[b])
    T.op("dve", lambda e: e.tensor_tensor(out=P[:R, :R], in0=a[:R, :R], in1=b[:R, :R], op=ALU.add), reads=[a, b], writes=[P])
    return P


def make_rope(C, R, n):
    T = C.T
    ln = int(math.log2(n))
    outs_pre = [C.sb([128, 2048], F32), C.sb([128, 2048], F32)]
    return _make_rope_inner(C, R, n, ln, outs_pre)


def _make_rope_inner(C, R, n, ln, outs_pre):
  T = C.T
  with scope(C):
    pi = C.sb([128, 1], I32)
    T.op("pool", lambda e: e.iota(pi[:R, :], pattern=[[0, 1]], base=0, channel_multiplier=1), writes=[pi])

    def ibit(shift, mask):
        t = C.sb([128, 1], I32)
        o = C.sb([128, 1], F32)
        T.op("dve", lambda e: e.tensor_scalar(out=t[:R, :], in0=pi[:R, :], scalar1=shift, scalar2=mask,
                                              op0=ALU.logical_shift_right, op1=ALU.bitwise_and), reads=[pi], writes=[t])
        T.op("dve", lambda e: e.tensor_copy(out=o[:R, :], in_=t[:R, :]), reads=[t], writes=[o])
        return o
    f = ibit(0, n - 1)
    axis = ibit(ln, 1)
    half = ibit(ln + 1, 1)
    inv = C.sb([128, 1], F32)
    T.op("act", lambda e: e.activation(out=inv[:R, :], in_=f[:R, :], func=AF.Exp, scale=-math.log(10000.0) / n), reads=[f], writes=[inv])
    A = C.sb([128, 1], F32)
    B = C.sb([128, 1], F32)
    sgn = C.sb([128, 1], F32)
    T.op("dve", lambda e: e.tensor_tensor(out=B[:R, :], in0=inv[:R, :], in1=axis[:R, :], op=ALU.mult), reads=[inv, axis], writes=[B])
    T.op("dve", lambda e: e.tensor_tensor(out=A[:R, :], in0=inv[:R, :], in1=B[:R, :], op=ALU.subtract), reads=[inv, B], writes=[A])
    T.op("dve", lambda e: e.tensor_scalar(out=sgn[:R, :], in0=half[:R, :], scalar1=2.0, scalar2=-1.0, op0=ALU.mult, op1=ALU.add), reads=[half], writes=[sgn])
    rowp = C.sb([128, 32, 64], F32)
    colp = C.sb([128, 32, 64], F32)
    T.op("pool", lambda e: e.iota(rowp[:R], pattern=[[1, 32], [0, 64]], base=0, channel_multiplier=0, allow_small_or_imprecise_dtypes=True), writes=[rowp])
    T.op("pool", lambda e: e.iota(colp[:R], pattern=[[0, 32], [1, 64]], base=0, channel_multiplier=0, allow_small_or_imprecise_dtypes=True), writes=[colp])
    ang = C.sb([128, 2048], F32)
    rp = rowp.t.rearrange("p a b -> p (a b)")
    cp = colp.t.rearrange("p a b -> p (a b)")
    T.op("dve", lambda e: e.tensor_scalar(out=cp[:R], in0=cp[:R], scalar1=B[:R, 0:1], scalar2=None, op0=ALU.mult), reads=[colp, B], writes=[colp])
    T.op("dve", lambda e: e.scalar_tensor_tensor(out=ang[:R], in0=rp[:R], scalar=A[:R, 0:1], in1=cp[:R], op0=ALU.mult, op1=ALU.add),
         reads=[rowp, colp, A], writes=[ang])
    outs = []
    ki = C.sb([128, 2048], I32)
    kf = C.sb([128, 2048], F32)
    m = C.sb([128, 2048], F32)
    for si, shift in enumerate((math.pi / 2, 0.0)):
        r1 = outs_pre[si]
        T.op("dve", lambda e: e.tensor_scalar(out=kf[:R], in0=ang[:R], scalar1=shift, scalar2=1.0 / (2 * math.pi), op0=ALU.add, op1=ALU.mult),
             reads=[ang], writes=[kf])
        T.op("dve", lambda e: e.tensor_copy(out=ki[:R], in_=kf[:R]), reads=[kf], writes=[ki])
        T.op("dve", lambda e: e.tensor_copy(out=kf[:R], in_=ki[:R]), reads=[ki], writes=[kf])
        T.op("dve", lambda e: e.scalar_tensor_tensor(out=r1[:R], in0=kf[:R], scalar=-2 * math.pi, in1=ang[:R], op0=ALU.mult, op1=ALU.add),
             reads=[kf, ang], writes=[r1])
        if shift != 0.0:
            T.op("dve", lambda e: e.tensor_scalar(out=r1[:R], in0=r1[:R], scalar1=shift, scalar2=None, op0=ALU.add), reads=[r1], writes=[r1])
        T.op("dve", lambda e: e.tensor_single_scalar(out=m[:R], in_=r1[:R], scalar=math.pi, op=ALU.is_gt), reads=[r1], writes=[m])
        T.op("dve", lambda e: e.scalar_tensor_tensor(out=r1[:R], in0=m[:R], scalar=-2 * math.pi, in1=r1[:R], op0=ALU.mult, op1=ALU.add),
             reads=[m, r1], writes=[r1])
        T.op("dve", lambda e: e.tensor_single_scalar(out=m[:R], in_=r1[:R], scalar=-math.pi, op=ALU.is_lt), reads=[r1], writes=[m])
        T.op("dve", lambda e: e.scalar_tensor_tensor(out=r1[:R], in0=m[:R], scalar=2 * math.pi, in1=r1[:R], op0=ALU.mult, op1=ALU.add),
             reads=[m, r1], writes=[r1])
        T.op("dve", lambda e: e.tensor_scalar(out=r1[:R], in0=r1[:R], scalar1=math.pi, scalar2=-math.pi, op0=ALU.min, op1=ALU.max), reads=[r1], writes=[r1])
        T.op("act", lambda e: e.activation(out=r1[:R], in_=r1[:R], func=AF.Sin), reads=[r1], writes=[r1])
        outs.append(r1)
    cos, sin = outs
    T.op("dve", lambda e: e.tensor_scalar(out=sin[:R], in0=sin[:R], scalar1=sgn[:R, 0:1], scalar2=None, op0=ALU.mult), reads=[sin, sgn], writes=[sin])
  return cos, sin


def rstd_from_ss(C, ss, n, P=128, W=1):
    T = C.T
    v = C.sb([128, W], F32)
    T.op("dve", lambda e: e.tensor_scalar(out=v[:P], in0=ss[:P, :W], scalar1=1.0 / n, scalar2=EPS, op0=ALU.mult, op1=ALU.add), reads=[ss], writes=[v])
    T.op("act", lambda e: e.activation(out=v[:P], in_=v[:P], func=AF.Sqrt), reads=[v], writes=[v])
    T.op("dve", lambda e: e.reciprocal(out=v[:P], in_=v[:P]), reads=[v], writes=[v])
    return v


def bcast_load(C, q, dram_ap_1d, n, name=None):
    t = C.sb([128, n], F32, name)
    C.T.dma(q, t[:], dram_ap_1d.partition_broadcast(128), writes=[t])
    return t

from contextlib import contextmanager


@contextmanager
def scope(C):
    old = C.es
    with ExitStack() as es2:
        C.es = es2
        try:
            yield
        finally:
            C.T.barrier()
            C.es = old


def evac(C, i, out_ap, in_ap, reads, writes):
    if i % 2 == 0:
        C.T.op("act", lambda e: e.copy(out=out_ap, in_=in_ap), reads=reads, writes=writes)
    else:
        C.T.op("dve", lambda e: e.tensor_copy(out=out_ap, in_=in_ap), reads=reads, writes=writes)


def norm_mod_transpose(C, k, x_tiles, g_d, shift_l, scale_l, shift_c, scale_c, hT, ps_bf, h_tok=None, tile_cb=None, tiles=None):
    T = C.T
    with scope(C):
        G = bcast_load(C, "sp", g_d, D)
        A = {}
        B = {}
        for nm, sh, sc, q in (("l", shift_l, scale_l, "act"), ("c", shift_c, scale_c, "pool")):
            S = bcast_load(C, q, sc, D)
            A[nm] = C.sb([128, D], F32)
            T.op("dve", lambda e: e.scalar_tensor_tensor(out=A[nm][:], in0=S[:], scalar=1.0, in1=G[:], op0=ALU.add, op1=ALU.mult),
                 reads=[S, G], writes=[A[nm]])
            B[nm] = bcast_load(C, q, sh, D)
        NBF = 3
        xt = [C.sb([128, D], F32) for _ in range(NBF)]
        junk = C.sb([128, D], BF16)
        hb = [C.sb([128, D], BF16) for _ in range(NBF)]
        hf = [C.sb([128, D], F32) for _ in range(NBF)]
        ss = [C.sb([128, 1], F32) for _ in range(NBF)]

        def stage1(i, t):
            nm = "c" if t < 2 else "l"
            x = xt[i % NBF]
            T.dma(["sp", "act"][i % 2], x[:], x_tiles[t].t, reads=[x_tiles[t]], writes=[x])
            s = ss[i % NBF]
            T.op("act", lambda e: e.activation(out=junk[:], in_=x[:], func=AF.Square, accum_out=s[:, 0:1]), reads=[x], writes=[junk, s])
            r = rstd_from_ss(C, s, D)
            h32 = hf[i % NBF]
            T.op("dve", lambda e: e.scalar_tensor_tensor(out=h32[:], in0=x[:], scalar=r[:, 0:1], in1=A[nm][:], op0=ALU.mult, op1=ALU.mult),
                 reads=[x, r, A[nm]], writes=[h32])
            hcur = h_tok[t] if h_tok is not None else hb[i % NBF]
            T.op("dve", lambda e: e.tensor_tensor(out=hcur[:], in0=h32[:], in1=B[nm][:], op=ALU.add), reads=[h32, B[nm]], writes=[hcur])
            if tile_cb is not None:
                T.op("dve", lambda e: e.tensor_tensor(out=h32[:], in0=h32[:], in1=B[nm][:], op=ALU.add), reads=[h32, B[nm]], writes=[h32])
            return (i, t, h32, hcur)

        def stage2(st):
            i, t, h32, hcur = st
            if tile_cb is not None:
                tile_cb(t, h32)
            if hT is not None:
                p = ps_bf[i % 2]
                pv = p.t[:].bitcast(BF16)
                for c in range(8):
                    T.op("pe", lambda e: e.transpose(pv[:, c * 128:(c + 1) * 128], hcur[:, c * 128:(c + 1) * 128], k["identb"][:]),
                         reads=[hcur, k["identb"]], writes=[p])
                evac(C, i, hT[t].t, pv.rearrange("p (c n) -> p c n", c=8), [p], [hT[t]])

        prev = None
        for i, t in enumerate(tiles if tiles is not None else range(NT_TILES)):
            cur = stage1(i, t)
            if prev is not None:
                stage2(prev)
            prev = cur
        stage2(prev)


def proj_fm(C, k, dst, src, nk, w, col0, R, ps, rope=None, scale=None, tmp=None):
    T = C.T

    def mm_block(bi):
        t0, n = TOKBLOCKS[bi]
        p = ps[bi % 2]
        ntile = n // 128
        tiles_ = list(src[t0 // 128:t0 // 128 + ntile])
        for kk in range(nk):
            T.mm(p, lambda e: e.matmul(p[:R, :n], lhsT=w[:, kk, col0:col0 + R], rhs=src.full[:, kk, t0:t0 + n],
                                       start=(kk == 0), stop=(kk == nk - 1)), [w] + tiles_, kk == 0, kk == nk - 1)

    def post_block(bi):
        t0, n = TOKBLOCKS[bi]
        p = ps[bi % 2]
        if rope is not None and bi > 0:
            cos, sin, Pm = rope
            raw, t1, p2 = tmp[0][bi % 2], tmp[1][bi % 2], tmp[2][bi % 2]
            pos0 = t0 - 256
            T.op("act", lambda e: e.copy(out=raw[:R, :n], in_=p[:R, :n]), reads=[p], writes=[raw])
            T.mm(p2, lambda e: e.matmul(p2[:R, :n], lhsT=Pm[:R, :R], rhs=raw[:R, :n], start=True, stop=True), [Pm, raw], True, True)
            T.op("dve", lambda e: e.tensor_tensor(out=t1[:R, :n], in0=raw[:R, :n], in1=cos[:R, pos0:pos0 + n], op=ALU.mult), reads=[raw, cos], writes=[t1])
            T.op("dve", lambda e: e.tensor_tensor(out=raw[:R, :n], in0=p2[:R, :n], in1=sin[:R, pos0:pos0 + n], op=ALU.mult), reads=[p2, sin, raw], writes=[raw])
            T.op("dve", lambda e: e.tensor_tensor(out=dst[:R, t0:t0 + n], in0=t1[:R, :n], in1=raw[:R, :n], op=ALU.add), reads=[t1, raw], writes=[dst])
        else:
            evac(C, bi, dst[:R, t0:t0 + n], p[:R, :n], [p], [dst])

    nb = len(TOKBLOCKS)
    mm_block(0)
    for bi in range(nb):
        if bi + 1 < nb:
            mm_block(bi + 1)
        post_block(bi)


def proj_tm(C, vaug, src, nk, w, col0, W, ps):
    T = C.T
    for t in range(NT_TILES):
        p = ps[t % 2]
        for kk in range(nk):
            T.mm(p, lambda e: e.matmul(p[:, :W], lhsT=src[t][:, kk, :], rhs=w[:, kk, col0:col0 + W], start=(kk == 0), stop=(kk == nk - 1)),
                 [src[t], w], kk == 0, kk == nk - 1)
        evac(C, t, vaug[:, t, 0:W], p[:, :W], [p], [vaug])


def attend(C, pairs, vaug, W, scale, on, psS, psO, pts):
    T = C.T
    rec = C.sb([128, 1], F32)
    cnt = 0
    for (q0, nq, ktiles) in [(0, 2, [0, 1])] + [(256 + i * 512, 4, list(range(18))) for i in range(4)]:
        n = nq * 128

        def issueS(ii):
            kt = ktiles[ii]
            p = psS[ii % 2]
            for pi_, (kT, qT, R) in enumerate(pairs):
                T.mm(p, lambda e: e.matmul(p[:, :n], lhsT=kT[:R, kt * 128:(kt + 1) * 128], rhs=qT[:R, q0:q0 + n],
                                           start=(pi_ == 0), stop=(pi_ == len(pairs) - 1)), [kT, qT], pi_ == 0, pi_ == len(pairs) - 1)
        issueS(0)
        for ii, kt in enumerate(ktiles):
            p = psS[ii % 2]
            pt = pts[cnt % len(pts)]
            cnt += 1
            T.op("act", lambda e: e.activation(out=pt[:, :n], in_=p[:, :n], func=AF.Exp, scale=scale), reads=[p], writes=[pt])
            if ii + 1 < len(ktiles):
                issueS(ii + 1)
            for qs in range(nq):
                po = psO[qs]
                T.mm(po, lambda e: e.matmul(po[:, :W + 1], lhsT=pt[:, qs * 128:(qs + 1) * 128], rhs=vaug[:, kt, :],
                                            start=(ii == 0), stop=(ii == len(ktiles) - 1)), [pt, vaug], ii == 0, ii == len(ktiles) - 1)
        for qs in range(nq):
            po = psO[qs]
            qt = q0 // 128 + qs
            T.op("dve", lambda e: e.reciprocal(out=rec[:], in_=po[:, W:W + 1]), reads=[po], writes=[rec])
            T.op("dve", lambda e: e.tensor_scalar(out=on[:, qt, :], in0=po[:, 0:W], scalar1=rec[:, 0:1], scalar2=None, op0=ALU.mult),
                 reads=[po, rec], writes=[on])


def route_and_gather(C, k, h_tok, logits, xeT_d, meta_d, ps, with_ctx):
    T = C.T
    NS = 288 if with_ctx else 256
    t_first = 0 if with_ctx else 2
    tiles = list(range(t_first, NT_TILES))
    identf = k["identf"]
    with scope(C):
        mx = C.sb([128, 18], F32)
        ez = C.sb([128, 18, 16], F32)
        aff = C.sb([128, 18, 16], F32)
        sm = C.sb([128, 18], F32)
        T.op("dve", lambda e: e.tensor_reduce(out=mx[:], in_=logits[:], axis=AX.X, op=ALU.max), reads=[logits], writes=[mx])
        for t in range(NT_TILES):
            T.op("dve", lambda e: e.tensor_scalar(out=ez[:, t, :], in0=logits[:, t, :], scalar1=mx[:, t:t + 1], scalar2=None, op0=ALU.subtract),
                 reads=[logits, mx], writes=[ez])
        T.op("act", lambda e: e.activation(out=ez[:], in_=ez[:], func=AF.Exp), reads=[ez], writes=[ez])
        T.op("dve", lambda e: e.tensor_reduce(out=sm[:], in_=ez[:], axis=AX.X, op=ALU.add), reads=[ez], writes=[sm])
        T.op("dve", lambda e: e.reciprocal(out=sm[:], in_=sm[:]), reads=[sm], writes=[sm])
        for t in range(NT_TILES):
            T.op("dve", lambda e: e.tensor_scalar(out=aff[:, t, :], in0=ez[:, t, :], scalar1=sm[:, t:t + 1], scalar2=None, op0=ALU.mult),
                 reads=[ez, sm], writes=[aff])
        affT = C.sb([16, NTOK], F32)
        for t in range(NT_TILES):
            p = ps[t % 2]
            T.op("pe", lambda e: e.transpose(p[:16, :128], aff[:, t, :], identf[:]), reads=[aff, identf], writes=[p])
            evac(C, t, affT[:, t * 128:(t + 1) * 128], p[:16, :128], [p], [affT])
        maskT = C.sb([16, NTOK], F32)
        m8 = C.sb([16, 8], F32)

        def thresh(lo, n, cap):
            wk = C.sb([16, n], F32)
            T.op("dve", lambda e: e.tensor_copy(out=wk[:], in_=affT[:, lo:lo + n]), reads=[affT], writes=[wk])
            for r in range(cap // 8):
                T.op("dve", lambda e: e.max(out=m8[:], in_=wk[:]), reads=[wk], writes=[m8])
                if r < cap // 8 - 1:
                    T.op("dve", lambda e: e.match_replace(out=wk[:], in_to_replace=m8[:], in_values=wk[:], imm_value=-1.0), reads=[wk, m8], writes=[wk])
            T.op("dve", lambda e: e.tensor_scalar(out=maskT[:, lo:lo + n], in0=affT[:, lo:lo + n], scalar1=m8[:, 7:8], scalar2=None, op0=ALU.is_ge),
                 reads=[affT, m8], writes=[maskT])
        thresh(256, SEQ, 2 * SEQ // NE)
        if with_ctx:
            thresh(0, CTX, 2 * CTX // NE)
        else:
            T.op("dve", lambda e: e.memset(maskT[:, 0:256], 0.0), writes=[maskT])
        mask = C.sb([128, 18, 16], F32)
        maskb = C.sb([128, 18, 16], BF16)
        for t in range(NT_TILES):
            p = ps[t % 2]
            T.op("pe", lambda e: e.transpose(p[:, :16], maskT[:, t * 128:(t + 1) * 128], identf[:16, :16]), reads=[maskT, identf], writes=[p])
            evac(C, t, mask[:, t, :], p[:, :16], [p], [mask])
        T.op("dve", lambda e: e.tensor_copy(out=maskb[:], in_=mask[:]), reads=[mask], writes=[maskb])
        pre_p, tot_p = ps[0], ps[1]
        mb2 = maskb.t.rearrange("p t e -> p (t e)")
        T.mm(pre_p, lambda e: e.matmul(pre_p[:, :288], lhsT=k["utri"][:], rhs=mb2, start=True, stop=True), [k["utri"], maskb], True, True)
        T.mm(tot_p, lambda e: e.matmul(tot_p[:, :288], lhsT=k["onesb"][:], rhs=mb2, start=True, stop=True), [k["onesb"], maskb], True, True)
        tot = C.sb([128, 18, 16], F32)
        off = C.sb([128, 18, 16], F32)
        slot = C.sb([128, 18, 16], F32)
        T.op("act", lambda e: e.copy(out=tot.t.rearrange("p t e -> p (t e)"), in_=tot_p[:, :288]), reads=[tot_p], writes=[tot])
        T.op("dve", lambda e: e.memset(off[:], 0.0), writes=[off])
        T.op("dve", lambda e: e.memset(off[:, 0:2, :], 256.0), reads=[off], writes=[off])
        T.op("dve", lambda e: e.tensor_tensor(out=off[:, 1, :], in0=off[:, 0, :], in1=tot[:, 0, :], op=ALU.add), reads=[off, tot], writes=[off])
        for t in range(3, NT_TILES):
            T.op("dve", lambda e: e.tensor_tensor(out=off[:, t, :], in0=off[:, t - 1, :], in1=tot[:, t - 1, :], op=ALU.add), reads=[off, tot], writes=[off])
        T.op("dve", lambda e: e.tensor_tensor(out=slot.t.rearrange("p t e -> p (t e)"), in0=pre_p[:, :288],
                                              in1=off.t.rearrange("p t e -> p (t e)"), op=ALU.add), reads=[pre_p, off], writes=[slot])
        T.op("dve", lambda e: e.scalar_tensor_tensor(out=slot.t.rearrange("p t e -> p (t e)"), in0=slot.t.rearrange("p t e -> p (t e)"), scalar=1.0,
                                                     in1=mask.t.rearrange("p t e -> p (t e)"), op0=ALU.add, op1=ALU.mult), reads=[slot, mask], writes=[slot])
        T.op("dve", lambda e: e.tensor_scalar(out=slot[:], in0=slot[:], scalar1=-1.0, scalar2=None, op0=ALU.add), reads=[slot], writes=[slot])
        vals = C.sb([128, 18, 16, 4], BF16)
        tcol = C.sb([128, 18, 16], F32)
        pcol = C.sb([128, 18, 16], F32)
        T.op("pool", lambda e: e.iota(tcol[:], pattern=[[1, 18], [0, 16]], base=0, channel_multiplier=0, allow_small_or_imprecise_dtypes=True), writes=[tcol])
        T.op("pool", lambda e: e.iota(pcol[:], pattern=[[0, 18], [0, 16]], base=0, channel_multiplier=1, allow_small_or_imprecise_dtypes=True), writes=[pcol])
        ahi = C.sb([128, 18, 16], BF16)
        alo = C.sb([128, 18, 16], F32)
        T.op("dve", lambda e: e.tensor_copy(out=ahi[:], in_=aff[:]), reads=[aff], writes=[ahi])
        T.op("dve", lambda e: e.tensor_tensor(out=alo[:], in0=aff[:], in1=ahi[:], op=ALU.subtract), reads=[aff, ahi], writes=[alo])
        for ci, src in enumerate((tcol, pcol, ahi, alo)):
            T.op("dve", lambda e: e.tensor_copy(out=vals[:, :, :, ci], in_=src[:]), reads=[src], writes=[vals])
        iota_s = C.sb([128, 288], U16)
        T.op("pool", lambda e: e.iota(iota_s[:], pattern=[[1, 288]], base=0, channel_multiplier=0, allow_small_or_imprecise_dtypes=True), writes=[iota_s])
        ohl = [C.sb([128, 16, 256], BF16) for _ in range(2)]
        ohc = [C.sb([128, 2, 32], BF16) for _ in range(2)]
        xes = [C.sb([128, 8, NS], BF16) for _ in range(2)]
        meta_sb = C.sb([4, 16, NS], F32)
        pm = ps[2]
        pgs = [ps[3], ps[4]]
        ei = 0

        def build_oh(ex):
            ol, oc = ohl[ex % 2], ohc[ex % 2]
            for t in tiles:
                if t >= 2:
                    T.op("dve", lambda e: e.tensor_scalar(out=ol[:, t - 2, :], in0=iota_s[:, 0:256], scalar1=slot[:, t, ex:ex + 1], scalar2=None, op0=ALU.is_equal),
                         reads=[iota_s, slot], writes=[ol])
                else:
                    T.op("dve", lambda e: e.tensor_scalar(out=oc[:, t, :], in0=iota_s[:, 256:288], scalar1=slot[:, t, ex:ex + 1], scalar2=None, op0=ALU.is_equal),
                         reads=[iota_s, slot], writes=[oc])
        build_oh(0)
        for ex in range(NE):
            ol, oc, xe = ohl[ex % 2], ohc[ex % 2], xes[ex % 2]
            if ex + 1 < NE:
                build_oh(ex + 1)
            for t in range(2, NT_TILES):
                T.mm(pm, lambda e: e.matmul(pm[:4, 0:256], lhsT=vals[:, t, ex, :], rhs=ol[:, t - 2, :], start=(t == 2), stop=(t == NT_TILES - 1)),
                     [vals, ol], t == 2, t == NT_TILES - 1)
            if with_ctx:
                for t in range(2):
                    T.mm(pm, lambda e: e.matmul(pm[:4, 256:288], lhsT=vals[:, t, ex, :], rhs=oc[:, t, :], start=(t == 0), stop=(t == 1)),
                         [vals, oc], t == 0, t == 1)
            T.op("act", lambda e: e.copy(out=meta_sb[:, ex, :], in_=pm[:4, :NS]), reads=[pm], writes=[meta_sb])
            for c in range(8):
                pg = pgs[ei % 2]
                ei += 1
                for t in range(2, NT_TILES):
                    T.mm(pg, lambda e: e.matmul(pg[:, 0:256], lhsT=h_tok[t][:, c * 128:(c + 1) * 128], rhs=ol[:, t - 2, :], start=(t == 2), stop=(t == NT_TILES - 1)),
                         [h_tok[t], ol], t == 2, t == NT_TILES - 1)
                if with_ctx:
                    for t in range(2):
                        T.mm(pg, lambda e: e.matmul(pg[:, 256:288], lhsT=h_tok[t][:, c * 128:(c + 1) * 128], rhs=oc[:, t, :], start=(t == 0), stop=(t == 1)),
                             [h_tok[t], oc], t == 0, t == 1)
                evac(C, c, xe[:, c, :], pg[:, :NS], [pg], [xe])
            T.dma(["sp", "act"][ex % 2], xeT_d.t[ex].rearrange("(c p) s -> p c s", p=128), xe[:], reads=[xe], writes=[xeT_d])
        T.dma("sp", meta_d.t, meta_sb[:], reads=[meta_sb], writes=[meta_d])


DA_SCALE = 64 ** -0.5
MLA_SCALE = 96 ** -0.5


def load_cast(C, q, dram_ap, shape, stage=None):
    out = C.sb(shape, BF16)
    C.T.dma("pool", out[:], dram_ap, writes=[out])
    return out


def router_cb_factory(C, k, router_sb, logits, psT, psL):
    T = C.T
    hfT = [C.sb([128, 8, 128], F32) for _ in range(2)]

    def cb(t, h32):
        hT_ = hfT[t % 2]
        for half in range(2):
            p = psT[half]
            for c in range(4):
                cc = half * 4 + c
                T.op("pe", lambda e: e.transpose(p[:, c * 128:(c + 1) * 128], h32[:, cc * 128:(cc + 1) * 128], k["identf"][:]),
                     reads=[h32, k["identf"]], writes=[p])
            evac(C, half, hT_[:, half * 4:(half + 1) * 4, :], p.t[:].rearrange("p (c n) -> p c n", c=4), [p], [hT_])
        for c in range(8):
            T.mm(psL, lambda e: e.matmul(psL[:, :16], lhsT=hT_[:, c, :], rhs=router_sb[:, c, :], start=(c == 0), stop=(c == 7)),
                 [hT_, router_sb], c == 0, c == 7)
        T.op("act", lambda e: e.copy(out=logits[:, t, :], in_=psL[:, :16]), reads=[psL], writes=[logits])
    return cb


def body_attn(C):
    nc = C.nc
    LAM_INIT = 0.8 - 0.6 * math.exp(-0.3 * 0)
    T = C.T
    x_d = C.din("xin", [NTOK, D], F32)
    mod_d = C.din("mod", [2, 6 * D], F32)
    n1g_d = C.din("n1g", [D], F32)
    n2g_d = C.din("n2g", [D], F32)
    win_d = C.din("w_in", [D, 2208], F32)
    qng_d = C.din("qng", [384], F32)
    qup_d = C.din("q_up", [384, 768], F32)
    kvng_d = C.din("kvng", [256], F32)
    kvup_d = C.din("kv_up", [256, 1024], F32)
    lam_d = C.din("lamv", [4, 64], F32)
    subg_d = C.din("subg", [128], F32)
    wout_d = C.din("w_out", [D, D], F32)
    rt_d = C.din("router", [D, NE], F32)
    xmid_d = C.dout("xmid", [NTOK, D], F32)
    xeT_d = C.dout("xeT", [NE, D, 288], BF16)
    meta_d = C.dout("meta", [4, NE, 288], F32)
    attn_d = Buf(nc.dram_tensor("attn_scr", [NTOK, D], BF16, kind="Internal").ap(), "attn_scr")
    x_tiles = [Buf(x_d.t[t * 128:(t + 1) * 128, :]) for t in range(NT_TILES)]
    xmid_tiles = [Buf(xmid_d.t[t * 128:(t + 1) * 128, :]) for t in range(NT_TILES)]
    attn_cols = {}
    ps = [C.ps([128, 512]) for _ in range(8)]
    k = make_consts(C)
    win_v = win_d.t.rearrange("(k p) f -> p k f", p=128)

    def modv(row, i):
        return mod_d.t[row, i * D:(i + 1) * D]

    with scope(C):
        hT_t = C.sb([128, 8, NTOK], BF16)
        hT = tile_views(hT_t)
        norm_mod_transpose(C, k, x_tiles, n1g_d.t, modv(0, 0), modv(0, 1), modv(1, 0), modv(1, 1), hT, ps[0:2])
        with scope(C):
            cosD, sinD = make_rope(C, 64, 16)
            P64 = make_swap(C, k, 64)
            lamb = C.sb([128, 4, 64], F32)
            T.dma("sp", lamb.t.rearrange("p a b -> p (a b)"), lam_d.t.rearrange("a b -> (a b)").partition_broadcast(128), writes=[lamb])
            lp = C.sb([128, 2, 64], F32)
            ls = C.sb([128, 2], F32)
            neglam = C.sb([128, 1], F32)
            T.op("dve", lambda e: e.tensor_tensor(out=lp[:, 0, :], in0=lamb[:, 0, :], in1=lamb[:, 1, :], op=ALU.mult), reads=[lamb], writes=[lp])
            T.op("dve", lambda e: e.tensor_tensor(out=lp[:, 1, :], in0=lamb[:, 2, :], in1=lamb[:, 3, :], op=ALU.mult), reads=[lamb, lp], writes=[lp])
            T.op("dve", lambda e: e.tensor_reduce(out=ls[:], in_=lp[:], axis=AX.X, op=ALU.add), reads=[lp], writes=[ls])
            T.op("act", lambda e: e.activation(out=ls[:], in_=ls[:], func=AF.Exp), reads=[ls], writes=[ls])
            T.op("dve", lambda e: e.tensor_scalar(out=neglam[:], in0=ls[:, 1:2], scalar1=-LAM_INIT, scalar2=None, op0=ALU.add), reads=[ls], writes=[neglam])
            T.op("dve", lambda e: e.tensor_tensor(out=neglam[:], in0=neglam[:], in1=ls[:, 0:1], op=ALU.subtract), reads=[neglam, ls], writes=[neglam])
            Gs = bcast_load(C, "act", subg_d.t, 128)
            T.op("dve", lambda e: e.tensor_scalar(out=Gs[:], in0=Gs[:], scalar1=1.0 - LAM_INIT, scalar2=None, op0=ALU.mult), reads=[Gs], writes=[Gs])
            wst = C.sb([128, 8, 384], F32)
            wda = C.sb([128, 8, 384], BF16)
            qk = [C.sb([128, NTOK], BF16) for _ in range(4)]
            for u in range(4):
                T.op("dve", lambda e: e.memset(qk[u][64:128, :], 0.0), writes=[qk[u]])
            vaug = C.sb([128, 18, 129], BF16)
            T.op("dve", lambda e: e.memset(vaug[:, :, 128:129], 1.0), writes=[vaug])
            raw = [C.sb([64, 512], BF16) for _ in range(2)]
            t1 = [C.sb([64, 512], F32) for _ in range(2)]
            o1n = C.sb([128, 18, 128], F32)
            o2n = C.sb([128, 18, 128], F32)
            sq = C.sb([128, 18, 128], F32)
            ssq = C.sb([128, 18], F32)
            ob = C.sb([128, 18, 128], BF16)
            pts = [C.sb([128, 512], BF16) for _ in range(3)]
            for h in range(4):
                for j, c0 in enumerate((h * 128, 512 + h * 128, 1024 + h * 128)):
                    T.dma(["sp", "act", "pool"][j], wst[:, :, j * 128:(j + 1) * 128], win_v[:, :, c0:c0 + 128], writes=[wst])
                T.op("dve", lambda e: e.tensor_copy(out=wda[:], in_=wst[:]), reads=[wst], writes=[wda])
                for u in range(4):
                    proj_fm(C, k, qk[u], hT, 8, wda, (u // 2) * 128 + (u % 2) * 64, 64, ps[0:2], rope=(cosD, sinD, P64), tmp=(raw, t1, ps[2:4]))
                proj_tm(C, vaug, hT, 8, wda, 256, 128, ps[0:2])
                attend(C, [(qk[2], qk[0], 128)], vaug, 128, DA_SCALE, o1n, ps[0:2], ps[4:8], pts)
                attend(C, [(qk[3], qk[1], 128)], vaug, 128, DA_SCALE, o2n, ps[0:2], ps[4:8], pts)
                o1f = o1n.t.rearrange("p a b -> p (a b)")
                o2f = o2n.t.rearrange("p a b -> p (a b)")
                T.op("dve", lambda e: e.scalar_tensor_tensor(out=o1f, in0=o2f, scalar=neglam[:, 0:1], in1=o1f, op0=ALU.mult, op1=ALU.add),
                     reads=[o1n, o2n, neglam], writes=[o1n])
                T.op("act", lambda e: e.activation(out=sq[:], in_=o1n[:], func=AF.Square), reads=[o1n], writes=[sq])
                T.op("dve", lambda e: e.tensor_reduce(out=ssq[:], in_=sq[:], axis=AX.X, op=ALU.add), reads=[sq], writes=[ssq])
                r = rstd_from_ss(C, ssq, 128, W=18)
                for t in range(NT_TILES):
                    T.op("dve", lambda e: e.scalar_tensor_tensor(out=ob[:, t, :], in0=o1n[:, t, :], scalar=r[:, t:t + 1], in1=Gs[:], op0=ALU.mult, op1=ALU.mult),
                         reads=[o1n, r, Gs], writes=[ob])
                T.dma("sp", attn_d.t.rearrange("(t p) d -> p t d", p=128)[:, :, h * 128:(h + 1) * 128], ob[:], reads=[ob], writes=[attn_d])
        with scope(C):
            cosR, sinR = make_rope(C, 32, 8)
            P32 = make_swap(C, k, 32)
            wlat = load_cast(C, "sp", win_v[:, :, 1536:2208], [128, 8, 672])
            qupb = load_cast(C, "act", qup_d.t.rearrange("(c p) f -> p c f", p=128), [128, 3, 768])
            kvupb = load_cast(C, "sp", kvup_d.t.rearrange("(c p) f -> p c f", p=128), [128, 2, 1024])
            gq = C.sb([128, 3], F32)
            gkv = C.sb([128, 2], F32)
            T.dma("pool", gq[:], qng_d.t.rearrange("(c p) -> p c", p=128), writes=[gq], allow_slow_non_contiguous=True)
            T.dma("pool", gkv[:], kvng_d.t.rearrange("(c p) -> p c", p=128), writes=[gkv], allow_slow_non_contiguous=True)
            cqn_t = C.sb([128, 3, NTOK], BF16)
            ckvn_t = C.sb([128, 2, NTOK], BF16)
            cqn = tile_views(cqn_t)
            ckvn = tile_views(ckvn_t)
            kr = C.sb([128, NTOK], BF16)
            T.op("dve", lambda e: e.memset(kr[32:64, :], 0.0), writes=[kr])
            T.op("dve", lambda e: e.memset(kr[64:128, :], 0.0), writes=[kr])
            raw = [C.sb([64, 512], BF16) for _ in range(2)]
            t1 = [C.sb([64, 512], F32) for _ in range(2)]
            with scope(C):
                rawf = [C.sb([128, 512], F32) for _ in range(3)]
                sqb = [C.sb([128, 512], BF16) for _ in range(3)]
                rs = C.sb([128, 512], F32)
                for (nchunk, col0, g, dst_t, dst, nfeat) in ((3, 0, gq, cqn_t, cqn, 384), (2, 384, gkv, ckvn_t, ckvn, 256)):
                    for bi, (t0, n) in enumerate(TOKBLOCKS):
                        ntile = n // 128
                        for c in range(nchunk):
                            p = ps[c]
                            tiles_ = list(hT[t0 // 128:t0 // 128 + ntile])
                            for kk in range(8):
                                T.mm(p, lambda e: e.matmul(p[:, :n], lhsT=wlat[:, kk, col0 + c * 128:col0 + (c + 1) * 128], rhs=hT.full[:, kk, t0:t0 + n],
                                                           start=(kk == 0), stop=(kk == 7)), [wlat] + tiles_, kk == 0, kk == 7)
                            T.op("act", lambda e: e.copy(out=rawf[c][:, :n], in_=p[:, :n]), reads=[p], writes=[rawf[c]])
                            T.op("dve", lambda e: e.tensor_tensor(out=sqb[c][:, :n], in0=rawf[c][:, :n], in1=p[:, :n], op=ALU.mult), reads=[rawf[c], p], writes=[sqb[c]])
                        pss = ps[3]
                        for c in range(nchunk):
                            T.mm(pss, lambda e: e.matmul(pss[:, :n], lhsT=k["onesb"][:], rhs=sqb[c][:, :n], start=(c == 0), stop=(c == nchunk - 1)),
                                 [k["onesb"], sqb[c]], c == 0, c == nchunk - 1)
                        T.op("dve", lambda e: e.tensor_scalar(out=rs[:, :n], in0=pss[:, :n], scalar1=1.0 / nfeat, scalar2=EPS, op0=ALU.mult, op1=ALU.add), reads=[pss], writes=[rs])
                        T.op("act", lambda e: e.activation(out=rs[:, :n], in_=rs[:, :n], func=AF.Sqrt), reads=[rs], writes=[rs])
                        T.op("dve", lambda e: e.reciprocal(out=rs[:, :n], in_=rs[:, :n]), reads=[rs], writes=[rs])
                        for c in range(nchunk):
                            T.op("dve", lambda e: e.scalar_tensor_tensor(out=dst_t[:, c, t0:t0 + n], in0=rawf[c][:, :n], scalar=g[:, c:c + 1], in1=rs[:, :n], op0=ALU.mult, op1=ALU.mult),
                                 reads=[rawf[c], g, rs], writes=[dst[t0 // 128 + j] for j in range(ntile)])
            proj_fm(C, k, kr, hT, 8, wlat, 640, 32, ps[0:2], rope=(cosR, sinR, P32), tmp=(raw, t1, ps[2:4]))
            qn = C.sb([128, NTOK], BF16)
            qr = C.sb([128, NTOK], BF16)
            kn = C.sb([128, NTOK], BF16)
            T.op("dve", lambda e: e.memset(qn[64:128, :], 0.0), writes=[qn])
            T.op("dve", lambda e: e.memset(kn[64:128, :], 0.0), writes=[kn])
            T.op("dve", lambda e: e.memset(qr[32:64, :], 0.0), writes=[qr])
            T.op("dve", lambda e: e.memset(qr[64:128, :], 0.0), writes=[qr])
            vaug = C.sb([128, 18, 65], BF16)
            T.op("dve", lambda e: e.memset(vaug[:, :, 64:65], 1.0), writes=[vaug])
            om = C.sb([128, 18, 64], F32)
            omb = C.sb([128, 18, 64], BF16)
            pts = [C.sb([128, 512], BF16) for _ in range(3)]
            for h in range(8):
                proj_fm(C, k, qn, cqn, 3, qupb, h * 96, 64, ps[0:2])
                proj_fm(C, k, qr, cqn, 3, qupb, h * 96 + 64, 32, ps[0:2], rope=(cosR, sinR, P32), tmp=(raw, t1, ps[2:4]))
                proj_fm(C, k, kn, ckvn, 2, kvupb, h * 128, 64, ps[0:2])
                proj_tm(C, vaug, ckvn, 2, kvupb, h * 128 + 64, 64, ps[0:2])
                attend(C, [(kn, qn, 128), (kr, qr, 128)], vaug, 64, MLA_SCALE, om, ps[0:2], ps[4:8], pts)
                T.op("act", lambda e: e.copy(out=omb[:], in_=om[:]), reads=[om], writes=[omb])
                T.dma("sp", attn_d.t.rearrange("(t p) d -> p t d", p=128)[:, :, 512 + h * 64:512 + (h + 1) * 64], omb[:], reads=[omb], writes=[attn_d])
    woutb = load_cast(C, "sp", wout_d.t.rearrange("(c p) f -> p c f", p=128), [128, 8, D])
    with scope(C):
        M2 = {"l": bcast_load(C, "sp", modv(0, 2), D), "c": bcast_load(C, "act", modv(1, 2), D)}
        at = [C.sb([128, D], BF16) for _ in range(3)]
        aT = [C.sb([128, 8, 128], BF16) for _ in range(3)]
        xt = [C.sb([128, D], F32) for _ in range(3)]
        tmp = [C.sb([128, D], F32) for _ in range(3)]

        def wo_stage1(t):
            a, aTt, x = at[t % 3], aT[t % 3], xt[t % 3]
            T.dma("sp", a[:], attn_d.t[t * 128:(t + 1) * 128, :], reads=[attn_d], writes=[a])
            T.dma("act", x[:], x_tiles[t].t, reads=[x_tiles[t]], writes=[x])
            p = ps[t % 2]
            pv = p.t[:].bitcast(BF16)
            for c in range(8):
                T.op("pe", lambda e: e.transpose(pv[:, c * 128:(c + 1) * 128], a[:, c * 128:(c + 1) * 128], k["identb"][:]), reads=[a, k["identb"]], writes=[p])
            evac(C, t, aTt[:], pv.rearrange("p (c n) -> p c n", c=8), [p], [aTt])

        def wo_stage2(t):
            nm = "c" if t < 2 else "l"
            aTt, x, tm = aT[t % 3], xt[t % 3], tmp[t % 3]
            for half in range(2):
                po = ps[2 + 2 * (t % 2) + half]
                for c in range(8):
                    T.mm(po, lambda e: e.matmul(po[:], lhsT=aTt[:, c, :], rhs=woutb[:, c, half * 512:(half + 1) * 512], start=(c == 0), stop=(c == 7)),
                         [aTt, woutb], c == 0, c == 7)
                T.op("dve", lambda e: e.tensor_tensor(out=tm[:, half * 512:(half + 1) * 512], in0=po[:], in1=M2[nm][:, half * 512:(half + 1) * 512], op=ALU.mult),
                     reads=[po, M2[nm]], writes=[tm])
            T.op("dve", lambda e: e.tensor_tensor(out=tm[:], in0=tm[:], in1=x[:], op=ALU.add), reads=[tm, x], writes=[tm])
            T.dma("pool", xmid_tiles[t].t, tm[:], reads=[tm], writes=[xmid_tiles[t]])

        wo_stage1(0)
        for t in range(NT_TILES):
            if t + 1 < NT_TILES:
                wo_stage1(t + 1)
            wo_stage2(t)
    h_tok = [C.sb([128, D], BF16) for _ in range(NT_TILES)]
    logits = C.sb([128, 18, 16], F32)
    with scope(C):
        router_sb = C.sb([128, 8, NE], F32)
        T.dma("sp", router_sb[:], rt_d.t.rearrange("(c p) e -> p c e", p=128), writes=[router_sb])
        cb = router_cb_factory(C, k, router_sb, logits, ps[2:4], ps[4])
        norm_mod_transpose(C, k, xmid_tiles, n2g_d.t, modv(0, 3), modv(0, 4), modv(1, 3), modv(1, 4), None, ps[0:2], h_tok=h_tok, tile_cb=cb)
    route_and_gather(C, k, h_tok, logits, xeT_d, meta_d, ps, True)
    T.finish([xeT_d, meta_d] + xmid_tiles)


def build_attn():
    nc = new_nc()
    with ExitStack() as es:
        C = Ctx(nc, es)
        body_attn(C)
    return nc

def moe_scatter(C, k, xin_tiles, y_d, meta_d, m5_l, m5_c, ps, with_ctx, tile_done):
    T = C.T
    NS = 288 if with_ctx else 256
    t_first = 0 if with_ctx else 2
    with scope(C):
        xres = {}
        for t in range(t_first, NT_TILES):
            xres[t] = C.sb([128, D], F32)
            T.dma(["sp", "act"][t % 2], xres[t][:], xin_tiles[t].t, reads=[xin_tiles[t]], writes=[xres[t]])
        M5l = bcast_load(C, "sp", m5_l, D)
        M5c = bcast_load(C, "act", m5_c, D) if with_ctx else None
        ml = C.sb([128, 4, 2, 16], F32)
        for st in range(2):
            for c in range(4):
                T.dma(["sp", "act"][c % 2], ml[:, c, st, :], meta_d.t[c, :, st * 128:(st + 1) * 128].rearrange("e p -> p e"), writes=[ml], allow_slow_non_contiguous=True)
        idxl = C.sb([128, 2, 16], F32)
        gl = C.sb([128, 2, 16], F32)
        T.op("dve", lambda e: e.scalar_tensor_tensor(out=idxl.t.rearrange("p a b -> p (a b)"), in0=ml.t[:, 0].rearrange("p a b -> p (a b)"), scalar=128.0,
                                                     in1=ml.t[:, 1].rearrange("p a b -> p (a b)"), op0=ALU.mult, op1=ALU.add), reads=[ml], writes=[idxl])
        T.op("dve", lambda e: e.tensor_tensor(out=gl[:], in0=ml[:, 2], in1=ml[:, 3], op=ALU.add), reads=[ml], writes=[gl])
        if with_ctx:
            mc = C.sb([32, 4, 16], F32)
            for c in range(4):
                T.dma(["sp", "act"][c % 2], mc[:, c, :], meta_d.t[c, :, 256:288].rearrange("e p -> p e"), writes=[mc], allow_slow_non_contiguous=True)
            idxc = C.sb([32, 16], F32)
            gc = C.sb([32, 16], F32)
            T.op("dve", lambda e: e.scalar_tensor_tensor(out=idxc[:], in0=mc[:, 0, :], scalar=128.0, in1=mc[:, 1, :], op0=ALU.mult, op1=ALU.add), reads=[mc], writes=[idxc])
            T.op("dve", lambda e: e.tensor_tensor(out=gc[:], in0=mc[:, 2, :], in1=mc[:, 3, :], op=ALU.add), reads=[mc], writes=[gc])
        iota_t = C.sb([128, NTOK], U16)
        T.op("pool", lambda e: e.iota(iota_t[:], pattern=[[1, NTOK]], base=0, channel_multiplier=0, allow_small_or_imprecise_dtypes=True), writes=[iota_t])
        GE = 2
        ohs = [[C.sb([128, SEQ], BF16) for _ in range(2 * GE)] for _ in range(2)]
        yss = [[C.sb([128, D], BF16) for _ in range(2 * GE)] for _ in range(2)]
        yst = [C.sb([128, D], F32) for _ in range(3)]
        ohc = [[C.sb([32, CTX], BF16) for _ in range(GE)] for _ in range(2)]
        ysc = [[C.sb([32, D], BF16) for _ in range(GE)] for _ in range(2)]
        li = 0
        pi_ = 0

        def prep(g):
            nonlocal li
            gb = g % 2
            for ee in range(GE):
                ex = g * GE + ee
                for st in range(2):
                    oh, ys, yt = ohs[gb][ee * 2 + st], yss[gb][ee * 2 + st], yst[li % 3]
                    li += 1
                    T.dma(["sp", "act"][li % 2], yt[:], y_d.t[ex, st * 128:(st + 1) * 128, :], reads=[y_d], writes=[yt])
                    T.op("dve", lambda e: e.scalar_tensor_tensor(out=ys[:], in0=yt[:], scalar=gl[:, st, ex:ex + 1], in1=M5l[:], op0=ALU.mult, op1=ALU.mult),
                         reads=[yt, gl, M5l], writes=[ys])
                    T.op("dve", lambda e: e.tensor_scalar(out=oh[:], in0=iota_t[:, 256:NTOK], scalar1=idxl[:, st, ex:ex + 1], scalar2=None, op0=ALU.is_equal),
                         reads=[iota_t, idxl], writes=[oh])
                if with_ctx:
                    oh, ys, yt = ohc[gb][ee], ysc[gb][ee], yst[li % 3]
                    li += 1
                    T.dma("sp", yt[:32, :], y_d.t[ex, 256:288, :], reads=[y_d], writes=[yt])
                    T.op("dve", lambda e: e.scalar_tensor_tensor(out=ys[:], in0=yt[:32, :], scalar=gc[:, ex:ex + 1], in1=M5c[:32, :], op0=ALU.mult, op1=ALU.mult),
                         reads=[yt, gc, M5c], writes=[ys])
                    T.op("dve", lambda e: e.tensor_scalar(out=oh[:], in0=iota_t[:32, 0:CTX], scalar1=idxc[:, ex:ex + 1], scalar2=None, op0=ALU.is_equal),
                         reads=[iota_t, idxc], writes=[oh])
        def accum(g):
            nonlocal pi_
            gb = g % 2
            for t in range(t_first, NT_TILES):
                for half in range(2):
                    p = ps[pi_ % 4]
                    pi_ += 1
                    if t >= 2:
                        n = 2 * GE
                        for j in range(n):
                            T.mm(p, lambda e: e.matmul(p[:], lhsT=ohs[gb][j][:, (t - 2) * 128:(t - 1) * 128], rhs=yss[gb][j][:, half * 512:(half + 1) * 512],
                                                       start=(j == 0), stop=(j == n - 1)), [ohs[gb][j], yss[gb][j]], j == 0, j == n - 1)
                    else:
                        for j in range(GE):
                            T.mm(p, lambda e: e.matmul(p[:], lhsT=ohc[gb][j][:, t * 128:(t + 1) * 128], rhs=ysc[gb][j][:, half * 512:(half + 1) * 512],
                                                       start=(j == 0), stop=(j == GE - 1)), [ohc[gb][j], ysc[gb][j]], j == 0, j == GE - 1)
                    T.op("dve", lambda e: e.tensor_tensor(out=xres[t][:, half * 512:(half + 1) * 512], in0=p[:], in1=xres[t][:, half * 512:(half + 1) * 512], op=ALU.add),
                         reads=[p, xres[t]], writes=[xres[t]])
        NG = NE // GE
        prep(0)
        for g in range(NG):
            if g + 1 < NG:
                prep(g + 1)
            accum(g)
        for t in range(t_first, NT_TILES):
            tile_done(t, xres[t])


NCH = NTOK // 64
O_GQ, O_GK, O_GV, O_GG, O_GD, O_HQ, O_HF, O_HI, O_HG = 0, 256, 512, 1024, 1536, 1568, 2080, 3104, 3616
REC_IN = 4128


def body_rec(C):
    nc = C.nc
    T = C.T
    xmid_d = C.din("xmid", [NTOK, D], F32)
    y_d = C.din("y", [NE, 288, D], F32)
    meta_in = C.din("meta_in", [4, NE, 288], F32)
    mod0_d = C.din("mod0", [2, 6 * D], F32)
    mod_d = C.din("mod", [2, 6 * D], F32)
    n1g_d = C.din("n1g", [D], F32)
    n2g_d = C.din("n2g", [D], F32)
    win_d = C.din("w_in", [D, REC_IN], F32)
    gkup_d = C.din("gk_up", [2, 16, 256], F32)
    gkb_d = C.din("gk_bias", [2, 256], F32)
    glag_d = C.din("gla_g", [128], F32)
    hgg_d = C.din("hg_g", [128], F32)
    lbl_d = C.din("lb_logits", [2, 2, 512], F32)
    wout_d = C.din("w_out", [D, D], F32)
    rt_d = C.din("router", [D, NE], F32)
    xmid1_d = C.dout("xmid1", [NTOK, D], F32)
    xeT_d = C.dout("xeT", [NE, D, 256], BF16)
    meta_d = C.dout("meta", [4, NE, 256], F32)
    x1_d = Buf(nc.dram_tensor("x1_scr", [NTOK, D], F32, kind="Internal").ap())
    a_d = Buf(nc.dram_tensor("a_scr", [NTOK, D], BF16, kind="Internal").ap())
    xmid_tiles = [Buf(xmid_d.t[t * 128:(t + 1) * 128, :]) for t in range(NT_TILES)]
    x1_tiles = [Buf(x1_d.t[t * 128:(t + 1) * 128, :]) for t in range(NT_TILES)]
    xmid1_tiles = [Buf(xmid1_d.t[t * 128:(t + 1) * 128, :]) for t in range(NT_TILES)]
    ps = [C.ps([128, 512]) for _ in range(8)]
    k = make_consts(C)
    win_v = win_d.t.rearrange("(k p) f -> p k f", p=128)

    def modv(row, i):
        return mod_d.t[row, i * D:(i + 1) * D]

    def done0(t, xb):
        T.dma(["sp", "act"][t % 2], x1_tiles[t].t, xb[:], reads=[xb], writes=[x1_tiles[t]])
    moe_scatter(C, k, xmid_tiles, y_d, meta_in, mod0_d.t[0, 5 * D:6 * D], mod0_d.t[1, 5 * D:6 * D], ps, True, done0)

    with scope(C):
        hT_t = C.sb([128, 8, NTOK], BF16)
        hT = tile_views(hT_t)
        norm_mod_transpose(C, k, x1_tiles, n1g_d.t, modv(0, 0), modv(0, 1), modv(1, 0), modv(1, 1), hT, ps[0:2])
        dji = k["dji"]
        fb = C.sb([128, 128], F32)
        pb = C.sb([128, 1], F32)
        eq = C.sb([128, 128], F32)
        Mf = C.sb([128, 128], F32)
        Mb = C.sb([128, 128], F32)
        T.op("pool", lambda e: e.iota(fb[:], pattern=[[1, 128]], base=0, channel_multiplier=0, allow_small_or_imprecise_dtypes=True), writes=[fb])
        T.op("pool", lambda e: e.iota(pb[:], pattern=[[0, 1]], base=0, channel_multiplier=1, allow_small_or_imprecise_dtypes=True), writes=[pb])
        T.op("dve", lambda e: e.tensor_single_scalar(out=fb[:], in_=fb[:], scalar=64.0, op=ALU.is_ge), reads=[fb], writes=[fb])
        T.op("dve", lambda e: e.tensor_single_scalar(out=pb[:], in_=pb[:], scalar=64.0, op=ALU.is_ge), reads=[pb], writes=[pb])
        T.op("dve", lambda e: e.tensor_scalar(out=eq[:], in0=fb[:], scalar1=pb[:, 0:1], scalar2=None, op0=ALU.is_equal), reads=[fb, pb], writes=[eq])
        T.op("dve", lambda e: e.tensor_single_scalar(out=Mf[:], in_=dji[:], scalar=0.0, op=ALU.is_ge), reads=[dji], writes=[Mf])
        T.op("dve", lambda e: e.tensor_tensor(out=Mf[:], in0=Mf[:], in1=eq[:], op=ALU.mult), reads=[Mf, eq], writes=[Mf])
        T.op("dve", lambda e: e.tensor_single_scalar(out=Mb[:], in_=dji[:], scalar=0.0, op=ALU.is_le), reads=[dji], writes=[Mb])
        T.op("dve", lambda e: e.tensor_tensor(out=Mb[:], in0=Mb[:], in1=eq[:], op=ALU.mult), reads=[Mb, eq], writes=[Mb])
        rm = C.sb([128, NCH, 64], BF16)
        mA = C.sb([128, NT_TILES, 128], BF16)
        with scope(C):
            tmpi = C.sb([128, NTOK], F32)
            T.op("pool", lambda e: e.iota(tmpi.t.rearrange("p (a b) -> p a b", b=64), pattern=[[0, NCH], [1, 64]], base=0, channel_multiplier=0, allow_small_or_imprecise_dtypes=True), writes=[tmpi])
            T.op("dve", lambda e: e.tensor_single_scalar(out=rm.t.rearrange("p a b -> p (a b)"), in_=tmpi[:], scalar=0.0, op=ALU.is_gt), reads=[tmpi], writes=[rm])
            T.op("pool", lambda e: e.iota(tmpi.t.rearrange("p (a b) -> p a b", b=128), pattern=[[0, NT_TILES], [1, 128]], base=0, channel_multiplier=0, allow_small_or_imprecise_dtypes=True), reads=[tmpi], writes=[tmpi])
            T.op("dve", lambda e: e.tensor_single_scalar(out=mA.t.rearrange("p a b -> p (a b)"), in_=tmpi[:], scalar=64.0, op=ALU.is_lt), reads=[tmpi], writes=[mA])
        rmf = rm.t.rearrange("p a b -> p (a b)")
        mAf = mA.t.rearrange("p a b -> p (a b)")
        wgd = load_cast(C, "sp", win_v[:, :, O_GD:O_GD + 32], [128, 8, 32])
        gd = [C.sb([16, NTOK], BF16) for _ in range(2)]
        for d in range(2):
            proj_fm(C, k, gd[d], hT, 8, wgd, d * 16, 16, ps[0:2])
        gkupb = C.sb([16, 2, 256], BF16)
        with scope(C):
            st = C.sb([16, 2, 256], F32)
            T.dma("sp", st[:], gkup_d.t.rearrange("d r f -> r d f"), writes=[st])
            T.op("dve", lambda e: e.tensor_copy(out=gkupb[:], in_=st[:]), reads=[st], writes=[gkupb])
        ngkb = C.sb([64, 2, 4], F32)
        T.dma("pool", ngkb[:], gkb_d.t.rearrange("d (h p) -> p d h", p=64), writes=[ngkb], allow_slow_non_contiguous=True)
        T.op("dve", lambda e: e.tensor_scalar(out=ngkb[:], in0=ngkb[:], scalar1=-1.0, scalar2=None, op0=ALU.mult), reads=[ngkb], writes=[ngkb])
        lbt = C.sb([128, 2, 2, 4], F32)
        T.dma("pool", lbt[:], lbl_d.t.rearrange("l d (h p) -> p l d h", p=128), writes=[lbt], allow_slow_non_contiguous=True)
        lb = C.sb([128, 2, 4], F32)
        oml = C.sb([128, 2, 4], F32)
        T.op("dve", lambda e: e.tensor_tensor(out=lb[:], in0=lbt[:, 1], in1=lbt[:, 0], op=ALU.subtract), reads=[lbt], writes=[lb])
        T.op("act", lambda e: e.activation(out=lb[:], in_=lb[:], func=AF.Sigmoid), reads=[lb], writes=[lb])
        T.op("dve", lambda e: e.tensor_scalar(out=oml[:], in0=lb[:], scalar1=-1.0, scalar2=1.0, op0=ALU.mult, op1=ALU.add), reads=[lb], writes=[oml])
        Gg = {"gla": bcast_load(C, "sp", glag_d.t, 128), "hg": bcast_load(C, "act", hgg_d.t, 128)}
        wsts = [C.sb([128, 8, 128], F32) for _ in range(2)]
        whb = C.sb([128, 8, 640], BF16)
        qf = C.sb([128, NTOK], F32)
        kf = C.sb([128, NTOK], F32)
        la = C.sb([128, NTOK], F32)
        bb = C.sb([128, NTOK], F32)
        tA = C.sb([128, NTOK], F32)
        tB = C.sb([128, NTOK], F32)
        qe = C.sb([128, NTOK], BF16)
        qeA = C.sb([128, NTOK], BF16)
        qeB = C.sb([128, NTOK], BF16)
        qt = C.sb([128, NTOK], BF16)
        kt = C.sb([128, NTOK], BF16)
        kh = C.sb([128, NTOK], BF16)
        dec = C.sb([128, NCH], F32)
        khtm = C.sb([128, NT_TILES, 128], BF16)
        vtm = C.sb([128, NT_TILES, 128], BF16)
        sgate = C.sb([128, 16, 128], BF16)
        ofw = C.sb([128, 16, 128], F32)
        ssq = C.sb([128, 16], F32)
        ab = C.sb([128, 16, 128], BF16)
        Sf = [C.sb([128, 128], F32) for _ in range(2)]
        Sall_t = C.sb([128, NCH, 128], BF16)
        T.op("dve", lambda e: e.memset(Sall_t[64:128], 0.0), writes=[Sall_t])
        for zb in (qeA, qeB, qt, kt):
            T.op("dve", lambda e: e.memset(zb[64:128, :], 0.0), writes=[zb])
        Sall = [Buf(Sall_t.t[:, c, :]) for c in range(NCH)]
        scm = [C.sb([128, 128], BF16) for _ in range(2)]

        def v3(buf):
            return buf.t.rearrange("p (c l) -> p c l", l=64)

        for head in range(8):
            gla = head < 4
            h = head % 4
            dk = 64 if gla else 128
            if gla:
                cols = [(O_GQ + h * 64, 64), (O_GK + h * 64, 64), (O_GV + h * 128, 128), (O_GG + h * 128, 128)]
            else:
                cols = [(O_HQ + h * 128, 128), (O_HF + h * 128, 128), (O_HF + 512 + h * 128, 128), (O_HI + h * 128, 128), (O_HG + h * 128, 128)]
            offs = []
            o = 0
            for j, (c0, n) in enumerate(cols):
                wst = wsts[j % 2]
                T.dma(["sp", "act"][j % 2], wst[:, :, :n], win_v[:, :, c0:c0 + n], writes=[wst])
                T.op("dve", lambda e: e.tensor_copy(out=whb[:, :, o:o + n], in_=wst[:, :, :n]), reads=[wst], writes=[whb])
                offs.append(o)
                o += n
            vo, go = offs[-2], offs[-1]
            proj_tm(C, vtm, hT, 8, whb, vo, 128, ps[0:2])
            for t in range(2, NT_TILES):
                p = ps[t % 2]
                for kk in range(8):
                    T.mm(p, lambda e: e.matmul(p[:, :128], lhsT=hT[t][:, kk, :], rhs=whb[:, kk, go:go + 128], start=(kk == 0), stop=(kk == 7)),
                         [hT[t], whb], kk == 0, kk == 7)
                T.op("act", lambda e: e.activation(out=sgate[:, t - 2, :], in_=p[:, :128], func=AF.Silu), reads=[p], writes=[sgate])

            def fm_raw(dst, col0, func=None, scale=1.0):
                for bi, (t0, n) in enumerate(TOKBLOCKS):
                    p = ps[bi % 2]
                    ntile = n // 128
                    tiles_ = list(hT[t0 // 128:t0 // 128 + ntile])
                    for kk in range(8):
                        T.mm(p, lambda e: e.matmul(p[:dk, :n], lhsT=whb[:, kk, col0:col0 + dk], rhs=hT.full[:, kk, t0:t0 + n],
                                                   start=(kk == 0), stop=(kk == 7)), [whb] + tiles_, kk == 0, kk == 7)
                    T.op("act", lambda e: e.activation(out=dst[:dk, t0:t0 + n], in_=p[:dk, :n], func=(func or AF.Copy), scale=scale), reads=[p], writes=[dst])
            if gla:
                fm_raw(qf, offs[0], None, 64 ** -0.5)
                fm_raw(kf, offs[1])
            else:
                fm_raw(qf, offs[0], AF.Silu)
            for d in range(2):
                if gla:
                    for bi, (t0, n) in enumerate(TOKBLOCKS):
                        p = ps[bi % 2]
                        T.mm(p, lambda e: e.matmul(p[:64, :n], lhsT=gkupb[:, d, h * 64:(h + 1) * 64], rhs=gd[d][:, t0:t0 + n], start=True, stop=True),
                             [gkupb, gd[d]], True, True)
                        T.op("act", lambda e: e.activation(out=la[:64, t0:t0 + n], in_=p[:64, :n], func=AF.Exp, scale=-1.0, bias=ngkb[:, d, h:h + 1]),
                             reads=[p, ngkb], writes=[la])
                    T.op("act", lambda e: e.activation(out=la[:64], in_=la[:64], func=AF.Ln, bias=1.0), reads=[la], writes=[la])
                    T.op("dve", lambda e: e.tensor_scalar(out=la[:64], in0=la[:64], scalar1=-1.0 / 16.0, scalar2=None, op0=ALU.mult), reads=[la], writes=[la])
                else:
                    fm_raw(kf, offs[1 + d], AF.Sigmoid)
                    T.op("dve", lambda e: e.tensor_scalar(out=kf[:], in0=kf[:], scalar1=oml[:, d, h:h + 1], scalar2=lb[:, d, h:h + 1], op0=ALU.mult, op1=ALU.add),
                         reads=[kf, oml, lb], writes=[kf])
                    T.op("act", lambda e: e.activation(out=la[:], in_=kf[:], func=AF.Ln), reads=[kf], writes=[la])
                    T.op("dve", lambda e: e.tensor_scalar(out=kf[:], in0=kf[:], scalar1=-1.0, scalar2=1.0, op0=ALU.mult, op1=ALU.add), reads=[kf], writes=[kf])
                T.op("dve", lambda e: e.tensor_tensor_scan(out=bb[:dk], data0=rmf[:dk], data1=la[:dk], initial=0.0, op0=ALU.mult, op1=ALU.add),
                     reads=[rm, la], writes=[bb])
                b3 = v3(bb)
                T.op("act", lambda e: e.copy(out=dec[:dk], in_=b3[:dk, :, 63]), reads=[bb], writes=[dec])
                decb = dec[:dk].unsqueeze(2).to_broadcast([dk, NCH, 64])
                if d == 1:
                    T.op("dve", lambda e: e.tensor_tensor(out=tB[:dk], in0=bb[:dk], in1=la[:dk], op=ALU.subtract), reads=[bb, la], writes=[tB])
                    T.op("dve", lambda e: e.tensor_tensor(out=b3[:dk], in0=decb, in1=v3(tB)[:dk], op=ALU.subtract), reads=[dec, tB], writes=[bb])
                    iref = 31
                else:
                    T.op("dve", lambda e: e.tensor_tensor(out=v3(tB)[:dk], in0=decb, in1=b3[:dk], op=ALU.subtract), reads=[dec, bb], writes=[tB])
                    iref = 32
                T.op("dve", lambda e: e.tensor_tensor(out=v3(tA)[:dk], in0=b3[:dk], in1=b3[:dk, :, iref:iref + 1].to_broadcast([dk, NCH, 64]), op=ALU.subtract),
                     reads=[bb], writes=[tA])
                T.op("act", lambda e: e.activation(out=la[:dk], in_=bb[:dk], func=AF.Exp), reads=[bb], writes=[la])
                T.op("act", lambda e: e.activation(out=bb[:dk], in_=tA[:dk], func=AF.Exp), reads=[tA], writes=[bb])
                T.op("act", lambda e: e.activation(out=tB[:dk], in_=tB[:dk], func=AF.Exp), reads=[tB], writes=[tB])
                T.op("dve", lambda e: e.tensor_tensor(out=qe[:dk], in0=qf[:dk], in1=la[:dk], op=ALU.mult), reads=[qf, la], writes=[qe])
                T.op("act", lambda e: e.activation(out=la[:dk], in_=tA[:dk], func=AF.Exp, scale=-1.0), reads=[tA], writes=[la])
                T.op("dve", lambda e: e.tensor_tensor(out=qt[:dk], in0=qf[:dk], in1=bb[:dk], op=ALU.mult), reads=[qf, bb], writes=[qt])
                T.op("dve", lambda e: e.tensor_tensor(out=kh[:dk], in0=kf[:dk], in1=tB[:dk], op=ALU.mult), reads=[kf, tB], writes=[kh])
                T.op("dve", lambda e: e.tensor_tensor(out=qeA[:dk], in0=qe[:dk], in1=mAf[:dk], op=ALU.mult), reads=[qe, mA], writes=[qeA])
                T.op("dve", lambda e: e.tensor_tensor(out=qeB[:dk], in0=qe[:dk], in1=qeA[:dk], op=ALU.subtract), reads=[qe, qeA], writes=[qeB])
                T.op("dve", lambda e: e.tensor_tensor(out=kt[:dk], in0=kf[:dk], in1=la[:dk], op=ALU.mult), reads=[kf, la], writes=[kt])
                T.op("act", lambda e: e.activation(out=dec[:dk], in_=dec[:dk], func=AF.Exp), reads=[dec], writes=[dec])
                for t in range(NT_TILES):
                    p = ps[t % 2]
                    pv = p.t[:].bitcast(BF16)
                    T.op("pe", lambda e: e.transpose(pv[:, :dk], kh[:dk, t * 128:(t + 1) * 128], k["identb"][:dk, :dk]), reads=[kh, k["identb"]], writes=[p])
                    evac(C, t, khtm[:, t, :dk], pv[:, :dk], [p], [khtm])
                order = list(range(NCH)) if d == 0 else [3, 2, 1, 0] + list(range(NCH - 1, 3, -1))
                Mm = Mf if d == 0 else Mb
                T.op("dve", lambda e: e.memset(Sf[0][:], 0.0), writes=[Sf[0]])
                for ci, c in enumerate(order):
                    t, half = c // 2, c % 2
                    r0 = half * 64
                    Scur, Snew = Sf[ci % 2], Sf[(ci + 1) % 2]
                    if c >= 4:
                        T.op("act", lambda e: e.copy(out=Sall[c][:dk], in_=Scur[:dk]), reads=[Scur], writes=[Sall[c]])
                    if ci == len(order) - 1:
                        break
                    pk = ps[ci % 4]
                    T.mm(pk, lambda e: e.matmul(pk[:dk, :128], lhsT=khtm[r0:r0 + 64, t, :dk], rhs=vtm[r0:r0 + 64, t, :], start=True, stop=True),
                         [khtm, vtm], True, True)
                    T.op("dve", lambda e: e.scalar_tensor_tensor(out=Snew[:dk], in0=Scur[:dk], scalar=dec[:dk, c:c + 1], in1=pk[:dk, :128], op0=ALU.mult, op1=ALU.add),
                         reads=[Scur, dec, pk], writes=[Snew])
                def p2_scores(t):
                    pS = ps[4 + t % 2]
                    T.mm(pS, lambda e: e.matmul(pS[:, :128], lhsT=kt[:, t * 128:(t + 1) * 128], rhs=qt[:, t * 128:(t + 1) * 128], start=True, stop=True),
                         [kt, qt], True, True)
                    sc = scm[t % 2]
                    T.op("dve", lambda e: e.tensor_tensor(out=sc[:], in0=pS[:, :128], in1=Mm[:], op=ALU.mult), reads=[pS, Mm], writes=[sc])

                def p2_readout(t):
                    sc = scm[t % 2]
                    po = ps[6 + t % 2]
                    SA, SB_ = Sall[2 * t], Sall[2 * t + 1]
                    T.mm(po, lambda e: e.matmul(po[:, :128], lhsT=sc[:], rhs=vtm[:, t, :], start=True, stop=False), [sc, vtm], True, False)
                    T.mm(po, lambda e: e.matmul(po[:, :128], lhsT=qeA[:, t * 128:(t + 1) * 128], rhs=SA[:], start=False, stop=False), [qeA, SA], False, False)
                    T.mm(po, lambda e: e.matmul(po[:, :128], lhsT=qeB[:, t * 128:(t + 1) * 128], rhs=SB_[:], start=False, stop=True), [qeB, SB_], False, True)
                    if d == 0:
                        T.op("act", lambda e: e.copy(out=ofw[:, t - 2, :], in_=po[:, :128]), reads=[po], writes=[ofw])
                    else:
                        T.op("dve", lambda e: e.tensor_tensor(out=ofw[:, t - 2, :], in0=po[:, :128], in1=ofw[:, t - 2, :], op=ALU.add), reads=[po, ofw], writes=[ofw])

                p2_scores(2)
                for t in range(2, NT_TILES):
                    if t + 1 < NT_TILES:
                        p2_scores(t + 1)
                    p2_readout(t)
            sqv = tA.t[:, 0:2048].rearrange("p (a b) -> p a b", b=128)
            T.op("act", lambda e: e.activation(out=sqv, in_=ofw[:], func=AF.Square), reads=[ofw], writes=[tA])
            T.op("dve", lambda e: e.tensor_reduce(out=ssq[:], in_=sqv, axis=AX.X, op=ALU.add), reads=[tA], writes=[ssq])
            r = rstd_from_ss(C, ssq, 128, W=16)
            G = Gg["gla" if gla else "hg"]
            for t in range(16):
                T.op("dve", lambda e: e.scalar_tensor_tensor(out=sqv[:, t, :], in0=ofw[:, t, :], scalar=r[:, t:t + 1], in1=G[:], op0=ALU.mult, op1=ALU.mult),
                     reads=[ofw, r, G], writes=[tA])
            T.op("dve", lambda e: e.tensor_tensor(out=ab[:], in0=sqv, in1=sgate[:], op=ALU.mult), reads=[tA, sgate], writes=[ab])
            T.dma("sp", a_d.t.rearrange("(t p) d -> p t d", p=128)[:, 2:, head * 128:(head + 1) * 128], ab[:], reads=[ab], writes=[a_d])
    woutb = load_cast(C, "sp", wout_d.t.rearrange("(c p) f -> p c f", p=128), [128, 8, D])
    with scope(C):
        M2 = bcast_load(C, "sp", modv(0, 2), D)
        at = [C.sb([128, D], BF16) for _ in range(3)]
        aT = [C.sb([128, 8, 128], BF16) for _ in range(3)]
        xt = [C.sb([128, D], F32) for _ in range(3)]
        tmp = [C.sb([128, D], F32) for _ in range(3)]

        def wo_stage1(t):
            a, aTt, x = at[t % 3], aT[t % 3], xt[t % 3]
            T.dma("sp", a[:], a_d.t[t * 128:(t + 1) * 128, :], reads=[a_d], writes=[a])
            T.dma("act", x[:], x1_tiles[t].t, reads=[x1_tiles[t]], writes=[x])
            p = ps[t % 2]
            pv = p.t[:].bitcast(BF16)
            for c in range(8):
                T.op("pe", lambda e: e.transpose(pv[:, c * 128:(c + 1) * 128], a[:, c * 128:(c + 1) * 128], k["identb"][:]), reads=[a, k["identb"]], writes=[p])
            evac(C, t, aTt[:], pv.rearrange("p (c n) -> p c n", c=8), [p], [aTt])

        def wo_stage2(t):
            aTt, x, tm = aT[t % 3], xt[t % 3], tmp[t % 3]
            for half in range(2):
                po = ps[2 + 2 * (t % 2) + half]
                for c in range(8):
                    T.mm(po, lambda e: e.matmul(po[:], lhsT=aTt[:, c, :], rhs=woutb[:, c, half * 512:(half + 1) * 512], start=(c == 0), stop=(c == 7)),
                         [aTt, woutb], c == 0, c == 7)
                T.op("dve", lambda e: e.tensor_tensor(out=tm[:, half * 512:(half + 1) * 512], in0=po[:], in1=M2[:, half * 512:(half + 1) * 512], op=ALU.mult),
                     reads=[po, M2], writes=[tm])
            T.op("dve", lambda e: e.tensor_tensor(out=tm[:], in0=tm[:], in1=x[:], op=ALU.add), reads=[tm, x], writes=[tm])
            T.dma("pool", xmid1_tiles[t].t, tm[:], reads=[tm], writes=[xmid1_tiles[t]])

        wo_stage1(2)
        for t in range(2, NT_TILES):
            if t + 1 < NT_TILES:
                wo_stage1(t + 1)
            wo_stage2(t)
    h_tok = [C.sb([128, D], BF16) for _ in range(NT_TILES)]
    logits = C.sb([128, 18, 16], F32)
    T.op("dve", lambda e: e.memset(logits[:], 0.0), writes=[logits])
    with scope(C):
        router_sb = C.sb([128, 8, NE], F32)
        T.dma("sp", router_sb[:], rt_d.t.rearrange("(c p) e -> p c e", p=128), writes=[router_sb])
        cb = router_cb_factory(C, k, router_sb, logits, ps[2:4], ps[4])
        norm_mod_transpose(C, k, xmid1_tiles, n2g_d.t, modv(0, 3), modv(0, 4), modv(1, 3), modv(1, 4), None, ps[0:2], h_tok=h_tok, tile_cb=cb,
                           tiles=list(range(2, NT_TILES)))
    route_and_gather(C, k, h_tok, logits, xeT_d, meta_d, ps, False)
    T.finish([xeT_d, meta_d] + xmid1_tiles[2:])


def build_rec():
    nc = new_nc()
    with ExitStack() as es:
        C = Ctx(nc, es)
        body_rec(C)
    return nc

def body_final(C):
    nc = C.nc
    T = C.T
    xmid_d = C.din("xmid", [NTOK, D], F32)
    y_d = C.din("y", [NE, 256, D], F32)
    meta_in = C.din("meta_in", [4, NE, 256], F32)
    mod_d = C.din("mod", [2, 6 * D], F32)
    fg_d = C.din("fg", [D], F32)
    out_d = C.dout("out", [SEQ, D], F32)
    xmid_tiles = [Buf(xmid_d.t[t * 128:(t + 1) * 128, :]) for t in range(NT_TILES)]
    out_tiles = [Buf(out_d.t[t * 128:(t + 1) * 128, :]) for t in range(16)]
    ps = [C.ps([128, 512]) for _ in range(4)]
    k = make_consts(C)
    G = bcast_load(C, "sp", fg_d.t, D)
    junk = C.sb([128, D], BF16)
    ss = [C.sb([128, 1], F32) for _ in range(2)]
    ob = [C.sb([128, D], F32) for _ in range(2)]

    def done(t, xb):
        s = ss[t % 2]
        o = ob[t % 2]
        T.op("act", lambda e: e.activation(out=junk[:], in_=xb[:], func=AF.Square, accum_out=s[:, 0:1]), reads=[xb], writes=[junk, s])
        r = rstd_from_ss(C, s, D)
        T.op("dve", lambda e: e.scalar_tensor_tensor(out=o[:], in0=xb[:], scalar=r[:, 0:1], in1=G[:], op0=ALU.mult, op1=ALU.mult), reads=[xb, r, G], writes=[o])
        T.dma(["sp", "act"][t % 2], out_tiles[t - 2].t, o[:], reads=[o], writes=[out_tiles[t - 2]])
    moe_scatter(C, k, xmid_tiles, y_d, meta_in, mod_d.t[0, 5 * D:6 * D], None, ps, False, done)
    T.finish(out_tiles)


def build_final():
    nc = new_nc()
    with ExitStack() as es:
        C = Ctx(nc, es)
        body_final(C)
    return nc

DEBUG = {}
_NC_CACHE = {}


def _get(name, fn, *a):
    key = (name,) + a
    if key not in _NC_CACHE:
        _NC_CACHE[key] = fn(*a)
    return _NC_CACHE[key]


def _run(nc, ims):
    ims = [{k: np.ascontiguousarray(v) for k, v in im.items()} for im in ims]
    return run_bass_kernel_spmd(nc, ims, core_ids=list(range(NCORES))).results


def _ffn_launch(xeTs, wg, wu, wd, NS):
    nc = _get("ffn", build_ffn, NS * NCORES)
    ims = []
    for c in range(NCORES):
        xT = np.stack([np.concatenate([xeTs[b][2 * c + j] for b in range(NCORES)], axis=1) for j in range(2)])
        ims.append({"xT": xT, "wg": wg[2 * c:2 * c + 2], "wu": wu[2 * c:2 * c + 2], "wd": wd[2 * c:2 * c + 2]})
    res = _run(nc, ims)
    ys = []
    for b in range(NCORES):
        ys.append(np.stack([res[e // 2]["y"][e % 2, b * NS:(b + 1) * NS, :] for e in range(NE)]))
    return ys


def kernel_unfused(x, c, ctx, c_ctx, ada_w, ada_b, norm1_g, norm2_g, att_w_in, mla_q_norm_g, mla_q_up,
           mla_kv_norm_g, mla_kv_up, da_lam_q1, da_lam_k1, da_lam_q2, da_lam_k2, da_subln_g, att_w_out,
           rec_w_in, gla_gk_up, gla_gk_bias, gla_norm_g, hg_lb_logits, hg_norm_g, rec_w_out,
           moe_router, moe_w_gate, moe_w_up, moe_w_down, final_norm_g):
    f = lambda a: np.asarray(a, dtype=np.float32)
    x, c, ctx, c_ctx = f(x), f(c), f(ctx), f(c_ctx)
    cc = np.concatenate([c, c_ctx[None, :]], axis=0)
    res = _run(_get("ada", build_ada), [{"ccT": cc.T, "adaw": f(ada_w)[:, :, i * 768:(i + 1) * 768], "adab": f(ada_b)[:, i * 768:(i + 1) * 768]}
                                        for i in range(NCORES)])
    mods = np.concatenate([r["mods"] for r in res], axis=-1)
    DEBUG["mods"] = mods
    modrows = lambda l, b: np.stack([mods[l, b], mods[l, 8]])
    lamv = np.stack([f(da_lam_q1)[0], f(da_lam_k1)[0], f(da_lam_q2)[0], f(da_lam_k2)[0]])
    ims = []
    for b in range(NCORES):
        ims.append({"xin": np.concatenate([ctx[b], x[b]], axis=0), "mod": modrows(0, b), "n1g": f(norm1_g)[0], "n2g": f(norm2_g)[0],
                    "w_in": f(att_w_in)[0], "qng": f(mla_q_norm_g)[0], "q_up": f(mla_q_up)[0], "kvng": f(mla_kv_norm_g)[0], "kv_up": f(mla_kv_up)[0],
                    "lamv": lamv, "subg": f(da_subln_g)[0], "w_out": f(att_w_out)[0], "router": f(moe_router)[0]})
    r1 = _run(_get("attn", build_attn), ims)
    DEBUG["r1"] = r1
    y0 = _ffn_launch([r["xeT"] for r in r1], f(moe_w_gate)[0], f(moe_w_up)[0], f(moe_w_down)[0], 288)
    DEBUG["y0"] = y0
    ims = []
    for b in range(NCORES):
        ims.append({"xmid": r1[b]["xmid"], "y": y0[b], "meta_in": r1[b]["meta"], "mod0": modrows(0, b), "mod": modrows(1, b),
                    "n1g": f(norm1_g)[1], "n2g": f(norm2_g)[1], "w_in": f(rec_w_in)[0], "gk_up": f(gla_gk_up)[0], "gk_bias": f(gla_gk_bias)[0],
                    "gla_g": f(gla_norm_g)[0], "hg_g": f(hg_norm_g)[0], "lb_logits": f(hg_lb_logits), "w_out": f(rec_w_out)[0], "router": f(moe_router)[1]})
    r3 = _run(_get("rec", build_rec), ims)
    DEBUG["r3"] = r3
    y1 = _ffn_launch([r["xeT"] for r in r3], f(moe_w_gate)[1], f(moe_w_up)[1], f(moe_w_down)[1], 256)
    DEBUG["y1"] = y1
    ims = [{"xmid": r3[b]["xmid1"], "y": y1[b], "meta_in": r3[b]["meta"], "mod": modrows(1, b), "fg": f(final_norm_g)} for b in range(NCORES)]
    r5 = _run(_get("final", build_final), ims)
    return np.stack([r["out"] for r in r5]).astype(np.float32)


def body_ada_full(C, ccT_d, w_d, b_d, mods_d):
    T = C.T
    ccT = C.sb([128, 8, 2], F32)
    scT = C.sb([128, 8, 2], F32)
    T.dma("sp", ccT[:], ccT_d.t.rearrange("(k p) s -> p k s", p=128), writes=[ccT])
    T.op("act", lambda e: e.activation(out=scT[:], in_=ccT[:], func=AF.Silu), reads=[ccT], writes=[scT])
    bias = C.sb([2, 2, 6 * D], F32)
    res = C.sb([2, 2, 6 * D], F32)
    for l in range(2):
        T.dma("pool", bias[:, l, :], b_d.t[l].partition_broadcast(2), writes=[bias])
    wb = [C.sb([128, 8, 1536], F32) for _ in range(2)]
    pss = [C.ps([128, 512]) for _ in range(2)]
    i = 0
    for l in range(2):
        for blk in range(4):
            w = wb[i % 2]
            T.dma(["sp", "act"][i % 2], w[:], w_d.t[l].rearrange("(k p) f -> p k f", p=128)[:, :, blk * 1536:(blk + 1) * 1536], writes=[w])
            for n in range(3):
                p = pss[(i * 3 + n) % 2]
                c0 = blk * 1536 + n * 512
                for k_ in range(8):
                    T.mm(p, lambda e: e.matmul(p[:2, :], lhsT=scT[:, k_, :], rhs=w[:, k_, n * 512:(n + 1) * 512], start=(k_ == 0), stop=(k_ == 7)),
                         [scT, w], k_ == 0, k_ == 7)
                T.op("dve", lambda e: e.tensor_tensor(out=res[:, l, c0:c0 + 512], in0=p[:2, :], in1=bias[:, l, c0:c0 + 512], op=ALU.add),
                     reads=[p, bias], writes=[res])
            i += 1
    T.dma("sp", mods_d.t.rearrange("l s f -> s l f"), res[:], reads=[res], writes=[mods_d])


def body_ffn_bp(C, xeT_d, wg_d, wu_d, wd_d, y_d, NS):
    T = C.T
    ttiles = [(s, min(128, NS - s)) for s in range(0, NS, 128)]
    NB = 4
    xTs = [C.sb([128, 8, NS], BF16) for _ in range(2)]
    hid_t = [C.sb([128, NFC, NS], BF16) for _ in range(2)]
    wdb_t = [C.sb([128, NFC, D], BF16) for _ in range(2)]
    wgb = [C.sb([128, 8, 256], BF16) for _ in range(NB)]
    wub = [C.sb([128, 8, 256], BF16) for _ in range(NB)]
    sg = [C.sb([128, 512], F32) for _ in range(2)]
    ysb = [C.sb([128, D], F32) for _ in range(2)]
    pg = [C.ps([128, 512]) for _ in range(2)]
    pu = [C.ps([128, 512]) for _ in range(2)]
    py = [C.ps([128, 512]) for _ in range(2)]
    it = 0
    yi = 0
    for ex in range(NE):
        xT = xTs[ex % 2]
        hid = [Buf(hid_t[ex % 2].t[:, fc, :]) for fc in range(NFC)]
        wdbt = wdb_t[ex % 2]
        wdb = [Buf(wdbt.t[:, 2 * g:2 * g + 2, :]) for g in range(NFC // 2)]
        T.dma("act", xT[:], xeT_d.t[ex].rearrange("(k p) t -> p k t", p=128), reads=[xeT_d], writes=[xT])
        wgv = wg_d.t[ex].rearrange("(k p) f -> p k f", p=128)
        wuv = wu_d.t[ex].rearrange("(k p) f -> p k f", p=128)
        wdv = wd_d.t[ex].rearrange("(c p) d -> p c d", p=128)
        for g in range(NFC // 2):
            b = it % NB
            it += 1
            T.dma("pool", wgb[b][:], wgv[:, :, g * 256:(g + 1) * 256], writes=[wgb[b]])
            T.dma("pool", wub[b][:], wuv[:, :, g * 256:(g + 1) * 256], writes=[wub[b]])
            T.dma("pool", wdb[g].t, wdv[:, 2 * g:2 * g + 2, :], writes=[wdb[g]])
            for j in range(2):
                fc = 2 * g + j
                pgb, pub, sgb = pg[fc % 2], pu[fc % 2], sg[fc % 2]
                for k_ in range(8):
                    T.mm(pgb, lambda e: e.matmul(pgb[:, :NS], lhsT=wgb[b][:, k_, j * 128:(j + 1) * 128], rhs=xT[:, k_, :], start=(k_ == 0), stop=(k_ == 7)),
                         [wgb[b], xT], k_ == 0, k_ == 7)
                for k_ in range(8):
                    T.mm(pub, lambda e: e.matmul(pub[:, :NS], lhsT=wub[b][:, k_, j * 128:(j + 1) * 128], rhs=xT[:, k_, :], start=(k_ == 0), stop=(k_ == 7)),
                         [wub[b], xT], k_ == 0, k_ == 7)
                T.op("act", lambda e: e.activation(out=sgb[:, :NS], in_=pgb[:, :NS], func=AF.Silu), reads=[pgb], writes=[sgb])
                T.op("dve", lambda e: e.tensor_tensor(out=hid[fc][:], in0=sgb[:, :NS], in1=pub[:, :NS], op=ALU.mult), reads=[sgb, pub], writes=[hid[fc]])
        for (ts, tn) in ttiles:
            yb = ysb[yi % 2]
            yi += 1
            for dh in range(2):
                p = py[dh]
                for fc in range(NFC):
                    T.mm(p, lambda e: e.matmul(p[:tn, :], lhsT=hid[fc][:, ts:ts + tn], rhs=wdbt[:, fc, dh * 512:(dh + 1) * 512], start=(fc == 0), stop=(fc == NFC - 1)),
                         [hid[fc], wdb[fc // 2]], fc == 0, fc == NFC - 1)
                if dh == 0:
                    T.op("act", lambda e: e.copy(out=yb[:tn, 0:512], in_=p[:tn, :]), reads=[p], writes=[yb])
                else:
                    T.op("dve", lambda e: e.tensor_copy(out=yb[:tn, 512:1024], in_=p[:tn, :]), reads=[p], writes=[yb])
            T.dma("sp", y_d.t[ex, ts:ts + tn, :], yb[:tn, :], reads=[yb], writes=[y_d])


def build_fused():
    nc = new_nc()
    with ExitStack() as es:
        C = Ctx(nc, es)
        T = C.T
        I = {}

        def inp(name, shape, dt=F32):
            I[name] = Buf(nc.dram_tensor(name, list(shape), dt, kind="ExternalInput").ap(), name)
            return I[name]
        xin = inp("xin", [NTOK, D])
        ccT = inp("ccT", [D, 2])
        inp("ada_w", [2, D, 6 * D]); inp("ada_b", [2, 6 * D]); inp("norm1_g", [2, D]); inp("norm2_g", [2, D])
        inp("att_w_in", [D, 2208]); inp("qng", [384]); inp("q_up", [384, 768]); inp("kvng", [256]); inp("kv_up", [256, 1024])
        inp("lamv", [4, 64]); inp("subg", [128]); inp("att_w_out", [D, D])
        inp("rec_w_in", [D, REC_IN]); inp("gk_up", [2, 16, 256]); inp("gk_bias", [2, 256]); inp("gla_g", [128]); inp("hg_g", [128])
        inp("lb_logits", [2, 2, 512]); inp("rec_w_out", [D, D]); inp("router", [2, D, NE])
        inp("wg", [2, NE, D, FF]); inp("wu", [2, NE, D, FF]); inp("wd", [2, NE, FF, D]); inp("fg", [D])
        out_d = Buf(nc.dram_tensor("out", [SEQ, D], F32, kind="ExternalOutput").ap(), "out")
        mods = C.scratch("mods_scr", [2, 2, 6 * D], F32)
        xmid0 = C.scratch("xmid0_scr", [NTOK, D], F32)
        xe0 = C.scratch("xe0_scr", [NE, D, 288], BF16)
        meta0 = C.scratch("meta0_scr", [4, NE, 288], F32)
        y0 = C.scratch("y0_scr", [NE, 288, D], F32)
        xmid1 = C.scratch("xmid1_scr", [NTOK, D], F32)
        xe1 = C.scratch("xe1_scr", [NE, D, 256], BF16)
        meta1 = C.scratch("meta1_scr", [4, NE, 256], F32)
        y1 = C.scratch("y1_scr", [NE, 256, D], F32)
        sub = lambda b, i: Buf(b.t[i])
        with scope(C):
            body_ada_full(C, ccT, I["ada_w"], I["ada_b"], mods)
        with scope(C):
            C.over = {"xin": xin, "mod": sub(mods, 0), "n1g": sub(I["norm1_g"], 0), "n2g": sub(I["norm2_g"], 0), "w_in": I["att_w_in"],
                      "qng": I["qng"], "q_up": I["q_up"], "kvng": I["kvng"], "kv_up": I["kv_up"], "lamv": I["lamv"], "subg": I["subg"],
                      "w_out": I["att_w_out"], "router": sub(I["router"], 0), "xmid": xmid0, "xeT": xe0, "meta": meta0}
            body_attn(C)
        with scope(C):
            body_ffn_bp(C, xe0, sub(I["wg"], 0), sub(I["wu"], 0), sub(I["wd"], 0), y0, 288)
        with scope(C):
            C.over = {"xmid": xmid0, "y": y0, "meta_in": meta0, "mod0": sub(mods, 0), "mod": sub(mods, 1), "n1g": sub(I["norm1_g"], 1),
                      "n2g": sub(I["norm2_g"], 1), "w_in": I["rec_w_in"], "gk_up": I["gk_up"], "gk_bias": I["gk_bias"], "gla_g": I["gla_g"],
                      "hg_g": I["hg_g"], "lb_logits": I["lb_logits"], "w_out": I["rec_w_out"], "router": sub(I["router"], 1),
                      "xmid1": xmid1, "xeT": xe1, "meta": meta1}
            body_rec(C)
        with scope(C):
            body_ffn_bp(C, xe1, sub(I["wg"], 1), sub(I["wu"], 1), sub(I["wd"], 1), y1, 256)
        with scope(C):
            C.over = {"xmid": xmid1, "y": y1, "meta_in": meta1, "mod": sub(mods, 1), "fg": I["fg"], "out": out_d}
            body_final(C)
        C.over = None
    return nc


def kernel_fused(x, c, ctx, c_ctx, ada_w, ada_b, norm1_g, norm2_g, att_w_in, mla_q_norm_g, mla_q_up,
                 mla_kv_norm_g, mla_kv_up, da_lam_q1, da_lam_k1, da_lam_q2, da_lam_k2, da_subln_g, att_w_out,
                 rec_w_in, gla_gk_up, gla_gk_bias, gla_norm_g, hg_lb_logits, hg_norm_g, rec_w_out,
                 moe_router, moe_w_gate, moe_w_up, moe_w_down, final_norm_g, cores=None):
    f = lambda a: np.ascontiguousarray(np.asarray(a, dtype=np.float32))
    x, c, ctx, c_ctx = f(x), f(c), f(ctx), f(c_ctx)
    shared = {
        "ada_w": f(ada_w), "ada_b": f(ada_b), "norm1_g": f(norm1_g), "norm2_g": f(norm2_g), "att_w_in": f(att_w_in)[0],
        "qng": f(mla_q_norm_g)[0], "q_up": f(mla_q_up)[0], "kvng": f(mla_kv_norm_g)[0], "kv_up": f(mla_kv_up)[0],
        "lamv": np.stack([f(da_lam_q1)[0], f(da_lam_k1)[0], f(da_lam_q2)[0], f(da_lam_k2)[0]]), "subg": f(da_subln_g)[0],
        "att_w_out": f(att_w_out)[0], "rec_w_in": f(rec_w_in)[0], "gk_up": f(gla_gk_up)[0], "gk_bias": f(gla_gk_bias)[0],
        "gla_g": f(gla_norm_g)[0], "hg_g": f(hg_norm_g)[0], "lb_logits": f(hg_lb_logits), "rec_w_out": f(rec_w_out)[0],
        "router": f(moe_router), "wg": f(moe_w_gate), "wu": f(moe_w_up), "wd": f(moe_w_down), "fg": f(final_norm_g),
    }
    cores = list(range(NCORES)) if cores is None else cores
    ims = []
    for b in cores:
        im = dict(shared)
        im["xin"] = np.concatenate([ctx[b], x[b]], axis=0)
        im["ccT"] = np.ascontiguousarray(np.stack([c[b], c_ctx], axis=1))
        ims.append(im)
    nc = _get("fused", build_fused)
    res = run_bass_kernel_spmd(nc, ims, core_ids=list(range(len(cores)))).results
    return np.stack([r["out"] for r in res]).astype(np.float32)


def kernel(**inputs):
    return kernel_fused(**inputs)
```
